# Optimizing a Trainium2 kernel written in Bass

```python
import jax
import jax.numpy as jnp
from jax import lax
import numpy as np

D_MODEL = 1024
BATCH = 8
SEQ = 4096
DEPTH = 2

GRID_W = 64
CTX_LEN = 256

POOL_GROUPS = 4
POOL_WINDOWS = (2, 4, 8, 16)
POOL_WIDTH = D_MODEL // 2
POOL_GDIM = POOL_WIDTH // POOL_GROUPS

HG_HEADS = 4
HG_DK = 128
HG_DV = D_MODEL // 2 // HG_HEADS
HG_KW = HG_HEADS * HG_DK
HG_VW = HG_HEADS * HG_DV
HG_CHUNK = 16

NA_HEADS = 8
NA_HD = D_MODEL // 2 // NA_HEADS
NA_W = NA_HEADS * NA_HD
NA_ROWS = 8
NA_COLS = 16

N_BRANCH = 3

N_GROUPS = 4
EXP_PER_GROUP = 8
N_EXPERTS = N_GROUPS * EXP_PER_GROUP
TOP_K = 2
D_EXPERT = D_MODEL // 2

LN_EPS = 1e-5
RMS_EPS = 1e-6

OFF_A = 0
OFF_Q = OFF_A + POOL_WIDTH
OFF_FF = OFF_Q + HG_KW
OFF_FB = OFF_FF + HG_KW
OFF_I = OFF_FB + HG_KW
OFF_G = OFF_I + HG_VW
OFF_NA = OFF_G + HG_VW
OFF_GATE = OFF_NA + 3 * NA_W
D_IN = OFF_GATE + N_BRANCH * D_MODEL

kernel_name = "hybrid_pool_hgrn2_natten_hmoe_prefix_dit"


def _layernorm(x, g=None, b=None):
    xf = x.astype(jnp.float32)
    mu = jnp.mean(xf, axis=-1, keepdims=True)
    var = jnp.mean(jnp.square(xf - mu), axis=-1, keepdims=True)
    y = (xf - mu) * lax.rsqrt(var + LN_EPS)
    if g is not None:
        y = y * g.astype(jnp.float32) + b.astype(jnp.float32)
    return y.astype(x.dtype)


def _modulate(x, shift, scale):
    return _layernorm(x) * (1 + scale) + shift


def _heads(a, n):
    bsz, t_len, _ = a.shape
    return a.reshape(bsz, t_len, n, -1).transpose(0, 2, 1, 3)


def _merge_heads(a):
    bsz, nh, t_len, d = a.shape
    return a.transpose(0, 2, 1, 3).reshape(bsz, t_len, nh * d)


def _split_in(z):
    return (z[..., OFF_A:OFF_Q], z[..., OFF_Q:OFF_FF], z[..., OFF_FF:OFF_FB], z[..., OFF_FB:OFF_I],
            z[..., OFF_I:OFF_G], z[..., OFF_G:OFF_NA], z[..., OFF_NA:OFF_GATE], z[..., OFF_GATE:])


def _pool_mix(u, w_pool, pool_scale):
    bsz, t_len, _ = u.shape
    uf = u.reshape(bsz, t_len, POOL_GROUPS, POOL_GDIM).astype(jnp.float32)
    cs = jnp.concatenate([jnp.zeros_like(uf[:, :1]), lax.cumsum(uf, axis=1)], axis=1)
    pos = np.arange(t_len)
    means = []
    for g, win in enumerate(POOL_WINDOWS):
        lo = np.clip(pos - win // 2, 0, t_len)
        hi = np.clip(pos - win // 2 + win, 0, t_len)
        cnt = (hi - lo).astype(np.float32)[None, :, None]
        means.append((cs[:, hi, g] - cs[:, lo, g]) / cnt)
    y = (jnp.stack(means, axis=2) - uf).astype(u.dtype)
    y = jnp.einsum("btgc,gcd->btgd", y, w_pool)
    return y.reshape(bsz, t_len, POOL_WIDTH) * pool_scale


def _lower_bounds(logits):
    p = jax.nn.softmax(logits.astype(jnp.float32), axis=0)
    return jnp.cumsum(p, axis=0) - p[:1]


def _hgrn_inputs(zq, zf, zi, lb):
    zf = zf.astype(jnp.float32)
    sig = jax.nn.sigmoid(zf)
    log_f = jnp.log(lb + (1 - lb) * sig)
    k = (1 - lb) * jax.nn.sigmoid(-zf)
    q = jax.nn.silu(zq.astype(jnp.float32))
    return (_heads(q, HG_HEADS), _heads(k, HG_HEADS), _heads(zi.astype(jnp.float32), HG_HEADS), _heads(log_f, HG_HEADS))


def _hgrn_scan(q, k, v, log_f, s0):
    bsz, nh, t_len, _ = q.shape
    nc = t_len // HG_CHUNK

    def to_chunks(a):
        return jnp.moveaxis(a.reshape(bsz, nh, nc, HG_CHUNK, a.shape[-1]), 2, 0)

    mask = jnp.tril(jnp.ones((HG_CHUNK, HG_CHUNK), dtype=bool))

    def step(state, inp):
        qc, kc, vc, lfc = inp
        b = jnp.cumsum(lfc, axis=2)
        q_dec = qc * jnp.exp(b)
        a_mat = jnp.einsum("bhtk,bhsk->bhts", q_dec, kc * jnp.exp(-b))
        a_mat = jnp.where(mask, a_mat, 0.0)
        o = jnp.einsum("bhts,bhsv->bhtv", a_mat, vc) + jnp.einsum("bhtk,bhkv->bhtv", q_dec, state)
        b_last = b[:, :, -1:]
        state = (jnp.exp(b_last[:, :, 0])[..., None] * state
                 + jnp.einsum("bhsk,bhsv->bhkv", kc * jnp.exp(b_last - b), vc))
        return state, o

    s_fin, o = lax.scan(step, s0, (to_chunks(q), to_chunks(k), to_chunks(v), to_chunks(log_f)))
    return jnp.moveaxis(o, 0, 2).reshape(bsz, nh, t_len, -1), s_fin


def _hgrn_direction(lat, ctx, reverse):
    if reverse:
        lat = tuple(jnp.flip(a, axis=2) for a in lat)
        ctx = tuple(jnp.flip(a, axis=2) for a in ctx)
    s0 = jnp.zeros((lat[0].shape[0], HG_HEADS, HG_DK, HG_DV), jnp.float32)
    o_ctx, s_ctx = _hgrn_scan(*ctx, s0)
    o_lat, _ = _hgrn_scan(*lat, s_ctx)
    if reverse:
        o_lat = jnp.flip(o_lat, axis=2)
        o_ctx = jnp.flip(o_ctx, axis=2)
    return o_lat, o_ctx


def _hgrn_readout(o, zg, gain):
    o = o * lax.rsqrt(jnp.mean(o * o, axis=-1, keepdims=True) + RMS_EPS) * gain[None, :, None, :].astype(jnp.float32)
    return _merge_heads(o).astype(zg.dtype) * jax.nn.silu(zg)


def _na_latent(q, k, v, kc, vc, rpb):
    bsz, nh, t_len, hd = q.shape
    rows = t_len // GRID_W
    kr = min(NA_ROWS, rows)
    scale = hd ** -0.5
    qg = q.reshape(bsz, nh, rows, GRID_W, hd)
    kg = k.reshape(bsz, nh, rows, GRID_W, hd)
    vg = v.reshape(bsz, nh, rows, GRID_W, hd)
    col = np.arange(GRID_W)
    c0 = np.clip(col - NA_COLS // 2, 0, GRID_W - NA_COLS)
    col_idx = c0[:, None] + np.arange(NA_COLS)[None, :]
    dc = col_idx - col[:, None] + (NA_COLS - 1)
    rpb_c = rpb[:, :, dc]
    n_loc = kr * NA_COLS

    def row_block(r):
        r0 = jnp.clip(r - kr // 2, 0, rows - kr)
        qr = lax.dynamic_index_in_dim(qg, r, axis=2, keepdims=False)
        kn = lax.dynamic_slice_in_dim(kg, r0, kr, axis=2)[:, :, :, col_idx]
        vn = lax.dynamic_slice_in_dim(vg, r0, kr, axis=2)[:, :, :, col_idx]
        dr = r0 + jnp.arange(kr) - r + (NA_ROWS - 1)
        bias = jnp.transpose(rpb_c[:, dr], (0, 2, 1, 3))
        s_loc = jnp.einsum("bhqd,bhrqcd->bhqrc", qr, kn) * scale + bias[None]
        s_ctx = jnp.einsum("bhqd,bhld->bhql", qr, kc) * scale
        s = jnp.concatenate([s_loc.reshape(bsz, nh, GRID_W, n_loc), s_ctx], axis=-1)
        p = jax.nn.softmax(s.astype(jnp.float32), axis=-1).astype(v.dtype)
        p_loc = p[..., :n_loc].reshape(bsz, nh, GRID_W, kr, NA_COLS)
        return (jnp.einsum("bhqrc,bhrqcd->bhqd", p_loc, vn)
                + jnp.einsum("bhql,bhld->bhqd", p[..., n_loc:], vc))

    out = lax.map(row_block, jnp.arange(rows))
    return jnp.moveaxis(out, 0, 2).reshape(bsz, nh, t_len, hd)


def _ctx_attn(qc, kc, vc):
    s = jnp.einsum("bhqd,bhkd->bhqk", qc, kc) * (qc.shape[-1] ** -0.5)
    p = jax.nn.softmax(s.astype(jnp.float32), axis=-1).astype(vc.dtype)
    return jnp.einsum("bhqk,bhkd->bhqd", p, vc)


def _merge_branches(ya, yb, yc, zgate, w_br_a, w_br_b, w_br_c, w_out):
    gates = jax.nn.sigmoid(zgate.reshape(zgate.shape[:-1] + (N_BRANCH, D_MODEL)))
    m = (gates[..., 0, :] * (ya @ w_br_a) + gates[..., 1, :] * (yb @ w_br_b)
         + gates[..., 2, :] * (yc @ w_br_c))
    return m @ w_out


def _token_mixer(h, hc, w_in, w_pool, pool_scale, lb_f, lb_b, hg_gain, rpb,
                 w_br_a, w_br_b, w_br_c, w_out, with_ctx_out):
    za, zq, zff, zfb, zi, zg, zqkv, zgate = _split_in(h @ w_in)
    cza, czq, czff, czfb, czi, czg, czqkv, czgate = _split_in(hc @ w_in)
    o_f, oc_f = _hgrn_direction(_hgrn_inputs(zq, zff, zi, lb_f), _hgrn_inputs(czq, czff, czi, lb_f), reverse=False)
    o_b, oc_b = _hgrn_direction(_hgrn_inputs(zq, zfb, zi, lb_b), _hgrn_inputs(czq, czfb, czi, lb_b), reverse=True)
    yb = _hgrn_readout(o_f + o_b, zg, hg_gain)
    q, k, v = [_heads(a, NA_HEADS) for a in jnp.split(zqkv, 3, axis=-1)]
    qc, kc, vc = [_heads(a, NA_HEADS) for a in jnp.split(czqkv, 3, axis=-1)]
    yc = _merge_heads(_na_latent(q, k, v, kc, vc, rpb))
    ya = _pool_mix(za, w_pool, pool_scale)
    mix = _merge_branches(ya, yb, yc, zgate, w_br_a, w_br_b, w_br_c, w_out)
    if not with_ctx_out:
        return mix, None
    yac = _pool_mix(cza, w_pool, pool_scale)
    ybc = _hgrn_readout(oc_f + oc_b, czg, hg_gain)
    ycc = _merge_heads(_ctx_attn(qc, kc, vc))
    mixc = _merge_branches(yac, ybc, ycc, czgate, w_br_a, w_br_b, w_br_c, w_out)
    return mix, mixc


def _hier_moe(h, w_rg, b_rg, w_re, b_re, w_gate, w_up, w_down):
    shape = h.shape
    hf = h.reshape(-1, D_MODEL)
    n_tok = hf.shape[0]
    lg = (hf @ w_rg + b_rg).astype(jnp.float32)
    p_grp, grp = lax.top_k(jax.nn.softmax(lg, axis=-1), 1)
    le = (hf @ w_re + b_re).astype(jnp.float32).reshape(n_tok, N_GROUPS, EXP_PER_GROUP)
    le_sel = jnp.einsum("nge,ng->ne", le, jax.nn.one_hot(grp[:, 0], N_GROUPS, dtype=jnp.float32))
    p_top, e_top = lax.top_k(jax.nn.softmax(le_sel, axis=-1), TOP_K)
    w_tok = p_grp * p_top / jnp.sum(p_top, axis=-1, keepdims=True)
    expert_id = grp * EXP_PER_GROUP + e_top
    combine = jnp.sum(jax.nn.one_hot(expert_id, N_EXPERTS, dtype=jnp.float32) * w_tok[..., None], axis=1)
    out = jnp.zeros((n_tok, D_MODEL), jnp.float32)
    for e in range(N_EXPERTS):
        a = jax.nn.silu(hf @ w_gate[e]) * (hf @ w_up[e])
        out = out + combine[:, e:e + 1] * (a @ w_down[e])
    return out.astype(h.dtype).reshape(shape)


def setup_inputs(seed: int = 0) -> dict:
    key = jax.random.key(seed)
    ks = jax.random.split(key, 32)
    f32 = jnp.float32
    beta = (8.0 * DEPTH) ** -0.25

    def nrm(k, shape, s):
        return jax.random.normal(k, shape, f32) * s

    dm = D_MODEL
    return {
        "x": nrm(ks[0], (BATCH, SEQ, dm), 1.0),
        "c": nrm(ks[1], (BATCH, dm), 1.0),
        "ctx": nrm(ks[2], (BATCH, CTX_LEN, dm), 1.0),
        "c_ctx": nrm(ks[3], (dm,), 1.0),
        "w_ada": nrm(ks[4], (DEPTH, dm, 6 * dm), dm ** -0.5),
        "b_ada": nrm(ks[5], (DEPTH, 6 * dm), 0.02),
        "w_in": nrm(ks[6], (DEPTH, dm, D_IN), dm ** -0.5),
        "w_pool": nrm(ks[7], (DEPTH, POOL_GROUPS, POOL_GDIM, POOL_GDIM), POOL_GDIM ** -0.5),
        "pool_scale": 1.0 + nrm(ks[8], (DEPTH, POOL_WIDTH), 0.1),
        "lb_logits_fwd": nrm(ks[9], (DEPTH, HG_KW), 0.5),
        "lb_logits_bwd": nrm(ks[10], (DEPTH, HG_KW), 0.5),
        "hg_gain": 1.0 + nrm(ks[11], (DEPTH, HG_HEADS, HG_DV), 0.02),
        "rpb": nrm(ks[12], (DEPTH, NA_HEADS, 2 * NA_ROWS - 1, 2 * NA_COLS - 1), 0.1),
        "w_br_a": nrm(ks[13], (DEPTH, POOL_WIDTH, dm), POOL_WIDTH ** -0.5),
        "w_br_b": nrm(ks[14], (DEPTH, HG_VW, dm), HG_VW ** -0.5),
        "w_br_c": nrm(ks[15], (DEPTH, NA_W, dm), NA_W ** -0.5),
        "w_out": nrm(ks[16], (DEPTH, dm, dm), dm ** -0.5 * beta),
        "ln1_g": 1.0 + nrm(ks[17], (DEPTH, dm), 0.02),
        "ln1_b": nrm(ks[18], (DEPTH, dm), 0.02),
        "w_rg": nrm(ks[19], (DEPTH, dm, N_GROUPS), dm ** -0.5),
        "b_rg": nrm(ks[20], (DEPTH, N_GROUPS), 0.01),
        "w_re": nrm(ks[21], (DEPTH, dm, N_EXPERTS), dm ** -0.5),
        "b_re": nrm(ks[22], (DEPTH, N_EXPERTS), 0.01),
        "w_gate": nrm(ks[23], (DEPTH, N_EXPERTS, dm, D_EXPERT), dm ** -0.5),
        "w_up": nrm(ks[24], (DEPTH, N_EXPERTS, dm, D_EXPERT), dm ** -0.5),
        "w_down": nrm(ks[25], (DEPTH, N_EXPERTS, D_EXPERT, dm), D_EXPERT ** -0.5 * beta),
        "ln2_g": 1.0 + nrm(ks[26], (DEPTH, dm), 0.02),
        "ln2_b": nrm(ks[27], (DEPTH, dm), 0.02),
    }


def reference(x, c, ctx, c_ctx, w_ada, b_ada, w_in, w_pool, pool_scale, lb_logits_fwd, lb_logits_bwd,
              hg_gain, rpb, w_br_a, w_br_b, w_br_c, w_out, ln1_g, ln1_b, w_rg, b_rg, w_re, b_re,
              w_gate, w_up, w_down, ln2_g, ln2_b):
    alpha = (2.0 * DEPTH) ** 0.25
    lb_f_all = _lower_bounds(lb_logits_fwd)
    lb_b_all = _lower_bounds(lb_logits_bwd)
    silu_c = jax.nn.silu(c)
    silu_cc = jax.nn.silu(c_ctx)
    xc = ctx
    for l in range(DEPTH):
        last = l == DEPTH - 1
        ada = silu_c @ w_ada[l] + b_ada[l]
        adac = silu_cc @ w_ada[l] + b_ada[l]
        sh1, sc1, g1, sh2, sc2, g2 = [a[:, None, :] for a in jnp.split(ada, 6, axis=-1)]
        sh1c, sc1c, g1c, sh2c, sc2c, g2c = jnp.split(adac, 6, axis=-1)
        h = _modulate(x, sh1, sc1)
        hc = _modulate(xc, sh1c, sc1c)
        mix, mixc = _token_mixer(h, hc, w_in[l], w_pool[l], pool_scale[l], lb_f_all[l], lb_b_all[l],
                                 hg_gain[l], rpb[l], w_br_a[l], w_br_b[l], w_br_c[l], w_out[l],
                                 with_ctx_out=not last)
        x = _layernorm(alpha * x + g1 * mix, ln1_g[l], ln1_b[l])
        h = _modulate(x, sh2, sc2)
        moe = _hier_moe(h, w_rg[l], b_rg[l], w_re[l], b_re[l], w_gate[l], w_up[l], w_down[l])
        x = _layernorm(alpha * x + g2 * moe, ln2_g[l], ln2_b[l])
        if not last:
            xc = _layernorm(alpha * xc + g1c * mixc, ln1_g[l], ln1_b[l])
            hc = _modulate(xc, sh2c, sc2c)
            moec = _hier_moe(hc, w_rg[l], b_rg[l], w_re[l], b_re[l], w_gate[l], w_up[l], w_down[l])
            xc = _layernorm(alpha * xc + g2c * moec, ln2_g[l], ln2_b[l])
    return x
```

```python
import numpy as np
from contextlib import ExitStack
import concourse.bass as bass
import concourse.mybir as mybir
from concourse.bass_utils import run_bass_kernel_spmd

F32 = mybir.dt.float32
BF16 = mybir.dt.bfloat16
AF = mybir.ActivationFunctionType
ALU = mybir.AluOpType
AX = mybir.AxisListType

D = 1024
T = 4096
TC = 256
TT = T + TC
NT = TT // 128
DIN = 7680
ALPHA = (2.0 * 2) ** 0.25
NEG = -30000.0
PADL = 4480


class Ctx:
    def __init__(self, nc, es):
        self.nc = nc
        self.eng = {'pe': nc.tensor, 'act': nc.scalar, 'dve': nc.vector, 'pool': nc.gpsimd, 'sp': nc.sync}
        self.sem = {}
        self.cnt = {}
        for n in ['pe', 'act', 'dve', 'pool']:
            self.sem[n] = es.enter_context(nc.semaphore('s_' + n))
            self.cnt[n] = 0
        self.ring = {}
        self.ringpos = {}
        for q in ['sp', 'act', 'pool']:
            self.ring[q] = []
            for i in range(16):
                nm = 'd_%s%d' % (q, i)
                self.sem[nm] = es.enter_context(nc.semaphore(nm))
                self.cnt[nm] = 0
                self.ring[q].append(nm)
            self.ringpos[q] = 0
        self.seen = {e: {} for e in self.eng}
        self.lastw = {}
        self.readers = {}
        self.pending = {e: False for e in self.eng}
        self.nwaits = 0
        self.nins = 0

    def _wait(self, e, s, v):
        if self.seen[e].get(s, 0) >= v:
            return
        self.eng[e].wait_ge(self.sem[s], v)
        self.seen[e][s] = v
        self.nwaits += 1

    def _deps(self, e, reads, writes, is_dma):
        for r in reads:
            lw = self.lastw.get(r)
            if lw is not None:
                if lw[2] == 'pe' and e == 'pe' and not is_dma:
                    continue
                self._wait(e, lw[0], lw[1])
        for w in writes:
            lw = self.lastw.get(w)
            if lw is not None and (is_dma or lw[2] != e or lw[3]):
                self._wait(e, lw[0], lw[1])
            for (s, v, re, rdma) in self.readers.get(w, {}).values():
                if is_dma or rdma or re != e:
                    self._wait(e, s, v)

    def _commit(self, e, reads, writes, s, v, is_dma):
        for r in reads:
            self.readers.setdefault(r, {})[s] = (s, v, e, is_dma)
        for w in writes:
            self.lastw[w] = (s, v, e, is_dma)
            self.readers[w] = {}

    def op(self, e, fn, reads=(), writes=(), sig=True):
        self._deps(e, reads, writes, False)
        ins = fn(self.eng[e])
        if sig:
            self.cnt[e] += 1
            ins.then_inc(self.sem[e], 1)
            v = self.cnt[e]
            self.pending[e] = False
        else:
            v = self.cnt[e] + 1
            self.pending[e] = True
        self._commit(e, reads, writes, e, v, False)
        self.nins += 1
        return ins

    def dma(self, q, out, in_, reads=(), writes=(), **kw):
        self._deps(q, reads, writes, True)
        s = self.ring[q][self.ringpos[q] % len(self.ring[q])]
        self.ringpos[q] += 1
        if self.cnt[s] > 0:
            self._wait(q, s, self.cnt[s])
        ins = self.eng[q].dma_start(out=out, in_=in_, **kw)
        self.cnt[s] += 16
        ins.then_inc(self.sem[s], 16)
        self._commit(q, reads, writes, s, self.cnt[s], True)
        self.nins += 1
        return ins

    def barrier(self):
        for e in self.eng:
            assert not self.pending[e]
        for e in self.eng:
            for s in self.sem:
                if self.cnt[s] > 0:
                    self._wait(e, s, self.cnt[s])
        self.lastw = {}
        self.readers = {}


def build(dbg=False, nlayers=2, stop=None, lite=False, skip01=False):
    nc = bass.Bass("TRN2", target_bir_lowering=False)

    def din(name, shape, dt=F32):
        return nc.dram_tensor(name, list(shape), dt, kind="ExternalInput").ap()

    def scr(name, shape, dt):
        return nc.dram_tensor(name, list(shape), dt, kind=("ExternalOutput" if dbg else "Internal")).ap()

    x_in = din("x", [T, D])
    ctx_in = din("ctx", [TC, D])
    ccol_in = din("ccol", [128, 8, 2])
    w_ada = din("w_ada", [2, D, 6 * D])
    b_ada = din("b_ada", [2, 6 * D])
    b_ada_col = din("b_ada_col", [2, 128, 48])
    w_in = din("w_in", [2, D, DIN])
    w_pool = din("w_pool", [2, 4, 128, 128])
    pscale_col = din("pscale_col", [2, 128, 4])
    lbl_in = din("lbl", [128, 2, 2, 4])
    gain_col = din("gain_col", [2, 128, 4])
    tb_in = din("tb", [2, 8, 128, 8 * 4 * 64])
    w_br_a = din("w_br_a", [2, 512, D])
    w_br_b = din("w_br_b", [2, 512, D])
    w_br_c = din("w_br_c", [2, 512, D])
    w_out = din("w_out", [2, D, D])
    lnp_in = din("lnp", [2, 4, D])
    w_r = din("w_r", [2, D, 36])
    b_r = din("b_r", [2, 36])
    w_gate = din("w_gate", [2, 32, D, 512] if not lite else [2, 1, 1, 1])
    w_up = din("w_up", [2, 32, D, 512] if not lite else [2, 1, 1, 1])
    w_down = din("w_down", [2, 32, 512, D] if not lite else [2, 1, 1, 1])
    ident_in = din("ident", [128, 128])
    masks_in = din("masks", [2, 128, 128], mybir.dt.int32)
    invc_in = din("invc", [4, PADL])
    cm_in = din("cm", [128, 4])
    out = nc.dram_tensor("out", [T, D], F32, kind="ExternalOutput").ap()

    xs = scr("xs", [TT, D], F32)
    zaT = scr("zaT", [512, TT], F32)
    qsT = scr("qsT", [512, TT], F32)
    zfT = scr("zfT", [1024, TT], F32)
    sgT = scr("sgT", [512, TT], BF16)
    qT = scr("qT", [512, TT], BF16)
    kT = scr("kT", [512, TT], BF16)
    gT = scr("gT", [3072, TT], BF16)
    vi = scr("vi", [TT, 512], BF16)
    vv = scr("vv", [TT, 512], BF16)
    yaT = scr("yaT", [512, TT], BF16)
    ybT = scr("ybT", [512, TT], BF16)
    ycT = scr("ycT", [512, TT], BF16)
    h2T = scr("h2T", [D, TT], BF16)
    comb = scr("comb", [TT, 32], F32)

    with ExitStack() as es:
        c = Ctx(nc, es)

        uid = [0]

        def sb(st, name, shape, dt):
            uid[0] += 1
            return st.enter_context(nc.sbuf_tensor("sb%d_%s" % (uid[0], name), list(shape), dt))

        def ps(st, name, shape, dt=F32):
            uid[0] += 1
            return st.enter_context(nc.psum_tensor("ps%d_%s" % (uid[0], name), list(shape), dt))

        ident = sb(es, "ident", [128, 128], BF16)
        ones32 = sb(es, "ones32", [128, 128], F32)
        ones16 = sb(es, "ones16", [128, 128], BF16)
        masks = sb(es, "masks", [128, 2, 128], mybir.dt.int32)
        cm = sb(es, "cm", [128, 4], F32)
        eps_ln = sb(es, "eps_ln", [128, 1], F32)
        eps_rms = sb(es, "eps_rms", [128, 1], F32)
        lbc = sb(es, "lbc", [128, 2, 2, 4], F32)
        omlc = sb(es, "omlc", [128, 2, 2, 4], F32)
        lbl = sb(es, "lbl", [128, 2, 2, 4], F32)
        modc = sb(es, "modc", [128, 48, 2], F32)
        G = sb(es, "G", [128, 2, 2, 1024], F32)
        lnp = sb(es, "lnp", [128, 4, 1024], F32)
        gcol = sb(es, "gcol", [128, 4], F32)
        pscol = sb(es, "pscol", [128, 4], F32)

        c.dma('pool', ident[:], ident_in[:, :], writes=['ident'])
        c.dma('sp', masks[:], masks_in.rearrange("m p q -> p m q"), writes=['masks'])
        c.dma('sp', cm[:], cm_in[:, :], writes=['cm'])
        c.dma('sp', lbl[:], lbl_in[:, :, :, :], writes=['lbl'])
        c.op('dve', lambda v: v.memset(ones32[:], 1.0), writes=['ones32'])
        c.op('dve', lambda v: v.memset(ones16[:], 1.0), writes=['ones16'])
        c.op('dve', lambda v: v.memset(eps_ln[:], 1e-5), writes=['eps_ln'])
        c.op('dve', lambda v: v.memset(eps_rms[:], 1e-6), writes=['eps_rms'])
        c.op('dve', lambda v: v.memset(lbc[:], 0.0), writes=['lbc'])
        c.op('dve', lambda v: v.tensor_tensor(lbl[:, :, 1, :], lbl[:, :, 1, :], lbl[:, :, 0, :], ALU.subtract), reads=['lbl'], writes=['lbl'])
        c.op('act', lambda a: a.activation(lbc[:, :, 1, :], lbl[:, :, 1, :], AF.Sigmoid), reads=['lbl', 'lbc'], writes=['lbc'])
        c.op('dve', lambda v: v.tensor_scalar(omlc[:], lbc[:], -1.0, 1.0, ALU.mult, ALU.add), reads=['lbc'], writes=['omlc'])
        c.dma('sp', xs[0:TC, :], ctx_in[:, :])
        c.dma('sp', xs[TC:TT, :], x_in[:, :])

        rot = {}

        def nxt(name, n):
            i = rot.get(name, 0)
            rot[name] = i + 1
            return i % n

        def ln_stats(st, xap, xkey, slot):
            stt, mv, rs = st['st'][slot], st['mv'][slot], st['rs'][slot]
            for i in range(2):
                c.op('dve', lambda v: v.bn_stats(stt[:, i, :], xap[:, i * 512:(i + 1) * 512]), reads=[xkey], writes=['st%d_%d' % (slot, i)])
            c.op('dve', lambda v: v.bn_aggr(mv[:], stt[:].rearrange("p a b -> p (a b)")), reads=['st%d_0' % slot, 'st%d_1' % slot], writes=['mv%d' % slot])
            c.op('act', lambda a: a.activation(rs[:], mv[:, 1:2], AF.Sqrt, bias=eps_ln[:], scale=1.0), reads=['mv%d' % slot, 'eps_ln'], writes=['rs%d' % slot])
            c.op('dve', lambda v: v.reciprocal(rs[:], rs[:]), reads=['rs%d' % slot], writes=['rs%d' % slot])
            return mv, rs

        def ln_to_hT(st, xap, xkey, dst, dkey, sc_chunk0, sh_chunk0, w, slot=None):
            if slot is None:
                slot = nxt('lnslot', 2)
            mv, rs = ln_stats(st, xap, xkey, slot)
            hn, pT = st['hn'][slot], st['pT'][slot]
            c.op('dve', lambda v: v.tensor_scalar(hn[:], xap, mv[:, 0:1], rs[:], ALU.subtract, ALU.mult), reads=[xkey, 'mv%d' % slot, 'rs%d' % slot], writes=['hn%d' % slot])
            for k in range(8):
                c.op('pe', lambda t: t.transpose(pT[:, k, :], hn[:, k * 128:(k + 1) * 128], ident[:]), reads=['hn%d' % slot, 'ident'], writes=['pT%d' % slot])
            for k in range(8):
                c.op('act', lambda a: a.activation(dst(k), pT[:, k, :], AF.Identity, bias=modc[:, sh_chunk0 + k, w:w + 1], scale=modc[:, sc_chunk0 + k, w:w + 1]),
                     reads=['pT%d' % slot, 'modc'], writes=[dkey])

        def ln_bufs(st, full=True):
            d = {'st': [], 'mv': [], 'rs': [], 'hn': [], 'pT': []}
            for i in range(2):
                d['st'].append(sb(st, "lnst%d" % i, [128, 2, 6], F32))
                d['mv'].append(sb(st, "lnmv%d" % i, [128, 2], F32))
                d['rs'].append(sb(st, "lnrs%d" % i, [128, 1], F32))
                if full:
                    d['hn'].append(sb(st, "lnhn%d" % i, [128, 1024], BF16))
                    d['pT'].append(ps(st, "lnpT%d" % i, [128, 8, 128], BF16))
            return d

        for l in range(nlayers):
            last = (l == 1)
            c.barrier()
            if not skip01:
                with ExitStack() as st:
                    wada = sb(st, "wada", [128, 8, 6144], BF16)
                    ccol = sb(st, "ccol", [128, 8, 2], F32)
                    sc = sb(st, "sc", [128, 8, 2], BF16)
                    scb = sb(st, "scb", [128, 2, 8, 128], BF16)
                    bcol = sb(st, "bcol", [128, 48], F32)
                    bbc = sb(st, "bbc", [128, 2, 1024], F32)
                    pc = ps(st, "pc", [128, 48, 2])
                    pg = [ps(st, "pg%d" % i, [128, 512]) for i in range(2)]
                    for k in range(8):
                        c.dma('pool', wada[:, k, :], w_ada[l, k * 128:(k + 1) * 128, :], writes=['wada%d' % k])
                    c.dma('sp', ccol[:], ccol_in[:, :, :], writes=['ccol'])
                    c.dma('sp', gcol[:], gain_col[l], writes=['gcol'])
                    c.dma('sp', pscol[:], pscale_col[l], writes=['pscol'])
                    c.dma('sp', bcol[:], b_ada_col[l], writes=['bcol'])
                    c.dma('sp', bbc[:, 0, :], b_ada[l, 2048:3072].partition_broadcast(128), writes=['bbc0'])
                    c.dma('sp', bbc[:, 1, :], b_ada[l, 5120:6144].partition_broadcast(128), writes=['bbc1'])
                    for i in range(4):
                        c.dma('sp', lnp[:, i, :], lnp_in[l, i, :].partition_broadcast(128), writes=['lnp'])
                    c.op('act', lambda a: a.activation(sc[:], ccol[:], AF.Silu), reads=['ccol'], writes=['sc'])
                    for w in range(2):
                        for k in range(8):
                            c.op('dve', lambda v: v.tensor_copy(scb[:, w, k, :], sc[:, k, w:w + 1].to_broadcast([128, 128])), reads=['sc'], writes=['scb'])
                    for j in range(48):
                        for k in range(8):
                            c.op('pe', lambda t: t.matmul(pc[:, j, :], wada[:, k, j * 128:(j + 1) * 128], sc[:, k, :], start=(k == 0), stop=(k == 7)),
                                 reads=['wada%d' % k, 'sc'], writes=['pc'], sig=(k == 7))
                    for w in range(2):
                        c.op('dve', lambda v: v.tensor_tensor(modc[:, :, w], pc[:, :, w], bcol[:], ALU.add), reads=['pc', 'bcol'], writes=['modc'])
                    for ch0 in (8, 32):
                        c.op('dve', lambda v: v.tensor_scalar(modc[:, ch0:ch0 + 8, :], modc[:, ch0:ch0 + 8, :], 1.0, None, ALU.add), reads=['modc'], writes=['modc'])
                    for w in range(2):
                        for gi, c0 in enumerate((2048, 5120)):
                            for half in range(2):
                                pi = nxt('pg', 2)
                                for k in range(8):
                                    c.op('pe', lambda t: t.matmul(pg[pi][:], scb[:, w, k, :], wada[:, k, c0 + half * 512:c0 + (half + 1) * 512], start=(k == 0), stop=(k == 7)),
                                         reads=['scb', 'wada%d' % k], writes=['pg%d' % pi], sig=(k == 7))
                                c.op('dve', lambda v: v.tensor_tensor(G[:, gi, w, half * 512:(half + 1) * 512], pg[pi][:], bbc[:, gi, half * 512:(half + 1) * 512], ALU.add),
                                     reads=['pg%d' % pi, 'bbc%d' % gi], writes=['G'])
            if stop == 'p0':
                break

            c.barrier()
            if not skip01:
                with ExitStack() as st:
                    hT = sb(st, "hT", [128, 8, TT], BF16)
                    lb_ = ln_bufs(st)
                    xt = [sb(st, "xt%d" % i, [128, 1024], F32) for i in range(3)]
                    win = [sb(st, "win%d" % i, [128, 8, 512], BF16) for i in range(2)]
                    stg32 = [sb(st, "stg32_%d" % i, [128, 512], F32) for i in range(3)]
                    stg16 = [sb(st, "stg16_%d" % i, [128, 512], BF16) for i in range(3)]
                    pm = [ps(st, "pm%d" % i, [128, 512]) for i in range(4)]
                    def emit_ln(tl):
                        xi = nxt('xt', 3)
                        w = 1 if tl < 2 else 0
                        c.dma('sp', xt[xi][:], xs[tl * 128:(tl + 1) * 128, :], writes=['xt%d' % xi])
                        ln_to_hT(lb_, xt[xi][:], 'xt%d' % xi, lambda k: hT[:, k, tl * 128:(tl + 1) * 128], 'hT%d' % tl, 8, 0, w)
                    groups = [(0, 256)] + [(256 + i * 512, 512) for i in range(8)]
                    gtiles = [list(range(t0_ // 128, (t0_ + n_) // 128)) for (t0_, n_) in groups]
                    for gi_ in range(len(groups)):
                        for tl_ in gtiles[gi_]:
                            emit_ln(tl_)
                    fdst = {0: (zaT, 0, F32, None), 1: (qsT, 0, F32, AF.Silu), 2: (zfT, 0, F32, None), 3: (zfT, 512, F32, None),
                            5: (sgT, 0, BF16, AF.Silu), 6: (qT, 0, BF16, 'q'), 7: (kT, 0, BF16, None)}
                    for i in range(6):
                        fdst[9 + i] = (gT, i * 512, BF16, AF.Sigmoid)
                    for cc in range(15):
                        ws = nxt('win', 2)
                        c.dma('pool', win[ws][:], w_in[l, :, cc * 512:(cc + 1) * 512].rearrange("(k p) n -> p k n", p=128), writes=['win%d' % ws])
                        for gi_, (t0, n) in enumerate(groups):
                            tiles = list(range(t0 // 128, (t0 + n) // 128))
                            hkeys = ['hT%d' % t for t in tiles]
                            if cc in (4, 8):
                                dstd = vi if cc == 4 else vv
                                for tl in tiles:
                                    pi = nxt('pm', 4)
                                    for k in range(8):
                                        c.op('pe', lambda t: t.matmul(pm[pi][:], hT[:, k, tl * 128:(tl + 1) * 128], win[ws][:, k, :], start=(k == 0), stop=(k == 7)),
                                             reads=['hT%d' % tl, 'win%d' % ws], writes=['pm%d' % pi], sig=(k == 7))
                                    si = nxt('stg16', 3)
                                    c.op('dve', lambda v: v.tensor_copy(stg16[si][:], pm[pi][:]), reads=['pm%d' % pi], writes=['stg16_%d' % si])
                                    c.dma('sp', dstd[tl * 128:(tl + 1) * 128, :], stg16[si][:], reads=['stg16_%d' % si])
                            else:
                                dd, r0, dt, fn = fdst[cc]
                                for sub in range(4):
                                    pi = nxt('pm', 4)
                                    for k in range(8):
                                        c.op('pe', lambda t: t.matmul(pm[pi][:, :n], win[ws][:, k, sub * 128:(sub + 1) * 128], hT[:, k, t0:t0 + n], start=(k == 0), stop=(k == 7)),
                                             reads=hkeys + ['win%d' % ws], writes=['pm%d' % pi], sig=(k == 7))
                                    if dt == F32:
                                        si = nxt('stg32', 3)
                                        stg, skey = stg32[si], 'stg32_%d' % si
                                    else:
                                        si = nxt('stg16', 3)
                                        stg, skey = stg16[si], 'stg16_%d' % si
                                    if fn is None:
                                        c.op('dve', lambda v: v.tensor_copy(stg[:, :n], pm[pi][:, :n]), reads=['pm%d' % pi], writes=[skey])
                                    elif fn == 'q':
                                        c.op('act', lambda a: a.activation(stg[:, :n], pm[pi][:, :n], AF.Identity, scale=0.125), reads=['pm%d' % pi], writes=[skey])
                                    else:
                                        c.op('act', lambda a: a.activation(stg[:, :n], pm[pi][:, :n], fn), reads=['pm%d' % pi], writes=[skey])
                                    rr = r0 + sub * 128
                                    c.dma('sp', dd[rr:rr + 128, t0:t0 + n], stg[:, :n], reads=[skey])
            if stop == 'p1':
                break

            slabs = [(0, 256)] + [(256 + i * 512, 512) for i in range(8)]
            SM = 512
            order = [list(range(NT)), [1, 0] + list(range(NT - 1, 1, -1))]
            P2 = {'p2a0': 0, 'p2a1': 1, 'p2a2': 2, 'p2a': 3, 'p2b': 4}.get(stop, 5)
            for h in range(4):
                c.barrier()
                with ExitStack() as st:
                    seg = sb(st, "seg", [128, SM], F32)
                    tmp = [[sb(st, "tmp%d_%d" % (d_, i), [128, SM], F32) for i in range(6)] for d_ in range(2)]
                    tq = [sb(st, "tq%d" % d_, [128, SM], F32) for d_ in range(2)]
                    kdS = [sb(st, "kdS%d" % d_, [128, SM], BF16) for d_ in range(2)]
                    ksS = [sb(st, "ksS%d" % d_, [128, SM], BF16) for d_ in range(2)]
                    qd = [sb(st, "qd%d" % d, [128, TT], BF16) for d in range(2)]
                    AT = [sb(st, "AT%d" % d, [128, NT, 128], BF16) for d in range(2)]
                    ksTm = [sb(st, "ksT%d" % d, [128, NT, 128], BF16) for d in range(2)]
                    vihm = sb(st, "vihm", [128, NT, 4, 128], BF16)
                    gdec = [sb(st, "gdec%d" % d, [128, NT * 4], F32) for d in range(2)]
                    emdec = [sb(st, "emdec%d" % d, [128, NT * 4 + 1], F32) for d in range(2)]
                    vih = sb(st, "vih", [128, NT, 128], BF16)
                    od = [sb(st, "od%d" % d, [128, TT], F32) for d in range(2)]
                    S32 = [[sb(st, "S32_%d_%d" % (d, i), [128, 128], F32) for i in range(2)] for d in range(2)]
                    S16 = [[sb(st, "S16_%d_%d" % (d, i), [128, 128], BF16) for i in range(8)] for d in range(2)]
                    pK = ps(st, "pK", [128, 8, 128], BF16)
                    pO = [ps(st, "pO%d" % d, [128, 512]) for d in range(2)]
                    pKV = [ps(st, "pKV%d" % i, [128, 512]) for i in range(4)]
                    pA = ps(st, "pA", [128, 512])

                    c.op('dve', lambda v: v.memset(seg[:], 1.0), writes=['seg'])
                    for d in range(2):
                        c.op('pool', lambda g_: g_.memset(AT[d][:], 0.0), writes=['AT%d' % d])
                        c.op('dve', lambda v: v.memset(emdec[d][:], 1.0), writes=['emdec%d' % d])
                    c.op('dve', lambda v: v.memset(seg[:].rearrange("p (c k) -> p c k", k=32)[:, :, 0:1], 0.0), writes=['seg'])
                    c.dma('sp', vih[:], vi[:, h * 128:(h + 1) * 128].rearrange("(t p) d -> p t d", p=128), writes=['vih'])
                    for tl in range(NT):
                        for c4 in range(4):
                            if (tl * 4 + c4) % 2 == 0:
                                c.op('act', lambda a: a.activation(vihm[:, tl, c4, :], vih[:, tl, :], AF.Copy, scale=cm[:, c4:c4 + 1]), reads=['vih', 'cm'], writes=['vihm'])
                            else:
                                c.op('dve', lambda v: v.tensor_scalar(vihm[:, tl, c4, :], vih[:, tl, :], cm[:, c4:c4 + 1], None, ALU.mult), reads=['vih', 'cm'], writes=['vihm'])
                    def slab_ops(d, s0, n):
                        lbcol = lbc[:, d, l, h:h + 1]
                        omcol = omlc[:, d, l, h:h + 1]
                        K_ = ['t%d_%d' % (d, i) for i in range(6)]
                        A_, B_, C_, D_, E_, F_ = [t_[:, :n] for t_ in tmp[d]]
                        nch = n // 32
                        c.dma('sp', A_, zfT[d * 512 + h * 128:d * 512 + (h + 1) * 128, s0:s0 + n], writes=[K_[0]])
                        yield
                        c.dma('sp', tq[d][:, :n], qsT[h * 128:(h + 1) * 128, s0:s0 + n], writes=['tq%d' % d])
                        yield
                        c.op('act', lambda a: a.activation(B_, A_, AF.Sigmoid), reads=[K_[0]], writes=[K_[1]])
                        yield
                        c.op('act', lambda a: a.activation(C_, A_, AF.Sigmoid, scale=-1.0), reads=[K_[0]], writes=[K_[2]])
                        yield
                        c.op('act', lambda a: a.activation(B_, B_, AF.Ln, bias=lbcol, scale=omcol), reads=[K_[1], 'lbc', 'omlc'], writes=[K_[1]])
                        yield
                        c.op('dve', lambda v: v.tensor_tensor_scan(D_, seg[:, :n], B_, 0.0, ALU.mult, ALU.add), reads=['seg', K_[1]], writes=[K_[3]])
                        yield
                        D3 = D_.rearrange("p (c k) -> p c k", k=32)
                        if d == 0:
                            bb, bkey = D_, K_[3]
                            b3 = D3
                            blast = D3[:, :, 31:32]
                        else:
                            E3 = E_.rearrange("p (c k) -> p c k", k=32)
                            c.op('dve', lambda v: v.tensor_tensor(E_, B_, D_, ALU.subtract), reads=[K_[1], K_[3]], writes=[K_[4]])
                            yield
                            c.op('dve', lambda v: v.tensor_tensor(E3, E3, D3[:, :, 31:32].to_broadcast([128, nch, 32]), ALU.add), reads=[K_[4], K_[3]], writes=[K_[4]])
                            yield
                            bb, bkey = E_, K_[4]
                            b3 = E3
                            blast = E3[:, :, 0:1]
                        c.op('act', lambda a: a.activation(gdec[d][:, s0 // 32:s0 // 32 + nch], blast.rearrange("p c k -> p (c k)"), AF.Exp), reads=[bkey], writes=['gdec%d' % d])
                        yield
                        A3 = A_.rearrange("p (c k) -> p c k", k=32)
                        c.op('dve', lambda v: v.tensor_tensor(A3, blast.to_broadcast([128, nch, 32]), b3, ALU.subtract), reads=[bkey, K_[0]], writes=[K_[0]])
                        yield
                        c.op('act', lambda a: a.activation(A_, A_, AF.Exp), reads=[K_[0]], writes=[K_[0]])
                        yield
                        B3 = B_.rearrange("p (c k) -> p c k", k=32)
                        c.op('act', lambda a: a.activation(emdec[d][:, s0 // 32:s0 // 32 + nch], b3[:, :, 16:17].rearrange("p c k -> p (c k)"), AF.Exp), reads=[bkey], writes=['emdec%d' % d])
                        yield
                        c.op('dve', lambda v: v.tensor_tensor(B3, b3, b3[:, :, 16:17].to_broadcast([128, nch, 32]), ALU.subtract), reads=[bkey, K_[1]], writes=[K_[1]])
                        yield
                        c.op('act', lambda a: a.activation(F_, B_, AF.Exp, scale=-1.0), reads=[K_[1]], writes=[K_[5]])
                        yield
                        c.op('act', lambda a: a.activation(B_, B_, AF.Exp), reads=[K_[1]], writes=[K_[1]])
                        yield
                        c.op('dve', lambda v: v.tensor_tensor(qd[d][:, s0:s0 + n], tq[d][:, :n], B_, ALU.mult), reads=['tq%d' % d, K_[1]], writes=['qd%d' % d])
                        yield
                        c.op('dve', lambda v: v.scalar_tensor_tensor(kdS[d][:, :n], C_, omcol, F_, ALU.mult, ALU.mult), reads=[K_[2], K_[5], 'omlc'], writes=['kdS%d' % d])
                        yield
                        c.op('dve', lambda v: v.scalar_tensor_tensor(ksS[d][:, :n], C_, omcol, A_, ALU.mult, ALU.mult), reads=[K_[2], K_[0], 'omlc'], writes=['ksS%d' % d])
                        yield
                        for ti in range(n // 128 if P2 != 1 else 0):
                            tl = s0 // 128 + ti
                            c.op('pe', lambda t: t.matmul(pA[:, 0:128], kdS[d][:, ti * 128:(ti + 1) * 128], qd[d][:, tl * 128:(tl + 1) * 128], start=True, stop=True),
                                 reads=['kdS%d' % d, 'qd%d' % d], writes=['pA'])
                            c.op('dve', lambda v: v.copy_predicated(AT[d][:, tl, :], masks[:, d, :], pA[:, 0:128]), reads=['pA', 'masks'], writes=['AT%d' % d])
                            yield
                            c.op('pe', lambda t: t.transpose(pK[:, 0, :], ksS[d][:, ti * 128:(ti + 1) * 128], ident[:]), reads=['ksS%d' % d, 'ident'], writes=['pK'])
                            c.op('act', lambda a: a.copy(ksTm[d][:, tl, :], pK[:, 0, :]), reads=['pK'], writes=['ksTm%d' % d])
                            yield
                    for (s0, n) in slabs:
                        gens = [slab_ops(d_, s0, n) for d_ in range([0, 1, 1, 2, 2, 2][P2])]
                        while gens:
                            for g_ in list(gens):
                                try:
                                    next(g_)
                                except StopIteration:
                                    gens.remove(g_)
                    RING = 8
                    if P2 >= 4:
                        for d in range(2):
                            c.op('dve', lambda v: v.memset(S32[d][0][:], 0.0), writes=['S32_%d_0' % d])
                            c.op('dve', lambda v: v.memset(S16[d][0][:], 0.0), writes=['S16_%d_0' % d])
                        cseq = [[tl_ * 4 + ci_ for tl_ in order[d_] for ci_ in ([0, 1, 2, 3] if d_ == 0 else [3, 2, 1, 0])] + [NT * 4] for d_ in range(2)]

                        def opart(d, s_):
                            tl = order[d][s_]
                            po = pO[d]
                            pok = 'pO%d' % d
                            c.op('pe', lambda t: t.matmul(po[:, 0:128], vih[:, tl, :], AT[d][:, tl, :], start=True, stop=False), reads=['vih', 'AT%d' % d], writes=[pok], sig=False)
                            for n_i in range(4):
                                k = s_ * 4 + n_i
                                cpos = cseq[d][k]
                                ci = cpos % 4
                                c.op('pe', lambda t: t.matmul(po[:, ci * 32:(ci + 1) * 32], S16[d][k % RING][:], qd[d][:, cpos * 32:(cpos + 1) * 32], start=False, stop=(n_i == 3)),
                                     reads=['S16_%d_%d' % (d, k % RING), 'qd%d' % d], writes=[pok], sig=(n_i == 3))
                            c.op('act', lambda a: a.copy(od[d][:, tl * 128:(tl + 1) * 128], po[:, 0:128]), reads=[pok], writes=['od%d' % d])

                        for s_ in range(NT):
                            for d in range(2):
                                tl = order[d][s_]
                                bank = pKV[d * 2 + s_ % 2]
                                bkey = 'pKV%d' % (d * 2 + s_ % 2)
                                for n_i in range(4):
                                    ci = cseq[d][s_ * 4 + n_i] % 4
                                    c.op('pe', lambda t: t.matmul(bank[:, n_i * 128:(n_i + 1) * 128], ksTm[d][:, tl, :], vihm[:, tl, ci, :], start=True, stop=True),
                                         reads=['ksTm%d' % d, 'vihm'], writes=[bkey], sig=(n_i == 3))
                            if s_ >= 1:
                                for d in range(2):
                                    opart(d, s_ - 1)
                            for n_i in range(4):
                                for d in range(2):
                                    bank = pKV[d * 2 + s_ % 2]
                                    bkey = 'pKV%d' % (d * 2 + s_ % 2)
                                    k = s_ * 4 + n_i
                                    cur = k % 2
                                    nx = 1 - cur
                                    cpos = cseq[d][k]
                                    cnext = cseq[d][k + 1]
                                    c.op('dve', lambda v: v.scalar_tensor_tensor(S32[d][nx][:], S32[d][cur][:], gdec[d][:, cpos:cpos + 1], bank[:, n_i * 128:(n_i + 1) * 128], ALU.mult, ALU.add),
                                         reads=['S32_%d_%d' % (d, cur), 'gdec%d' % d, bkey], writes=['S32_%d_%d' % (d, nx)])
                                    c.op('pool', lambda g: g.tensor_scalar(S16[d][(k + 1) % RING][:], S32[d][nx][:], emdec[d][:, cnext:cnext + 1], 1.0, ALU.mult, ALU.mult),
                                         reads=['S32_%d_%d' % (d, nx), 'emdec%d' % d], writes=['S16_%d_%d' % (d, (k + 1) % RING)])
                        for d in range(2):
                            opart(d, NT - 1)
                    blocks = [(i * 512, min(512, TT - i * 512)) for i in range(9)] if P2 >= 5 else []
                    c.barrier()
                    sq = tmp[0][0:2]
                    rt = tmp[0][2:4]
                    sgt = [kdS[0], ksS[0]]
                    ybt = [sb(st, "ybt%d" % i, [128, 512], BF16) for i in range(2)]
                    for (b0, n) in blocks:
                        i2 = nxt('ro', 2)
                        osl = od[0][:, b0:b0 + n]
                        c.dma('sp', sgt[i2][:, :n], sgT[h * 128:(h + 1) * 128, b0:b0 + n], writes=['sgt%d' % i2])
                        c.op('dve', lambda v: v.tensor_tensor(osl, osl, od[1][:, b0:b0 + n], ALU.add), reads=['od0', 'od1'], writes=['od0'])
                        c.op('act', lambda a: a.activation(sq[i2][:, :n], osl, AF.Square), reads=['od0'], writes=['sq%d' % i2])
                        c.op('pe', lambda t: t.matmul(pA[:, :n], ones32[:], sq[i2][:, :n], start=True, stop=True), reads=['ones32', 'sq%d' % i2], writes=['pA'])
                        c.op('act', lambda a: a.activation(rt[i2][:, :n], pA[:, :n], AF.Sqrt, bias=eps_rms[:], scale=1.0 / 128), reads=['pA', 'eps_rms'], writes=['rt%d' % i2])
                        c.op('dve', lambda v: v.reciprocal(rt[i2][:, :n], rt[i2][:, :n]), reads=['rt%d' % i2], writes=['rt%d' % i2])
                        c.op('dve', lambda v: v.tensor_tensor(rt[i2][:, :n], rt[i2][:, :n], osl, ALU.mult), reads=['rt%d' % i2, 'od0'], writes=['rt%d' % i2])
                        c.op('dve', lambda v: v.scalar_tensor_tensor(ybt[i2][:, :n], rt[i2][:, :n], gcol[:, h:h + 1], sgt[i2][:, :n], ALU.mult, ALU.mult),
                             reads=['rt%d' % i2, 'gcol', 'sgt%d' % i2], writes=['ybt%d' % i2])
                        c.dma('sp', ybT[h * 128:(h + 1) * 128, b0:b0 + n], ybt[i2][:, :n], reads=['ybt%d' % i2])
                if P2 < 5:
                    break
            if stop == 'p2' or P2 < 5:
                break
            for h in range(8):
                c.barrier()
                with ExitStack() as st:
                    qh = sb(st, "qh", [64, TT], BF16)
                    kh = sb(st, "kh", [64, TT], BF16)
                    vA = sb(st, "vA", [128, 32, 64], BF16)
                    vB = sb(st, "vB", [128, 31, 64], BF16)
                    vC = sb(st, "vC", [128, 2, 64], BF16)
                    tbh = sb(st, "tbh", [128, 8, 4, 64], BF16)
                    ych = sb(st, "ych", [64, TT], BF16)
                    E = [sb(st, "E%d" % i, [128, 6, 64], BF16) for i in range(2)]
                    rec = [sb(st, "rec%d" % i, [64, 64], F32) for i in range(2)]
                    Esum = [sb(st, "Esum%d" % i, [128, 64], F32) for i in range(2)]
                    E2 = sb(st, "E2", [128, 2, 256], BF16)
                    rec2 = sb(st, "rec2", [64, 256], F32)
                    pS = [ps(st, "pS%d" % i, [128, 512]) for i in range(2)]
                    pN = [ps(st, "pN%d" % i, [128, 512]) for i in range(2)]
                    pS2 = ps(st, "pS2", [128, 512])
                    pN2 = ps(st, "pN2", [128, 512])
                    c.dma('sp', qh[:], qT[h * 64:(h + 1) * 64, :], writes=['qh'])
                    c.dma('sp', kh[:], kT[h * 64:(h + 1) * 64, :], writes=['kh'])
                    c.dma('sp', vA[:], vv[256:4352, h * 64:(h + 1) * 64].rearrange("(t p) d -> p t d", p=128), writes=['vA'])
                    c.dma('sp', vB[:], vv[320:4288, h * 64:(h + 1) * 64].rearrange("(t p) d -> p t d", p=128), writes=['vB'])
                    c.dma('sp', vC[:], vv[0:256, h * 64:(h + 1) * 64].rearrange("(t p) d -> p t d", p=128), writes=['vC'])
                    c.dma('pool', tbh[:], tb_in[l, h].rearrange("p (a b q) -> p a b q", a=8, b=4), writes=['tbh'])
                    c.op('act', lambda a: a.activation(tbh[:], tbh[:], AF.Exp), reads=['tbh'], writes=['tbh'])

                    def att_geom(r):
                        r0 = min(max(r - 4, 0), 56)
                        return r0, r - r0, 256 + r * 64, 256 + r0 * 64

                    def att_qk(r):
                        i2 = r % 2
                        r0, delta, qc0, kb = att_geom(r)
                        pSv = pS[i2][:, 0:384].rearrange("p (j q) -> p j q", q=64)
                        for j in range(4):
                            c.op('pe', lambda t: t.matmul(pSv[:, j, :], kh[:, kb + j * 128:kb + (j + 1) * 128], qh[:, qc0:qc0 + 64], start=True, stop=True),
                                 reads=['kh', 'qh'], writes=['pS%d' % i2], sig=False)
                        for j in range(2):
                            c.op('pe', lambda t: t.matmul(pSv[:, 4 + j, :], kh[:, j * 128:(j + 1) * 128], qh[:, qc0:qc0 + 64], start=True, stop=True),
                                 reads=['kh', 'qh'], writes=['pS%d' % i2], sig=(j == 1))
                        c.op('act', lambda a: a.activation(E[i2][:], pSv, AF.Exp), reads=['pS%d' % i2], writes=['E%d' % i2])
                        c.op('pool', lambda g_: g_.tensor_tensor(E[i2][:, 0:4, :], E[i2][:, 0:4, :], tbh[:, delta, :, :], ALU.mult), reads=['E%d' % i2, 'tbh'], writes=['E%d' % i2])
                        c.op('dve', lambda v: v.tensor_reduce(Esum[i2][:], E[i2][:].rearrange("p j q -> p q j"), AX.X, ALU.add), reads=['E%d' % i2], writes=['Esum%d' % i2])

                    def att_pv(r):
                        i2 = r % 2
                        r0, delta, qc0, kb = att_geom(r)

                        def Vt(j):
                            if j >= 4:
                                return vC[:, j - 4, :]
                            if r0 % 2 == 0:
                                return vA[:, r0 // 2 + j, :]
                            return vB[:, (r0 - 1) // 2 + j, :]
                        pNv = pN[i2][0:64, 0:128].rearrange("p (a q) -> p a q", q=64)
                        for j in range(6):
                            c.op('pe', lambda t: t.matmul(pNv[:, 0, :], Vt(j), E[i2][:, j, :], start=(j == 0), stop=(j == 5)),
                                 reads=['vA', 'vB', 'vC', 'E%d' % i2], writes=['pN%d' % i2], sig=False)
                        c.op('pe', lambda t: t.matmul(pNv[:, 1, :], ones32[:, 0:64], Esum[i2][:], start=True, stop=True), reads=['ones32', 'Esum%d' % i2], writes=['pN%d' % i2])
                        c.op('dve', lambda v: v.reciprocal(rec[i2][:], pNv[:, 1, :]), reads=['pN%d' % i2], writes=['rec%d' % i2])
                        c.op('dve', lambda v: v.tensor_tensor(ych[:, qc0:qc0 + 64], pNv[:, 0, :], rec[i2][:], ALU.mult), reads=['pN%d' % i2, 'rec%d' % i2], writes=['ych'])

                    for r in range(64):
                        att_qk(r)
                        if r >= 1:
                            att_pv(r - 1)
                    att_pv(63)
                    if not last:
                        pS2v = pS2[:, 0:512].rearrange("p (j q) -> p j q", q=256)
                        for j in range(2):
                            c.op('pe', lambda t: t.matmul(pS2v[:, j, :], kh[:, j * 128:(j + 1) * 128], qh[:, 0:256], start=True, stop=True),
                                 reads=['kh', 'qh'], writes=['pS2'], sig=(j == 1))
                        c.op('act', lambda a: a.activation(E2[:], pS2v, AF.Exp), reads=['pS2'], writes=['E2'])
                        pN2v = pN2[0:64, 0:512].rearrange("p (a q) -> p a q", q=256)
                        for j in range(2):
                            c.op('pe', lambda t: t.matmul(pN2v[:, 0, :], vC[:, j, :], E2[:, j, :], start=(j == 0), stop=(j == 1)), reads=['vC', 'E2'], writes=['pN2'], sig=False)
                        for j in range(2):
                            c.op('pe', lambda t: t.matmul(pN2v[:, 1, :], ones16[:, 0:64], E2[:, j, :], start=(j == 0), stop=(j == 1)), reads=['ones16', 'E2'], writes=['pN2'], sig=(j == 1))
                        c.op('dve', lambda v: v.reciprocal(rec2[:], pN2v[:, 1, :]), reads=['pN2'], writes=['rec2'])
                        c.op('dve', lambda v: v.tensor_tensor(ych[:, 0:256], pN2v[:, 0, :], rec2[:], ALU.mult), reads=['pN2', 'rec2'], writes=['ych'])
                    cc0 = 256 if last else 0
                    c.dma('sp', ycT[h * 64:(h + 1) * 64, cc0:TT], ych[:, cc0:TT], reads=['ych'])
            if stop == 'p3':
                break

            groups = [(0, 256)] + [(256 + i * 512, 512) for i in range(8)]
            c.barrier()
            with ExitStack() as st:
                U = sb(st, "U", [128, PADL], F32)
                B1 = sb(st, "B1", [128, PADL], F32)
                B2 = sb(st, "B2", [128, PADL], F32)
                invc = sb(st, "invc", [128, PADL], F32)
                Y = sb(st, "Y", [128, PADL], BF16)
                wp = sb(st, "wp", [128, 4, 128], BF16)
                stgp = [sb(st, "stgp%d" % i, [128, 512], BF16) for i in range(2)]
                pP = [ps(st, "pP%d" % i, [128, 512]) for i in range(2)]
                L = PADL
                c.op('dve', lambda v: v.memset(U[:], 0.0), writes=['U'])
                c.dma('pool', wp[:], w_pool[l].rearrange("g c d -> c g d"), writes=['wp'])
                for g in range(4):
                    c.dma('sp', U[:, 32:288], zaT[g * 128:(g + 1) * 128, 0:256], writes=['U'])
                    c.dma('sp', U[:, 352:4448], zaT[g * 128:(g + 1) * 128, 256:TT], writes=['U'])
                    c.dma('sp', invc[:], invc_in[g].partition_broadcast(128), writes=['invc'])
                    c.op('dve', lambda v: v.tensor_tensor(B1[:, 1:L], U[:, 0:L - 1], U[:, 1:L], ALU.add), reads=['U'], writes=['B1'])
                    S, skey = B1, 'B1'
                    if g >= 1:
                        c.op('dve', lambda v: v.tensor_tensor(B2[:, 2:L - 1], B1[:, 1:L - 2], B1[:, 3:L], ALU.add), reads=['B1'], writes=['B2'])
                        S, skey = B2, 'B2'
                    if g >= 2:
                        c.op('dve', lambda v: v.tensor_tensor(B1[:, 4:L - 3], B2[:, 2:L - 5], B2[:, 6:L - 1], ALU.add), reads=['B2'], writes=['B1'])
                        S, skey = B1, 'B1'
                    if g >= 3:
                        c.op('dve', lambda v: v.tensor_tensor(B2[:, 8:L - 7], B1[:, 4:L - 11], B1[:, 12:L - 3], ALU.add), reads=['B1'], writes=['B2'])
                        S, skey = B2, 'B2'
                    c.op('dve', lambda v: v.tensor_tensor(S[:, 32:4448], S[:, 32:4448], invc[:, 32:4448], ALU.mult), reads=[skey, 'invc'], writes=[skey])
                    c.op('dve', lambda v: v.tensor_tensor(Y[:, 32:4448], S[:, 32:4448], U[:, 32:4448], ALU.subtract), reads=[skey, 'U'], writes=['Y'])
                    for (t0, n) in groups:
                        pc0 = t0 + 32 if t0 < 256 else t0 + 96
                        pi = nxt('pP', 2)
                        c.op('pe', lambda t: t.matmul(pP[pi][:, :n], wp[:, g, :], Y[:, pc0:pc0 + n], start=True, stop=True), reads=['wp', 'Y'], writes=['pP%d' % pi])
                        si = nxt('stgp', 2)
                        c.op('act', lambda a: a.activation(stgp[si][:, :n], pP[pi][:, :n], AF.Identity, scale=pscol[:, g:g + 1]), reads=['pP%d' % pi, 'pscol'], writes=['stgp%d' % si])
                        c.dma('sp', yaT[g * 128:(g + 1) * 128, t0:t0 + n], stgp[si][:, :n], reads=['stgp%d' % si])
            if stop == 'p4':
                break

            c.barrier()
            with ExitStack() as st:
                wa = sb(st, "wa", [128, 4, 1024], BF16)
                wbb = sb(st, "wbb", [128, 4, 1024], BF16)
                wc = sb(st, "wc", [64, 8, 1024], BF16)
                wo = sb(st, "wo", [128, 8, 1024], BF16)
                wr = sb(st, "wr", [128, 8, 36], BF16)
                brb = sb(st, "brb", [128, 36], F32)
                ya = [sb(st, "ya%d" % i, [128, 4, 512], BF16) for i in range(2)]
                yb = [sb(st, "yb%d" % i, [128, 4, 512], BF16) for i in range(2)]
                yc = [sb(st, "yc%d" % i, [64, 8, 512], BF16) for i in range(2)]
                gt = [sb(st, "gt%d" % i, [128, 3, 512], BF16) for i in range(2)]
                m1 = sb(st, "m1", [128, 512], F32)
                m2 = sb(st, "m2", [128, 512], F32)
                m3 = sb(st, "m3", [128, 512], F32)
                mT = sb(st, "mT", [128, 8, 512], BF16)
                xt5 = [sb(st, "xt5_%d" % i, [128, 1024], F32) for i in range(2)]
                tt = [sb(st, "tt%d" % i, [128, 1024], F32) for i in range(2)]
                xr = [sb(st, "xr%d" % i, [128, 1024], F32) for i in range(2)]
                x1 = [sb(st, "x1_%d" % i, [128, 1024], F32) for i in range(2)]
                h2t = [sb(st, "h2t%d" % i, [128, 8, 128], BF16) for i in range(2)]
                lb_ = ln_bufs(st)
                rt_ = [{}, {}]
                for nm, shp in (("lg", [128, 36]), ("mg", [128, 1]), ("nmg", [128, 1]), ("eg", [128, 4]), ("sg", [128, 1]), ("pgv", [128, 1]), ("oh", [128, 4]),
                                ("sel", [128, 8]), ("top8", [128, 8]), ("dlt", [128, 1]), ("e2", [128, 1]), ("den", [128, 1]), ("w1", [128, 1]), ("w2", [128, 1]),
                                ("mk1", [128, 8]), ("mk2", [128, 8]), ("c8", [128, 8])):
                    for i_ in range(2):
                        rt_[i_][nm] = sb(st, "r%d_" % i_ + nm, shp, F32)
                cbt = [sb(st, "cbt%d" % i, [128, 32], F32) for i in range(2)]
                pa = ps(st, "pa", [128, 512])
                pb = ps(st, "pb", [128, 512])
                pcc = ps(st, "pcc", [128, 512])
                pmx = [ps(st, "pmx%d" % i, [128, 512]) for i in range(2)]
                pr = ps(st, "pr", [128, 512])
                c.dma('pool', wa[:], w_br_a[l].rearrange("(k p) n -> p k n", p=128), writes=['wa'])
                c.dma('pool', wbb[:], w_br_b[l].rearrange("(k p) n -> p k n", p=128), writes=['wbb'])
                c.dma('pool', wc[:], w_br_c[l].rearrange("(k p) n -> p k n", p=64), writes=['wc'])
                c.dma('pool', wo[:], w_out[l].rearrange("(k p) n -> p k n", p=128), writes=['wo'])
                c.dma('pool', wr[:], w_r[l].rearrange("(k p) n -> p k n", p=128), writes=['wr'])
                c.dma('sp', brb[:], b_r[l].partition_broadcast(128), writes=['brb'])
                gT3 = gT.rearrange("(g q) n -> q g n", g=3)
                grp5 = groups[1:] if last else groups
                for (t0, n) in grp5:
                    w = 1 if t0 < 256 else 0
                    i2 = nxt('g5', 2)
                    c.dma('sp', ya[i2][:, :, :n], yaT[:, t0:t0 + n].rearrange("(k p) n -> p k n", p=128), writes=['ya%d' % i2])
                    c.dma('sp', yb[i2][:, :, :n], ybT[:, t0:t0 + n].rearrange("(k p) n -> p k n", p=128), writes=['yb%d' % i2])
                    c.dma('sp', yc[i2][:, :, :n], ycT[:, t0:t0 + n].rearrange("(k p) n -> p k n", p=64), writes=['yc%d' % i2])
                    for oc in range(8):
                        gi = nxt('gt', 2)
                        c.dma('sp', gt[gi][:, :, :n], gT3[oc * 128:(oc + 1) * 128, :, t0:t0 + n], writes=['gt%d' % gi])
                        osl = slice(oc * 128, (oc + 1) * 128)
                        for k in range(4):
                            c.op('pe', lambda t: t.matmul(pa[:, :n], wa[:, k, osl], ya[i2][:, k, :n], start=(k == 0), stop=(k == 3)), reads=['wa', 'ya%d' % i2], writes=['pa'], sig=(k == 3))
                        for k in range(4):
                            c.op('pe', lambda t: t.matmul(pb[:, :n], wbb[:, k, osl], yb[i2][:, k, :n], start=(k == 0), stop=(k == 3)), reads=['wbb', 'yb%d' % i2], writes=['pb'], sig=(k == 3))
                        for k in range(8):
                            c.op('pe', lambda t: t.matmul(pcc[:, :n], wc[:, k, osl], yc[i2][:, k, :n], start=(k == 0), stop=(k == 7)), reads=['wc', 'yc%d' % i2], writes=['pcc'], sig=(k == 7))
                        c.op('dve', lambda v: v.tensor_tensor(m1[:, :n], pa[:, :n], gt[gi][:, 0, :n], ALU.mult), reads=['pa', 'gt%d' % gi], writes=['m1'])
                        c.op('dve', lambda v: v.tensor_tensor(m2[:, :n], pb[:, :n], gt[gi][:, 1, :n], ALU.mult), reads=['pb', 'gt%d' % gi], writes=['m2'])
                        c.op('pool', lambda g_: g_.tensor_tensor(m1[:, :n], m1[:, :n], m2[:, :n], ALU.add), reads=['m1', 'm2'], writes=['m1'])
                        c.op('dve', lambda v: v.tensor_tensor(m3[:, :n], pcc[:, :n], gt[gi][:, 2, :n], ALU.mult), reads=['pcc', 'gt%d' % gi], writes=['m3'])
                        c.op('pool', lambda g_: g_.tensor_tensor(mT[:, oc, :n], m1[:, :n], m3[:, :n], ALU.add), reads=['m1', 'm3'], writes=['mT'])
                    def tile_ops(tl, sl):
                        tcol = tl * 128 - t0
                        pmxs = pmx if sl == 0 else [pa, pb]
                        pmk = ['pmx0', 'pmx1'] if sl == 0 else ['pa', 'pb']
                        prs, prk = (pr, 'pr') if sl == 0 else (pcc, 'pcc')
                        tts, xrs = tt[sl], xr[sl]
                        tk, xk = 'tt%d' % sl, 'xr%d' % sl
                        K5 = lambda nm: nm + '_%d' % sl
                        c.dma('sp', xt5[sl][:], xs[tl * 128:(tl + 1) * 128, :], writes=['xt5_%d' % sl])
                        yield
                        for half in range(2):
                            for k in range(8):
                                c.op('pe', lambda t: t.matmul(pmxs[half][:], mT[:, k, tcol:tcol + 128], wo[:, k, half * 512:(half + 1) * 512], start=(k == 0), stop=(k == 7)),
                                     reads=['mT', 'wo'], writes=[pmk[half]], sig=(k == 7))
                            yield
                        for half in range(2):
                            hs = slice(half * 512, (half + 1) * 512)
                            c.op('dve', lambda v: v.tensor_tensor(tts[:, hs], pmxs[half][:], G[:, 0, w, hs], ALU.mult), reads=[pmk[half], 'G'], writes=[tk])
                            yield
                        c.op('dve', lambda v: v.scalar_tensor_tensor(xrs[:], xt5[sl][:], ALPHA, tts[:], ALU.mult, ALU.add), reads=['xt5_%d' % sl, tk], writes=[xk])
                        yield
                        mv, rs = ln_stats(lb_, xrs[:], xk, sl)
                        yield
                        c.op('dve', lambda v: v.tensor_scalar(tts[:], xrs[:], mv[:, 0:1], rs[:], ALU.subtract, ALU.mult), reads=[xk, 'mv%d' % sl, 'rs%d' % sl], writes=[tk])
                        yield
                        c.op('pool', lambda g_: g_.tensor_tensor(tts[:], tts[:], lnp[:, 0, :], ALU.mult), reads=[tk, 'lnp'], writes=[tk])
                        yield
                        c.op('pool', lambda g_: g_.tensor_tensor(x1[sl][:], tts[:], lnp[:, 1, :], ALU.add), reads=[tk, 'lnp'], writes=['x1_%d' % sl])
                        yield
                        c.dma('sp', xs[tl * 128:(tl + 1) * 128, :], x1[sl][:], reads=['x1_%d' % sl])
                        yield
                        ln_to_hT(lb_, x1[sl][:], 'x1_%d' % sl, lambda k: h2t[sl][:, k, :], 'h2t%d' % sl, 32, 24, w, slot=sl)
                        yield
                        c.dma('sp', h2T[:, tl * 128:(tl + 1) * 128].rearrange("(k p) n -> p k n", p=128), h2t[sl][:], reads=['h2t%d' % sl])
                        for k in range(8):
                            c.op('pe', lambda t: t.matmul(prs[:, 0:36], h2t[sl][:, k, :], wr[:, k, :], start=(k == 0), stop=(k == 7)), reads=['h2t%d' % sl, 'wr'], writes=[prk], sig=(k == 7))
                        yield
                        R_ = rt_[sl]

                        def V(fn, rd, wr_):
                            c.op('dve', fn, reads=[x_ if x_ in (prk, 'brb') else K5(x_) for x_ in rd], writes=[K5(x_) for x_ in wr_])
                        V(lambda v: v.tensor_tensor(R_['lg'][:], prs[:, 0:36], brb[:], ALU.add), [prk, 'brb'], ['lg'])
                        yield
                        V(lambda v: v.reduce_max(R_['mg'][:], R_['lg'][:, 0:4], AX.X), ['lg'], ['mg'])
                        yield
                        V(lambda v: v.tensor_scalar(R_['nmg'][:], R_['mg'][:], -1.0, None, ALU.mult), ['mg'], ['nmg'])
                        yield
                        c.op('act', lambda a: a.activation(R_['eg'][:], R_['lg'][:, 0:4], AF.Exp, bias=R_['nmg'][:], scale=1.0, accum_out=R_['sg'][:]), reads=[K5('lg'), K5('nmg')], writes=[K5('eg'), K5('sg')])
                        yield
                        V(lambda v: v.reciprocal(R_['pgv'][:], R_['sg'][:]), ['sg'], ['pgv'])
                        yield
                        V(lambda v: v.tensor_scalar(R_['oh'][:], R_['lg'][:, 0:4], R_['mg'][:], None, ALU.is_equal), ['lg', 'mg'], ['oh'])
                        yield
                        le = R_['lg'][:, 4:36].rearrange("p (g e) -> p g e", e=8)
                        V(lambda v: v.tensor_scalar(R_['sel'][:], le[:, 0, :], R_['oh'][:, 0:1], None, ALU.mult), ['lg', 'oh'], ['sel'])
                        yield
                        for g4 in range(1, 4):
                            V(lambda v: v.scalar_tensor_tensor(R_['sel'][:], le[:, g4, :], R_['oh'][:, g4:g4 + 1], R_['sel'][:], ALU.mult, ALU.add), ['lg', 'oh', 'sel'], ['sel'])
                            yield
                        V(lambda v: v.max(R_['top8'][:], R_['sel'][:]), ['sel'], ['top8'])
                        yield
                        V(lambda v: v.tensor_tensor(R_['dlt'][:], R_['top8'][:, 1:2], R_['top8'][:, 0:1], ALU.subtract), ['top8'], ['dlt'])
                        yield
                        c.op('act', lambda a: a.activation(R_['e2'][:], R_['dlt'][:], AF.Exp), reads=[K5('dlt')], writes=[K5('e2')])
                        yield
                        V(lambda v: v.tensor_scalar(R_['den'][:], R_['e2'][:], 1.0, None, ALU.add), ['e2'], ['den'])
                        yield
                        V(lambda v: v.reciprocal(R_['den'][:], R_['den'][:]), ['den'], ['den'])
                        yield
                        V(lambda v: v.tensor_tensor(R_['w1'][:], R_['pgv'][:], R_['den'][:], ALU.mult), ['pgv', 'den'], ['w1'])
                        yield
                        V(lambda v: v.tensor_tensor(R_['w2'][:], R_['w1'][:], R_['e2'][:], ALU.mult), ['w1', 'e2'], ['w2'])
                        yield
                        V(lambda v: v.tensor_scalar(R_['mk1'][:], R_['sel'][:], R_['top8'][:, 0:1], R_['w1'][:], ALU.is_equal, ALU.mult), ['sel', 'top8', 'w1'], ['mk1'])
                        yield
                        V(lambda v: v.tensor_scalar(R_['mk2'][:], R_['sel'][:], R_['top8'][:, 1:2], R_['w2'][:], ALU.is_equal, ALU.mult), ['sel', 'top8', 'w2'], ['mk2'])
                        yield
                        V(lambda v: v.tensor_tensor(R_['c8'][:], R_['mk1'][:], R_['mk2'][:], ALU.add), ['mk1', 'mk2'], ['c8'])
                        yield
                        for g4 in range(4):
                            V(lambda v: v.tensor_scalar(cbt[sl][:, g4 * 8:(g4 + 1) * 8], R_['c8'][:], R_['oh'][:, g4:g4 + 1], None, ALU.mult), ['c8', 'oh'], ['cbt'])
                            yield
                        c.dma('sp', comb[tl * 128:(tl + 1) * 128, :], cbt[sl][:], reads=[K5('cbt')])
                        yield

                    tls = list(range(t0 // 128, (t0 + n) // 128))
                    for i0 in range(0, len(tls), 2):
                        gens = [tile_ops(tl_, j_) for j_, tl_ in enumerate(tls[i0:i0 + 2])]
                        while gens:
                            for g_ in list(gens):
                                try:
                                    next(g_)
                                except StopIteration:
                                    gens.remove(g_)
            if stop == 'p5':
                break

            blocks6 = [(2, 13), (13, 24), (24, 34)] if last else [(0, 12), (12, 23), (23, 34)]
            for (ta, tb_) in blocks6:
                ntile = tb_ - ta
                ntok = ntile * 128
                c0 = ta * 128
                c.barrier()
                with ExitStack() as st:
                    acc = sb(st, "acc", [128, 12, 1024], F32)
                    with ExitStack() as st2:
                        h2 = sb(st2, "h2", [128, 8, 1536], BF16)
                        cbm = sb(st2, "cbm", [128, 12, 32], F32)
                        wg = [sb(st2, "wg%d" % i, [128, 8, 512], BF16) for i in range(2)]
                        wu = [sb(st2, "wu%d" % i, [128, 8, 512], BF16) for i in range(2)]
                        wd = [sb(st2, "wd%d" % i, [128, 4, 1024], BF16) for i in range(2)]
                        sgl = [sb(st2, "sgl%d" % i, [128, 512], F32) for i in range(2)]
                        actT = [sb(st2, "actT%d" % i, [128, 4, 512], BF16) for i in range(2)]
                        pG = [ps(st2, "pG%d" % i, [128, 512]) for i in range(2)]
                        pU = [ps(st2, "pU%d" % i, [128, 512]) for i in range(2)]
                        pO6 = [ps(st2, "pO6_%d" % i, [128, 512]) for i in range(4)]
                        c.dma('sp', h2[:, :, :ntok], h2T[:, c0:c0 + ntok].rearrange("(k p) n -> p k n", p=128), writes=['h2'])
                        c.dma('sp', cbm[:, :ntile, :], comb[c0:c0 + ntok, :].rearrange("(t p) e -> p t e", p=128), writes=['cbm'])
                        def moe_down(e, s, sb0, n, ai):
                            for ti in range(n // 128):
                                tl = sb0 // 128 + ti
                                for half in range(2):
                                    oi = nxt('pO6', 4)
                                    hs = slice(half * 512, (half + 1) * 512)
                                    for dc in range(4):
                                        c.op('pe', lambda t: t.matmul(pO6[oi][:], actT[ai][:, dc, ti * 128:(ti + 1) * 128], wd[s][:, dc, hs], start=(dc == 0), stop=(dc == 3)),
                                             reads=['actT%d' % ai, 'wd%d' % s], writes=['pO6_%d' % oi], sig=(dc == 3))
                                    akey = 'acc%d_%d' % (tl, half)
                                    if e == 0:
                                        c.op('dve', lambda v: v.tensor_scalar(acc[:, tl, hs], pO6[oi][:], cbm[:, tl, 0:1], None, ALU.mult), reads=['pO6_%d' % oi, 'cbm'], writes=[akey])
                                    else:
                                        c.op('dve', lambda v: v.scalar_tensor_tensor(acc[:, tl, hs], pO6[oi][:], cbm[:, tl, e:e + 1], acc[:, tl, hs], ALU.mult, ALU.add),
                                             reads=['pO6_%d' % oi, 'cbm', akey], writes=[akey])

                        prev = None
                        for e in range(32):
                            s = e % 2
                            c.dma('pool', wg[s][:], w_gate[l, e].rearrange("(k p) n -> p k n", p=128), writes=['wg%d' % s])
                            c.dma('pool', wu[s][:], w_up[l, e].rearrange("(k p) n -> p k n", p=128), writes=['wu%d' % s])
                            for sb0 in range(0, ntok, 512):
                                n = min(512, ntok - sb0)
                                ai = nxt('actT', 2)
                                for dc in range(4):
                                    gi = nxt('pG', 2)
                                    for k in range(8):
                                        c.op('pe', lambda t: t.matmul(pG[gi][:, :n], wg[s][:, k, dc * 128:(dc + 1) * 128], h2[:, k, sb0:sb0 + n], start=(k == 0), stop=(k == 7)),
                                             reads=['wg%d' % s, 'h2'], writes=['pG%d' % gi], sig=(k == 7))
                                    for k in range(8):
                                        c.op('pe', lambda t: t.matmul(pU[gi][:, :n], wu[s][:, k, dc * 128:(dc + 1) * 128], h2[:, k, sb0:sb0 + n], start=(k == 0), stop=(k == 7)),
                                             reads=['wu%d' % s, 'h2'], writes=['pU%d' % gi], sig=(k == 7))
                                    c.op('act', lambda a: a.activation(sgl[gi][:, :n], pG[gi][:, :n], AF.Silu), reads=['pG%d' % gi], writes=['sgl%d' % gi])
                                    c.op('dve', lambda v: v.tensor_tensor(actT[ai][:, dc, :n], sgl[gi][:, :n], pU[gi][:, :n], ALU.mult), reads=['sgl%d' % gi, 'pU%d' % gi], writes=['actT%d' % ai])
                                if prev is not None:
                                    moe_down(*prev)
                                if sb0 == 0:
                                    c.dma('pool', wd[s][:], w_down[l, e].rearrange("(k p) n -> p k n", p=128), writes=['wd%d' % s])
                                prev = (e, s, sb0, n, ai)
                        moe_down(*prev)
                    c.barrier()
                    with ExitStack() as st2:
                        lb6 = ln_bufs(st2, full=False)
                        xt6 = [sb(st2, "xt6_%d" % i, [128, 1024], F32) for i in range(2)]
                        t6 = [sb(st2, "t6_%d" % i, [128, 1024], F32) for i in range(2)]
                        for tl in range(ntile):
                            gt_ = ta + tl
                            w = 1 if gt_ < 2 else 0
                            xi = nxt('xt6', 2)
                            c.dma('sp', xt6[xi][:], xs[gt_ * 128:(gt_ + 1) * 128, :], writes=['xt6_%d' % xi])
                            c.op('pool', lambda g_: g_.tensor_tensor(acc[:, tl, :], acc[:, tl, :], G[:, 1, w, :], ALU.mult), reads=['G'], writes=['acct%d' % tl])
                            c.op('dve', lambda v: v.scalar_tensor_tensor(t6[xi][:], xt6[xi][:], ALPHA, acc[:, tl, :], ALU.mult, ALU.add), reads=['xt6_%d' % xi, 'acct%d' % tl], writes=['t6_%d' % xi])
                            slot = nxt('lnslot', 2)
                            mv, rs = ln_stats(lb6, t6[xi][:], 't6_%d' % xi, slot)
                            c.op('dve', lambda v: v.tensor_scalar(t6[xi][:], t6[xi][:], mv[:, 0:1], rs[:], ALU.subtract, ALU.mult), reads=['t6_%d' % xi, 'mv%d' % slot, 'rs%d' % slot], writes=['t6_%d' % xi])
                            c.op('pool', lambda g_: g_.tensor_tensor(t6[xi][:], t6[xi][:], lnp[:, 2, :], ALU.mult), reads=['t6_%d' % xi, 'lnp'], writes=['t6_%d' % xi])
                            c.op('pool', lambda g_: g_.tensor_tensor(xt6[xi][:], t6[xi][:], lnp[:, 3, :], ALU.add), reads=['t6_%d' % xi, 'lnp'], writes=['xt6_%d' % xi])
                            if last:
                                c.dma('sp', out[(gt_ - 2) * 128:(gt_ - 1) * 128, :], xt6[xi][:], reads=['xt6_%d' % xi])
                            else:
                                c.dma('sp', xs[gt_ * 128:(gt_ + 1) * 128, :], xt6[xi][:], reads=['xt6_%d' % xi])
            if stop == 'p6':
                break
        c.barrier()
    return nc


def _host_consts():
    ident = np.eye(128, dtype=np.float32)
    s = np.arange(128)[:, None]
    t = np.arange(128)[None, :]
    same = (s // 32) == (t // 32)
    masks = np.stack([(same & (s <= t)), (same & (s >= t))]).astype(np.int32)
    invc = np.zeros((4, PADL), np.float32)
    for g, win in enumerate((2, 4, 8, 16)):
        for (n, off) in ((TC, 32), (T, 352)):
            pos = np.arange(n)
            lo = np.clip(pos - win // 2, 0, n)
            hi = np.clip(pos - win // 2 + win, 0, n)
            invc[g, off:off + n] = 1.0 / (hi - lo).astype(np.float32)
    cm = (np.arange(128)[:, None] // 32 == np.arange(4)[None, :]).astype(np.float32)
    return ident, masks, invc, cm


def _bias_table(rpb):
    p = np.arange(128)
    krow_l = p // 64
    kc = p % 64
    qc = np.arange(64)
    c0 = np.clip(qc - 8, 0, 48)
    valid = (kc[:, None] >= c0[None, :]) & (kc[:, None] < c0[None, :] + 16)
    dc = np.clip(kc[:, None] - qc[None, :] + 15, 0, 30)
    tb = np.full((2, 8, 128, 8, 4, 64), NEG, np.float32)
    for delta in range(8):
        for j in range(4):
            dr = (2 * j + krow_l) - delta + 7
            val = rpb[:, :, dr[:, None], dc]
            tb[:, :, :, delta, j, :] = np.where(valid[None, None], val, NEG)
    return np.ascontiguousarray(tb.reshape(2, 8, 128, 8 * 4 * 64))


def prep_inputs(inp):
    f = lambda a: np.ascontiguousarray(np.asarray(a, dtype=np.float32))
    ident, masks, invc, cm = _host_consts()
    shared = {
        "w_ada": f(inp["w_ada"]), "b_ada": f(inp["b_ada"]),
        "b_ada_col": f(np.asarray(inp["b_ada"]).reshape(2, 48, 128).transpose(0, 2, 1)),
        "w_in": f(inp["w_in"]), "w_pool": f(inp["w_pool"]),
        "pscale_col": f(np.asarray(inp["pool_scale"]).reshape(2, 4, 128).transpose(0, 2, 1)),
        "lbl": f(np.stack([np.asarray(inp["lb_logits_fwd"]).reshape(2, 4, 128), np.asarray(inp["lb_logits_bwd"]).reshape(2, 4, 128)]).transpose(3, 0, 1, 2)),
        "gain_col": f(np.asarray(inp["hg_gain"]).transpose(0, 2, 1)),
        "tb": _bias_table(np.asarray(inp["rpb"], dtype=np.float32)),
        "w_br_a": f(inp["w_br_a"]), "w_br_b": f(inp["w_br_b"]), "w_br_c": f(inp["w_br_c"]), "w_out": f(inp["w_out"]),
        "lnp": f(np.stack([inp["ln1_g"], inp["ln1_b"], inp["ln2_g"], inp["ln2_b"]], axis=1)),
        "w_r": f(np.concatenate([inp["w_rg"], inp["w_re"]], axis=2)),
        "b_r": f(np.concatenate([inp["b_rg"], inp["b_re"]], axis=1)),
        "w_gate": f(inp["w_gate"]), "w_up": f(inp["w_up"]), "w_down": f(inp["w_down"]),
        "ident": ident, "masks": masks, "invc": invc, "cm": cm,
    }
    x = np.asarray(inp["x"], dtype=np.float32)
    ctx = np.asarray(inp["ctx"], dtype=np.float32)
    cc = np.asarray(inp["c"], dtype=np.float32)
    c_ctx = np.asarray(inp["c_ctx"], dtype=np.float32)
    maps = []
    for b in range(8):
        d = dict(shared)
        d["x"] = np.ascontiguousarray(x[b])
        d["ctx"] = np.ascontiguousarray(ctx[b])
        d["ccol"] = np.ascontiguousarray(np.stack([cc[b].reshape(8, 128).T, c_ctx.reshape(8, 128).T], axis=2))
        maps.append(d)
    return maps


def kernel(**inputs):
    nc = build()
    maps = prep_inputs(inputs)
    res = run_bass_kernel_spmd(nc, maps, core_ids=list(range(8)))
    return np.stack([np.asarray(r["out"], dtype=np.float32) for r in res.results], axis=0)
```

```python
import numpy as np
from contextlib import ExitStack
import concourse.bass as bass
import concourse.mybir as mybir
from concourse.bass_utils import run_bass_kernel_spmd

F32 = mybir.dt.float32
BF16 = mybir.dt.bfloat16
AF = mybir.ActivationFunctionType
ALU = mybir.AluOpType
AX = mybir.AxisListType

D = 1024
T = 4096
TC = 256
TT = T + TC
NT = TT // 128
DIN = 7680
ALPHA = (2.0 * 2) ** 0.25
NEG = -30000.0
PADL = 4480


class Ctx:
    def __init__(self, nc, es):
        self.nc = nc
        self.eng = {'pe': nc.tensor, 'act': nc.scalar, 'dve': nc.vector, 'pool': nc.gpsimd, 'sp': nc.sync}
        self.sem = {}
        self.cnt = {}
        for n in ['pe', 'act', 'dve', 'pool']:
            self.sem[n] = es.enter_context(nc.semaphore('s_' + n))
            self.cnt[n] = 0
        self.ring = {}
        self.ringpos = {}
        for q in ['sp', 'act', 'pool']:
            self.ring[q] = []
            for i in range(16):
                nm = 'd_%s%d' % (q, i)
                self.sem[nm] = es.enter_context(nc.semaphore(nm))
                self.cnt[nm] = 0
                self.ring[q].append(nm)
            self.ringpos[q] = 0
        self.seen = {e: {} for e in self.eng}
        self.lastw = {}
        self.readers = {}
        self.pending = {e: False for e in self.eng}
        self.nwaits = 0
        self.nins = 0

    def _wait(self, e, s, v):
        if self.seen[e].get(s, 0) >= v:
            return
        self.eng[e].wait_ge(self.sem[s], v)
        self.seen[e][s] = v
        self.nwaits += 1

    def _deps(self, e, reads, writes, is_dma):
        for r in reads:
            lw = self.lastw.get(r)
            if lw is not None:
                if lw[2] == 'pe' and e == 'pe' and not is_dma:
                    continue
                self._wait(e, lw[0], lw[1])
        for w in writes:
            lw = self.lastw.get(w)
            if lw is not None and (is_dma or lw[2] != e or lw[3]):
                self._wait(e, lw[0], lw[1])
            for (s, v, re, rdma) in self.readers.get(w, {}).values():
                if is_dma or rdma or re != e:
                    self._wait(e, s, v)

    def _commit(self, e, reads, writes, s, v, is_dma):
        for r in reads:
            self.readers.setdefault(r, {})[s] = (s, v, e, is_dma)
        for w in writes:
            self.lastw[w] = (s, v, e, is_dma)
            self.readers[w] = {}

    def op(self, e, fn, reads=(), writes=(), sig=True):
        self._deps(e, reads, writes, False)
        ins = fn(self.eng[e])
        if sig:
            self.cnt[e] += 1
            ins.then_inc(self.sem[e], 1)
            v = self.cnt[e]
            self.pending[e] = False
        else:
            v = self.cnt[e] + 1
            self.pending[e] = True
        self._commit(e, reads, writes, e, v, False)
        self.nins += 1
        return ins

    def dma(self, q, out, in_, reads=(), writes=(), **kw):
        self._deps(q, reads, writes, True)
        s = self.ring[q][self.ringpos[q] % len(self.ring[q])]
        self.ringpos[q] += 1
        if self.cnt[s] > 0:
            self._wait(q, s, self.cnt[s])
        ins = self.eng[q].dma_start(out=out, in_=in_, **kw)
        self.cnt[s] += 16
        ins.then_inc(self.sem[s], 16)
        self._commit(q, reads, writes, s, self.cnt[s], True)
        self.nins += 1
        return ins

    def barrier(self):
        for e in self.eng:
            assert not self.pending[e]
        for e in self.eng:
            for s in self.sem:
                if self.cnt[s] > 0:
                    self._wait(e, s, self.cnt[s])
        self.lastw = {}
        self.readers = {}


def build(dbg=False, nlayers=2, stop=None, lite=False, skip01=False):
    nc = bass.Bass("TRN2", target_bir_lowering=False)

    def din(name, shape, dt=F32):
        return nc.dram_tensor(name, list(shape), dt, kind="ExternalInput").ap()

    def scr(name, shape, dt):
        return nc.dram_tensor(name, list(shape), dt, kind=("ExternalOutput" if dbg else "Internal")).ap()

    x_in = din("x", [T, D])
    ctx_in = din("ctx", [TC, D])
    ccol_in = din("ccol", [128, 8, 2])
    w_ada = din("w_ada", [2, D, 6 * D])
    b_ada = din("b_ada", [2, 6 * D])
    b_ada_col = din("b_ada_col", [2, 128, 48])
    w_in = din("w_in", [2, D, DIN])
    w_pool = din("w_pool", [2, 4, 128, 128])
    pscale_col = din("pscale_col", [2, 128, 4])
    lbl_in = din("lbl", [128, 2, 2, 4])
    gain_col = din("gain_col", [2, 128, 4])
    tb_in = din("tb", [2, 8, 128, 8 * 4 * 64])
    w_br_a = din("w_br_a", [2, 512, D])
    w_br_b = din("w_br_b", [2, 512, D])
    w_br_c = din("w_br_c", [2, 512, D])
    w_out = din("w_out", [2, D, D])
    lnp_in = din("lnp", [2, 4, D])
    w_r = din("w_r", [2, D, 36])
    b_r = din("b_r", [2, 36])
    w_gate = din("w_gate", [2, 32, D, 512] if not lite else [2, 1, 1, 1])
    w_up = din("w_up", [2, 32, D, 512] if not lite else [2, 1, 1, 1])
    w_down = din("w_down", [2, 32, 512, D] if not lite else [2, 1, 1, 1])
    ident_in = din("ident", [128, 128])
    masks_in = din("masks", [2, 128, 128], mybir.dt.int32)
    invc_in = din("invc", [4, PADL])
    cm_in = din("cm", [128, 4])
    out = nc.dram_tensor("out", [T, D], F32, kind="ExternalOutput").ap()

    xs = scr("xs", [TT, D], F32)
    zaT = scr("zaT", [512, TT], F32)
    qsT = scr("qsT", [512, TT], F32)
    zfT = scr("zfT", [1024, TT], F32)
    sgT = scr("sgT", [512, TT], BF16)
    qT = scr("qT", [512, TT], BF16)
    kT = scr("kT", [512, TT], BF16)
    gT = scr("gT", [3072, TT], BF16)
    vi = scr("vi", [TT, 512], BF16)
    vv = scr("vv", [TT, 512], BF16)
    yaT = scr("yaT", [512, TT], BF16)
    ybT = scr("ybT", [512, TT], BF16)
    ycT = scr("ycT", [512, TT], BF16)
    h2T = scr("h2T", [D, TT], BF16)
    comb = scr("comb", [TT, 32], F32)

    with ExitStack() as es:
        c = Ctx(nc, es)

        uid = [0]

        def sb(st, name, shape, dt):
            uid[0] += 1
            return st.enter_context(nc.sbuf_tensor("sb%d_%s" % (uid[0], name), list(shape), dt))

        def ps(st, name, shape, dt=F32):
            uid[0] += 1
            return st.enter_context(nc.psum_tensor("ps%d_%s" % (uid[0], name), list(shape), dt))

        ident = sb(es, "ident", [128, 128], BF16)
        ones32 = sb(es, "ones32", [128, 128], F32)
        ones16 = sb(es, "ones16", [128, 128], BF16)
        masks = sb(es, "masks", [128, 2, 128], mybir.dt.int32)
        cm = sb(es, "cm", [128, 4], F32)
        eps_ln = sb(es, "eps_ln", [128, 1], F32)
        eps_rms = sb(es, "eps_rms", [128, 1], F32)
        lbc = sb(es, "lbc", [128, 2, 2, 4], F32)
        omlc = sb(es, "omlc", [128, 2, 2, 4], F32)
        lbl = sb(es, "lbl", [128, 2, 2, 4], F32)
        modc = sb(es, "modc", [128, 48, 2], F32)
        G = sb(es, "G", [128, 2, 2, 1024], F32)
        lnp = sb(es, "lnp", [128, 4, 1024], F32)
        gcol = sb(es, "gcol", [128, 4], F32)
        pscol = sb(es, "pscol", [128, 4], F32)

        c.dma('pool', ident[:], ident_in[:, :], writes=['ident'])
        c.dma('sp', masks[:], masks_in.rearrange("m p q -> p m q"), writes=['masks'])
        c.dma('sp', cm[:], cm_in[:, :], writes=['cm'])
        c.dma('sp', lbl[:], lbl_in[:, :, :, :], writes=['lbl'])
        c.op('dve', lambda v: v.memset(ones32[:], 1.0), writes=['ones32'])
        c.op('dve', lambda v: v.memset(ones16[:], 1.0), writes=['ones16'])
        c.op('dve', lambda v: v.memset(eps_ln[:], 1e-5), writes=['eps_ln'])
        c.op('dve', lambda v: v.memset(eps_rms[:], 1e-6), writes=['eps_rms'])
        c.op('dve', lambda v: v.memset(lbc[:], 0.0), writes=['lbc'])
        c.op('dve', lambda v: v.tensor_tensor(lbl[:, :, 1, :], lbl[:, :, 1, :], lbl[:, :, 0, :], ALU.subtract), reads=['lbl'], writes=['lbl'])
        c.op('act', lambda a: a.activation(lbc[:, :, 1, :], lbl[:, :, 1, :], AF.Sigmoid), reads=['lbl', 'lbc'], writes=['lbc'])
        c.op('dve', lambda v: v.tensor_scalar(omlc[:], lbc[:], -1.0, 1.0, ALU.mult, ALU.add), reads=['lbc'], writes=['omlc'])
        c.dma('sp', xs[0:TC, :], ctx_in[:, :])
        c.dma('sp', xs[TC:TT, :], x_in[:, :])

        rot = {}

        def nxt(name, n):
            i = rot.get(name, 0)
            rot[name] = i + 1
            return i % n

        def ln_stats(st, xap, xkey, slot):
            stt, mv, rs = st['st'][slot], st['mv'][slot], st['rs'][slot]
            for i in range(2):
                c.op('dve', lambda v: v.bn_stats(stt[:, i, :], xap[:, i * 512:(i + 1) * 512]), reads=[xkey], writes=['st%d_%d' % (slot, i)])
            c.op('dve', lambda v: v.bn_aggr(mv[:], stt[:].rearrange("p a b -> p (a b)")), reads=['st%d_0' % slot, 'st%d_1' % slot], writes=['mv%d' % slot])
            c.op('act', lambda a: a.activation(rs[:], mv[:, 1:2], AF.Sqrt, bias=eps_ln[:], scale=1.0), reads=['mv%d' % slot, 'eps_ln'], writes=['rs%d' % slot])
            c.op('dve', lambda v: v.reciprocal(rs[:], rs[:]), reads=['rs%d' % slot], writes=['rs%d' % slot])
            return mv, rs

        def ln_to_hT(st, xap, xkey, dst, dkey, sc_chunk0, sh_chunk0, w, slot=None):
            if slot is None:
                slot = nxt('lnslot', 2)
            mv, rs = ln_stats(st, xap, xkey, slot)
            hn, pT = st['hn'][slot], st['pT'][slot]
            c.op('dve', lambda v: v.tensor_scalar(hn[:], xap, mv[:, 0:1], rs[:], ALU.subtract, ALU.mult), reads=[xkey, 'mv%d' % slot, 'rs%d' % slot], writes=['hn%d' % slot])
            for k in range(8):
                c.op('pe', lambda t: t.transpose(pT[:, k, :], hn[:, k * 128:(k + 1) * 128], ident[:]), reads=['hn%d' % slot, 'ident'], writes=['pT%d' % slot])
            for k in range(8):
                c.op('act', lambda a: a.activation(dst(k), pT[:, k, :], AF.Identity, bias=modc[:, sh_chunk0 + k, w:w + 1], scale=modc[:, sc_chunk0 + k, w:w + 1]),
                     reads=['pT%d' % slot, 'modc'], writes=[dkey])

        def ln_bufs(st, full=True):
            d = {'st': [], 'mv': [], 'rs': [], 'hn': [], 'pT': []}
            for i in range(2):
                d['st'].append(sb(st, "lnst%d" % i, [128, 2, 6], F32))
                d['mv'].append(sb(st, "lnmv%d" % i, [128, 2], F32))
                d['rs'].append(sb(st, "lnrs%d" % i, [128, 1], F32))
                if full:
                    d['hn'].append(sb(st, "lnhn%d" % i, [128, 1024], BF16))
                    d['pT'].append(ps(st, "lnpT%d" % i, [128, 8, 128], BF16))
            return d

        for l in range(nlayers):
            last = (l == 1)
            c.barrier()
            if not skip01:
                with ExitStack() as st:
                    wada = sb(st, "wada", [128, 8, 6144], BF16)
                    ccol = sb(st, "ccol", [128, 8, 2], F32)
                    sc = sb(st, "sc", [128, 8, 2], BF16)
                    scb = sb(st, "scb", [128, 2, 8, 128], BF16)
                    bcol = sb(st, "bcol", [128, 48], F32)
                    bbc = sb(st, "bbc", [128, 2, 1024], F32)
                    pc = ps(st, "pc", [128, 48, 2])
                    pg = [ps(st, "pg%d" % i, [128, 512]) for i in range(2)]
                    for k in range(8):
                        c.dma('pool', wada[:, k, :], w_ada[l, k * 128:(k + 1) * 128, :], writes=['wada%d' % k])
                    c.dma('sp', ccol[:], ccol_in[:, :, :], writes=['ccol'])
                    c.dma('sp', gcol[:], gain_col[l], writes=['gcol'])
                    c.dma('sp', pscol[:], pscale_col[l], writes=['pscol'])
                    c.dma('sp', bcol[:], b_ada_col[l], writes=['bcol'])
                    c.dma('sp', bbc[:, 0, :], b_ada[l, 2048:3072].partition_broadcast(128), writes=['bbc0'])
                    c.dma('sp', bbc[:, 1, :], b_ada[l, 5120:6144].partition_broadcast(128), writes=['bbc1'])
                    for i in range(4):
                        c.dma('sp', lnp[:, i, :], lnp_in[l, i, :].partition_broadcast(128), writes=['lnp'])
                    c.op('act', lambda a: a.activation(sc[:], ccol[:], AF.Silu), reads=['ccol'], writes=['sc'])
                    for w in range(2):
                        for k in range(8):
                            c.op('dve', lambda v: v.tensor_copy(scb[:, w, k, :], sc[:, k, w:w + 1].to_broadcast([128, 128])), reads=['sc'], writes=['scb'])
                    for j in range(48):
                        for k in range(8):
                            c.op('pe', lambda t: t.matmul(pc[:, j, :], wada[:, k, j * 128:(j + 1) * 128], sc[:, k, :], start=(k == 0), stop=(k == 7)),
                                 reads=['wada%d' % k, 'sc'], writes=['pc'], sig=(k == 7))
                    for w in range(2):
                        c.op('dve', lambda v: v.tensor_tensor(modc[:, :, w], pc[:, :, w], bcol[:], ALU.add), reads=['pc', 'bcol'], writes=['modc'])
                    for ch0 in (8, 32):
                        c.op('dve', lambda v: v.tensor_scalar(modc[:, ch0:ch0 + 8, :], modc[:, ch0:ch0 + 8, :], 1.0, None, ALU.add), reads=['modc'], writes=['modc'])
                    for w in range(2):
                        for gi, c0 in enumerate((2048, 5120)):
                            for half in range(2):
                                pi = nxt('pg', 2)
                                for k in range(8):
                                    c.op('pe', lambda t: t.matmul(pg[pi][:], scb[:, w, k, :], wada[:, k, c0 + half * 512:c0 + (half + 1) * 512], start=(k == 0), stop=(k == 7)),
                                         reads=['scb', 'wada%d' % k], writes=['pg%d' % pi], sig=(k == 7))
                                c.op('dve', lambda v: v.tensor_tensor(G[:, gi, w, half * 512:(half + 1) * 512], pg[pi][:], bbc[:, gi, half * 512:(half + 1) * 512], ALU.add),
                                     reads=['pg%d' % pi, 'bbc%d' % gi], writes=['G'])
            if stop == 'p0':
                break

            c.barrier()
            if not skip01:
                with ExitStack() as st:
                    hT = sb(st, "hT", [128, 8, TT], BF16)
                    lb_ = ln_bufs(st)
                    xt = [sb(st, "xt%d" % i, [128, 1024], F32) for i in range(3)]
                    win = [sb(st, "win%d" % i, [128, 8, 512], BF16) for i in range(2)]
                    stg32 = [sb(st, "stg32_%d" % i, [128, 512], F32) for i in range(3)]
                    stg16 = [sb(st, "stg16_%d" % i, [128, 512], BF16) for i in range(3)]
                    pm = [ps(st, "pm%d" % i, [128, 512]) for i in range(4)]
                    def emit_ln(tl):
                        xi = nxt('xt', 3)
                        w = 1 if tl < 2 else 0
                        c.dma('sp', xt[xi][:], xs[tl * 128:(tl + 1) * 128, :], writes=['xt%d' % xi])
                        ln_to_hT(lb_, xt[xi][:], 'xt%d' % xi, lambda k: hT[:, k, tl * 128:(tl + 1) * 128], 'hT%d' % tl, 8, 0, w)
                    groups = [(0, 256)] + [(256 + i * 512, 512) for i in range(8)]
                    gtiles = [list(range(t0_ // 128, (t0_ + n_) // 128)) for (t0_, n_) in groups]
                    for gi_ in range(len(groups)):
                        for tl_ in gtiles[gi_]:
                            emit_ln(tl_)
                    fdst = {0: (zaT, 0, F32, None), 1: (qsT, 0, F32, AF.Silu), 2: (zfT, 0, F32, None), 3: (zfT, 512, F32, None),
                            5: (sgT, 0, BF16, AF.Silu), 6: (qT, 0, BF16, 'q'), 7: (kT, 0, BF16, None)}
                    for i in range(6):
                        fdst[9 + i] = (gT, i * 512, BF16, AF.Sigmoid)
                    for cc in range(15):
                        ws = nxt('win', 2)
                        c.dma('pool', win[ws][:], w_in[l, :, cc * 512:(cc + 1) * 512].rearrange("(k p) n -> p k n", p=128), writes=['win%d' % ws])
                        for gi_, (t0, n) in enumerate(groups):
                            tiles = list(range(t0 // 128, (t0 + n) // 128))
                            hkeys = ['hT%d' % t for t in tiles]
                            if cc in (4, 8):
                                dstd = vi if cc == 4 else vv
                                for tl in tiles:
                                    pi = nxt('pm', 4)
                                    for k in range(8):
                                        c.op('pe', lambda t: t.matmul(pm[pi][:], hT[:, k, tl * 128:(tl + 1) * 128], win[ws][:, k, :], start=(k == 0), stop=(k == 7)),
                                             reads=['hT%d' % tl, 'win%d' % ws], writes=['pm%d' % pi], sig=(k == 7))
                                    si = nxt('stg16', 3)
                                    c.op('dve', lambda v: v.tensor_copy(stg16[si][:], pm[pi][:]), reads=['pm%d' % pi], writes=['stg16_%d' % si])
                                    c.dma('sp', dstd[tl * 128:(tl + 1) * 128, :], stg16[si][:], reads=['stg16_%d' % si])
                            else:
                                dd, r0, dt, fn = fdst[cc]
                                for sub in range(4):
                                    pi = nxt('pm', 4)
                                    for k in range(8):
                                        c.op('pe', lambda t: t.matmul(pm[pi][:, :n], win[ws][:, k, sub * 128:(sub + 1) * 128], hT[:, k, t0:t0 + n], start=(k == 0), stop=(k == 7)),
                                             reads=hkeys + ['win%d' % ws], writes=['pm%d' % pi], sig=(k == 7))
                                    if dt == F32:
                                        si = nxt('stg32', 3)
                                        stg, skey = stg32[si], 'stg32_%d' % si
                                    else:
                                        si = nxt('stg16', 3)
                                        stg, skey = stg16[si], 'stg16_%d' % si
                                    if fn is None:
                                        c.op('dve', lambda v: v.tensor_copy(stg[:, :n], pm[pi][:, :n]), reads=['pm%d' % pi], writes=[skey])
                                    elif fn == 'q':
                                        c.op('act', lambda a: a.activation(stg[:, :n], pm[pi][:, :n], AF.Identity, scale=0.125), reads=['pm%d' % pi], writes=[skey])
                                    else:
                                        c.op('act', lambda a: a.activation(stg[:, :n], pm[pi][:, :n], fn), reads=['pm%d' % pi], writes=[skey])
                                    rr = r0 + sub * 128
                                    c.dma('sp', dd[rr:rr + 128, t0:t0 + n], stg[:, :n], reads=[skey])
            if stop == 'p1':
                break

            slabs = [(0, 256)] + [(256 + i * 512, 512) for i in range(8)]
            SM = 512
            order = [list(range(NT)), [1, 0] + list(range(NT - 1, 1, -1))]
            P2 = {'p2a0': 0, 'p2a1': 1, 'p2a2': 2, 'p2a': 3, 'p2b': 4}.get(stop, 5)
            for h in range(4):
                c.barrier()
                with ExitStack() as st:
                    seg = sb(st, "seg", [128, SM], F32)
                    tmp = [[sb(st, "tmp%d_%d" % (d_, i), [128, SM], F32) for i in range(6)] for d_ in range(2)]
                    tq = [sb(st, "tq%d" % d_, [128, SM], F32) for d_ in range(2)]
                    kdS = [sb(st, "kdS%d" % d_, [128, SM], BF16) for d_ in range(2)]
                    ksS = [sb(st, "ksS%d" % d_, [128, SM], BF16) for d_ in range(2)]
                    qd = [sb(st, "qd%d" % d, [128, TT], BF16) for d in range(2)]
                    AT = [sb(st, "AT%d" % d, [128, NT, 128], BF16) for d in range(2)]
                    ksTm = [sb(st, "ksT%d" % d, [128, NT, 128], BF16) for d in range(2)]
                    vihm = sb(st, "vihm", [128, NT, 4, 128], BF16)
                    gdec = [sb(st, "gdec%d" % d, [128, NT * 4], F32) for d in range(2)]
                    emdec = [sb(st, "emdec%d" % d, [128, NT * 4 + 1], F32) for d in range(2)]
                    vih = sb(st, "vih", [128, NT, 128], BF16)
                    od = [sb(st, "od%d" % d, [128, TT], F32) for d in range(2)]
                    S32 = [[sb(st, "S32_%d_%d" % (d, i), [128, 128], F32) for i in range(2)] for d in range(2)]
                    S16 = [[sb(st, "S16_%d_%d" % (d, i), [128, 128], BF16) for i in range(8)] for d in range(2)]
                    pK = ps(st, "pK", [128, 8, 128], BF16)
                    pO = [ps(st, "pO%d" % d, [128, 512]) for d in range(2)]
                    pKV = [ps(st, "pKV%d" % i, [128, 512]) for i in range(4)]
                    pA = ps(st, "pA", [128, 512])

                    c.op('dve', lambda v: v.memset(seg[:], 1.0), writes=['seg'])
                    for d in range(2):
                        c.op('pool', lambda g_: g_.memset(AT[d][:], 0.0), writes=['AT%d' % d])
                        c.op('dve', lambda v: v.memset(emdec[d][:], 1.0), writes=['emdec%d' % d])
                    c.op('dve', lambda v: v.memset(seg[:].rearrange("p (c k) -> p c k", k=32)[:, :, 0:1], 0.0), writes=['seg'])
                    c.dma('sp', vih[:], vi[:, h * 128:(h + 1) * 128].rearrange("(t p) d -> p t d", p=128), writes=['vih'])
                    for tl in range(NT):
                        for c4 in range(4):
                            if (tl * 4 + c4) % 2 == 0:
                                c.op('act', lambda a: a.activation(vihm[:, tl, c4, :], vih[:, tl, :], AF.Copy, scale=cm[:, c4:c4 + 1]), reads=['vih', 'cm'], writes=['vihm'])
                            else:
                                c.op('dve', lambda v: v.tensor_scalar(vihm[:, tl, c4, :], vih[:, tl, :], cm[:, c4:c4 + 1], None, ALU.mult), reads=['vih', 'cm'], writes=['vihm'])
                    def slab_ops(d, s0, n):
                        lbcol = lbc[:, d, l, h:h + 1]
                        omcol = omlc[:, d, l, h:h + 1]
                        K_ = ['t%d_%d' % (d, i) for i in range(6)]
                        A_, B_, C_, D_, E_, F_ = [t_[:, :n] for t_ in tmp[d]]
                        nch = n // 32
                        c.dma('sp', A_, zfT[d * 512 + h * 128:d * 512 + (h + 1) * 128, s0:s0 + n], writes=[K_[0]])
                        yield
                        c.dma('sp', tq[d][:, :n], qsT[h * 128:(h + 1) * 128, s0:s0 + n], writes=['tq%d' % d])
                        yield
                        c.op('act', lambda a: a.activation(B_, A_, AF.Sigmoid), reads=[K_[0]], writes=[K_[1]])
                        yield
                        c.op('act', lambda a: a.activation(C_, A_, AF.Sigmoid, scale=-1.0), reads=[K_[0]], writes=[K_[2]])
                        yield
                        c.op('act', lambda a: a.activation(B_, B_, AF.Ln, bias=lbcol, scale=omcol), reads=[K_[1], 'lbc', 'omlc'], writes=[K_[1]])
                        yield
                        c.op('dve', lambda v: v.tensor_tensor_scan(D_, seg[:, :n], B_, 0.0, ALU.mult, ALU.add), reads=['seg', K_[1]], writes=[K_[3]])
                        yield
                        D3 = D_.rearrange("p (c k) -> p c k", k=32)
                        if d == 0:
                            bb, bkey = D_, K_[3]
                            b3 = D3
                            blast = D3[:, :, 31:32]
                        else:
                            E3 = E_.rearrange("p (c k) -> p c k", k=32)
                            c.op('dve', lambda v: v.tensor_tensor(E_, B_, D_, ALU.subtract), reads=[K_[1], K_[3]], writes=[K_[4]])
                            yield
                            c.op('dve', lambda v: v.tensor_tensor(E3, E3, D3[:, :, 31:32].to_broadcast([128, nch, 32]), ALU.add), reads=[K_[4], K_[3]], writes=[K_[4]])
                            yield
                            bb, bkey = E_, K_[4]
                            b3 = E3
                            blast = E3[:, :, 0:1]
                        c.op('act', lambda a: a.activation(gdec[d][:, s0 // 32:s0 // 32 + nch], blast.rearrange("p c k -> p (c k)"), AF.Exp), reads=[bkey], writes=['gdec%d' % d])
                        yield
                        A3 = A_.rearrange("p (c k) -> p c k", k=32)
                        c.op('dve', lambda v: v.tensor_tensor(A3, blast.to_broadcast([128, nch, 32]), b3, ALU.subtract), reads=[bkey, K_[0]], writes=[K_[0]])
                        yield
                        c.op('act', lambda a: a.activation(A_, A_, AF.Exp), reads=[K_[0]], writes=[K_[0]])
                        yield
                        B3 = B_.rearrange("p (c k) -> p c k", k=32)
                        c.op('act', lambda a: a.activation(emdec[d][:, s0 // 32:s0 // 32 + nch], b3[:, :, 16:17].rearrange("p c k -> p (c k)"), AF.Exp), reads=[bkey], writes=['emdec%d' % d])
                        yield
                        c.op('dve', lambda v: v.tensor_tensor(B3, b3, b3[:, :, 16:17].to_broadcast([128, nch, 32]), ALU.subtract), reads=[bkey, K_[1]], writes=[K_[1]])
                        yield
                        c.op('act', lambda a: a.activation(F_, B_, AF.Exp, scale=-1.0), reads=[K_[1]], writes=[K_[5]])
                        yield
                        c.op('act', lambda a: a.activation(B_, B_, AF.Exp), reads=[K_[1]], writes=[K_[1]])
                        yield
                        c.op('dve', lambda v: v.tensor_tensor(qd[d][:, s0:s0 + n], tq[d][:, :n], B_, ALU.mult), reads=['tq%d' % d, K_[1]], writes=['qd%d' % d])
                        yield
                        c.op('dve', lambda v: v.scalar_tensor_tensor(kdS[d][:, :n], C_, omcol, F_, ALU.mult, ALU.mult), reads=[K_[2], K_[5], 'omlc'], writes=['kdS%d' % d])
                        yield
                        c.op('dve', lambda v: v.scalar_tensor_tensor(ksS[d][:, :n], C_, omcol, A_, ALU.mult, ALU.mult), reads=[K_[2], K_[0], 'omlc'], writes=['ksS%d' % d])
                        yield
                        for ti in range(n // 128 if P2 != 1 else 0):
                            tl = s0 // 128 + ti
                            c.op('pe', lambda t: t.matmul(pA[:, 0:128], kdS[d][:, ti * 128:(ti + 1) * 128], qd[d][:, tl * 128:(tl + 1) * 128], start=True, stop=True),
                                 reads=['kdS%d' % d, 'qd%d' % d], writes=['pA'])
                            c.op('dve', lambda v: v.copy_predicated(AT[d][:, tl, :], masks[:, d, :], pA[:, 0:128]), reads=['pA', 'masks'], writes=['AT%d' % d])
                            yield
                            c.op('pe', lambda t: t.transpose(pK[:, 0, :], ksS[d][:, ti * 128:(ti + 1) * 128], ident[:]), reads=['ksS%d' % d, 'ident'], writes=['pK'])
                            c.op('act', lambda a: a.copy(ksTm[d][:, tl, :], pK[:, 0, :]), reads=['pK'], writes=['ksTm%d' % d])
                            yield
                    for (s0, n) in slabs:
                        gens = [slab_ops(d_, s0, n) for d_ in range([0, 1, 1, 2, 2, 2][P2])]
                        while gens:
                            for g_ in list(gens):
                                try:
                                    next(g_)
                                except StopIteration:
                                    gens.remove(g_)
                    RING = 8
                    if P2 >= 4:
                        for d in range(2):
                            c.op('dve', lambda v: v.memset(S32[d][0][:], 0.0), writes=['S32_%d_0' % d])
                            c.op('dve', lambda v: v.memset(S16[d][0][:], 0.0), writes=['S16_%d_0' % d])
                        cseq = [[tl_ * 4 + ci_ for tl_ in order[d_] for ci_ in ([0, 1, 2, 3] if d_ == 0 else [3, 2, 1, 0])] + [NT * 4] for d_ in range(2)]

                        def opart(d, s_):
                            tl = order[d][s_]
                            po = pO[d]
                            pok = 'pO%d' % d
                            c.op('pe', lambda t: t.matmul(po[:, 0:128], vih[:, tl, :], AT[d][:, tl, :], start=True, stop=False), reads=['vih', 'AT%d' % d], writes=[pok], sig=False)
                            for n_i in range(4):
                                k = s_ * 4 + n_i
                                cpos = cseq[d][k]
                                ci = cpos % 4
                                c.op('pe', lambda t: t.matmul(po[:, ci * 32:(ci + 1) * 32], S16[d][k % RING][:], qd[d][:, cpos * 32:(cpos + 1) * 32], start=False, stop=(n_i == 3)),
                                     reads=['S16_%d_%d' % (d, k % RING), 'qd%d' % d], writes=[pok], sig=(n_i == 3))
                            c.op('act', lambda a: a.copy(od[d][:, tl * 128:(tl + 1) * 128], po[:, 0:128]), reads=[pok], writes=['od%d' % d])

                        for s_ in range(NT):
                            for d in range(2):
                                tl = order[d][s_]
                                bank = pKV[d * 2 + s_ % 2]
                                bkey = 'pKV%d' % (d * 2 + s_ % 2)
                                for n_i in range(4):
                                    ci = cseq[d][s_ * 4 + n_i] % 4
                                    c.op('pe', lambda t: t.matmul(bank[:, n_i * 128:(n_i + 1) * 128], ksTm[d][:, tl, :], vihm[:, tl, ci, :], start=True, stop=True),
                                         reads=['ksTm%d' % d, 'vihm'], writes=[bkey], sig=(n_i == 3))
                            if s_ >= 1:
                                for d in range(2):
                                    opart(d, s_ - 1)
                            for n_i in range(4):
                                for d in range(2):
                                    bank = pKV[d * 2 + s_ % 2]
                                    bkey = 'pKV%d' % (d * 2 + s_ % 2)
                                    k = s_ * 4 + n_i
                                    cur = k % 2
                                    nx = 1 - cur
                                    cpos = cseq[d][k]
                                    cnext = cseq[d][k + 1]
                                    c.op('dve', lambda v: v.scalar_tensor_tensor(S32[d][nx][:], S32[d][cur][:], gdec[d][:, cpos:cpos + 1], bank[:, n_i * 128:(n_i + 1) * 128], ALU.mult, ALU.add),
                                         reads=['S32_%d_%d' % (d, cur), 'gdec%d' % d, bkey], writes=['S32_%d_%d' % (d, nx)])
                                    c.op('pool', lambda g: g.tensor_scalar(S16[d][(k + 1) % RING][:], S32[d][nx][:], emdec[d][:, cnext:cnext + 1], 1.0, ALU.mult, ALU.mult),
                                         reads=['S32_%d_%d' % (d, nx), 'emdec%d' % d], writes=['S16_%d_%d' % (d, (k + 1) % RING)])
                        for d in range(2):
                            opart(d, NT - 1)
                    blocks = [(i * 512, min(512, TT - i * 512)) for i in range(9)] if P2 >= 5 else []
                    c.barrier()
                    sq = tmp[0][0:2]
                    rt = tmp[0][2:4]
                    sgt = [kdS[0], ksS[0]]
                    ybt = [sb(st, "ybt%d" % i, [128, 512], BF16) for i in range(2)]
                    for (b0, n) in blocks:
                        i2 = nxt('ro', 2)
                        osl = od[0][:, b0:b0 + n]
                        c.dma('sp', sgt[i2][:, :n], sgT[h * 128:(h + 1) * 128, b0:b0 + n], writes=['sgt%d' % i2])
                        c.op('dve', lambda v: v.tensor_tensor(osl, osl, od[1][:, b0:b0 + n], ALU.add), reads=['od0', 'od1'], writes=['od0'])
                        c.op('act', lambda a: a.activation(sq[i2][:, :n], osl, AF.Square), reads=['od0'], writes=['sq%d' % i2])
                        c.op('pe', lambda t: t.matmul(pA[:, :n], ones32[:], sq[i2][:, :n], start=True, stop=True), reads=['ones32', 'sq%d' % i2], writes=['pA'])
                        c.op('act', lambda a: a.activation(rt[i2][:, :n], pA[:, :n], AF.Sqrt, bias=eps_rms[:], scale=1.0 / 128), reads=['pA', 'eps_rms'], writes=['rt%d' % i2])
                        c.op('dve', lambda v: v.reciprocal(rt[i2][:, :n], rt[i2][:, :n]), reads=['rt%d' % i2], writes=['rt%d' % i2])
                        c.op('dve', lambda v: v.tensor_tensor(rt[i2][:, :n], rt[i2][:, :n], osl, ALU.mult), reads=['rt%d' % i2, 'od0'], writes=['rt%d' % i2])
                        c.op('dve', lambda v: v.scalar_tensor_tensor(ybt[i2][:, :n], rt[i2][:, :n], gcol[:, h:h + 1], sgt[i2][:, :n], ALU.mult, ALU.mult),
                             reads=['rt%d' % i2, 'gcol', 'sgt%d' % i2], writes=['ybt%d' % i2])
                        c.dma('sp', ybT[h * 128:(h + 1) * 128, b0:b0 + n], ybt[i2][:, :n], reads=['ybt%d' % i2])
                if P2 < 5:
                    break
            if stop == 'p2' or P2 < 5:
                break
            for h in range(8):
                c.barrier()
                with ExitStack() as st:
                    qh = sb(st, "qh", [64, TT], BF16)
                    kh = sb(st, "kh", [64, TT], BF16)
                    vA = sb(st, "vA", [128, 32, 64], BF16)
                    vB = sb(st, "vB", [128, 31, 64], BF16)
                    vC = sb(st, "vC", [128, 2, 64], BF16)
                    tbh = sb(st, "tbh", [128, 8, 4, 64], BF16)
                    ych = sb(st, "ych", [64, TT], BF16)
                    E = [sb(st, "E%d" % i, [128, 6, 64], BF16) for i in range(2)]
                    rec = [sb(st, "rec%d" % i, [64, 64], F32) for i in range(2)]
                    Esum = [sb(st, "Esum%d" % i, [128, 64], F32) for i in range(2)]
                    E2 = sb(st, "E2", [128, 2, 256], BF16)
                    rec2 = sb(st, "rec2", [64, 256], F32)
                    pS = [ps(st, "pS%d" % i, [128, 512]) for i in range(2)]
                    pN = [ps(st, "pN%d" % i, [128, 512]) for i in range(2)]
                    pS2 = ps(st, "pS2", [128, 512])
                    pN2 = ps(st, "pN2", [128, 512])
                    c.dma('sp', qh[:], qT[h * 64:(h + 1) * 64, :], writes=['qh'])
                    c.dma('sp', kh[:], kT[h * 64:(h + 1) * 64, :], writes=['kh'])
                    c.dma('sp', vA[:], vv[256:4352, h * 64:(h + 1) * 64].rearrange("(t p) d -> p t d", p=128), writes=['vA'])
                    c.dma('sp', vB[:], vv[320:4288, h * 64:(h + 1) * 64].rearrange("(t p) d -> p t d", p=128), writes=['vB'])
                    c.dma('sp', vC[:], vv[0:256, h * 64:(h + 1) * 64].rearrange("(t p) d -> p t d", p=128), writes=['vC'])
                    c.dma('pool', tbh[:], tb_in[l, h].rearrange("p (a b q) -> p a b q", a=8, b=4), writes=['tbh'])
                    c.op('act', lambda a: a.activation(tbh[:], tbh[:], AF.Exp), reads=['tbh'], writes=['tbh'])

                    def att_geom(r):
                        r0 = min(max(r - 4, 0), 56)
                        return r0, r - r0, 256 + r * 64, 256 + r0 * 64

                    def att_qk(r):
                        i2 = r % 2
                        r0, delta, qc0, kb = att_geom(r)
                        pSv = pS[i2][:, 0:384].rearrange("p (j q) -> p j q", q=64)
                        for j in range(4):
                            c.op('pe', lambda t: t.matmul(pSv[:, j, :], kh[:, kb + j * 128:kb + (j + 1) * 128], qh[:, qc0:qc0 + 64], start=True, stop=True),
                                 reads=['kh', 'qh'], writes=['pS%d' % i2], sig=False)
                        for j in range(2):
                            c.op('pe', lambda t: t.matmul(pSv[:, 4 + j, :], kh[:, j * 128:(j + 1) * 128], qh[:, qc0:qc0 + 64], start=True, stop=True),
                                 reads=['kh', 'qh'], writes=['pS%d' % i2], sig=(j == 1))
                        c.op('act', lambda a: a.activation(E[i2][:], pSv, AF.Exp), reads=['pS%d' % i2], writes=['E%d' % i2])
                        c.op('pool', lambda g_: g_.tensor_tensor(E[i2][:, 0:4, :], E[i2][:, 0:4, :], tbh[:, delta, :, :], ALU.mult), reads=['E%d' % i2, 'tbh'], writes=['E%d' % i2])
                        c.op('dve', lambda v: v.tensor_reduce(Esum[i2][:], E[i2][:].rearrange("p j q -> p q j"), AX.X, ALU.add), reads=['E%d' % i2], writes=['Esum%d' % i2])

                    def att_pv(r):
                        i2 = r % 2
                        r0, delta, qc0, kb = att_geom(r)

                        def Vt(j):
                            if j >= 4:
                                return vC[:, j - 4, :]
                            if r0 % 2 == 0:
                                return vA[:, r0 // 2 + j, :]
                            return vB[:, (r0 - 1) // 2 + j, :]
                        pNv = pN[i2][0:64, 0:128].rearrange("p (a q) -> p a q", q=64)
                        for j in range(6):
                            c.op('pe', lambda t: t.matmul(pNv[:, 0, :], Vt(j), E[i2][:, j, :], start=(j == 0), stop=(j == 5)),
                                 reads=['vA', 'vB', 'vC', 'E%d' % i2], writes=['pN%d' % i2], sig=False)
                        c.op('pe', lambda t: t.matmul(pNv[:, 1, :], ones32[:, 0:64], Esum[i2][:], start=True, stop=True), reads=['ones32', 'Esum%d' % i2], writes=['pN%d' % i2])
                        c.op('dve', lambda v: v.reciprocal(rec[i2][:], pNv[:, 1, :]), reads=['pN%d' % i2], writes=['rec%d' % i2])
                        c.op('dve', lambda v: v.tensor_tensor(ych[:, qc0:qc0 + 64], pNv[:, 0, :], rec[i2][:], ALU.mult), reads=['pN%d' % i2, 'rec%d' % i2], writes=['ych'])

                    for r in range(64):
                        att_qk(r)
                        if r >= 1:
                            att_pv(r - 1)
                    att_pv(63)
                    if not last:
                        pS2v = pS2[:, 0:512].rearrange("p (j q) -> p j q", q=256)
                        for j in range(2):
                            c.op('pe', lambda t: t.matmul(pS2v[:, j, :], kh[:, j * 128:(j + 1) * 128], qh[:, 0:256], start=True, stop=True),
                                 reads=['kh', 'qh'], writes=['pS2'], sig=(j == 1))
                        c.op('act', lambda a: a.activation(E2[:], pS2v, AF.Exp), reads=['pS2'], writes=['E2'])
                        pN2v = pN2[0:64, 0:512].rearrange("p (a q) -> p a q", q=256)
                        for j in range(2):
                            c.op('pe', lambda t: t.matmul(pN2v[:, 0, :], vC[:, j, :], E2[:, j, :], start=(j == 0), stop=(j == 1)), reads=['vC', 'E2'], writes=['pN2'], sig=False)
                        for j in range(2):
                            c.op('pe', lambda t: t.matmul(pN2v[:, 1, :], ones16[:, 0:64], E2[:, j, :], start=(j == 0), stop=(j == 1)), reads=['ones16', 'E2'], writes=['pN2'], sig=(j == 1))
                        c.op('dve', lambda v: v.reciprocal(rec2[:], pN2v[:, 1, :]), reads=['pN2'], writes=['rec2'])
                        c.op('dve', lambda v: v.tensor_tensor(ych[:, 0:256], pN2v[:, 0, :], rec2[:], ALU.mult), reads=['pN2', 'rec2'], writes=['ych'])
                    cc0 = 256 if last else 0
                    c.dma('sp', ycT[h * 64:(h + 1) * 64, cc0:TT], ych[:, cc0:TT], reads=['ych'])
            if stop == 'p3':
                break

            groups = [(0, 256)] + [(256 + i * 512, 512) for i in range(8)]
            c.barrier()
            with ExitStack() as st:
                U = sb(st, "U", [128, PADL], F32)
                B1 = sb(st, "B1", [128, PADL], F32)
                B2 = sb(st, "B2", [128, PADL], F32)
                invc = sb(st, "invc", [128, PADL], F32)
                Y = sb(st, "Y", [128, PADL], BF16)
                wp = sb(st, "wp", [128, 4, 128], BF16)
                stgp = [sb(st, "stgp%d" % i, [128, 512], BF16) for i in range(2)]
                pP = [ps(st, "pP%d" % i, [128, 512]) for i in range(2)]
                L = PADL
                c.op('dve', lambda v: v.memset(U[:], 0.0), writes=['U'])
                c.dma('pool', wp[:], w_pool[l].rearrange("g c d -> c g d"), writes=['wp'])
                for g in range(4):
                    c.dma('sp', U[:, 32:288], zaT[g * 128:(g + 1) * 128, 0:256], writes=['U'])
                    c.dma('sp', U[:, 352:4448], zaT[g * 128:(g + 1) * 128, 256:TT], writes=['U'])
                    c.dma('sp', invc[:], invc_in[g].partition_broadcast(128), writes=['invc'])
                    c.op('dve', lambda v: v.tensor_tensor(B1[:, 1:L], U[:, 0:L - 1], U[:, 1:L], ALU.add), reads=['U'], writes=['B1'])
                    S, skey = B1, 'B1'
                    if g >= 1:
                        c.op('dve', lambda v: v.tensor_tensor(B2[:, 2:L - 1], B1[:, 1:L - 2], B1[:, 3:L], ALU.add), reads=['B1'], writes=['B2'])
                        S, skey = B2, 'B2'
                    if g >= 2:
                        c.op('dve', lambda v: v.tensor_tensor(B1[:, 4:L - 3], B2[:, 2:L - 5], B2[:, 6:L - 1], ALU.add), reads=['B2'], writes=['B1'])
                        S, skey = B1, 'B1'
                    if g >= 3:
                        c.op('dve', lambda v: v.tensor_tensor(B2[:, 8:L - 7], B1[:, 4:L - 11], B1[:, 12:L - 3], ALU.add), reads=['B1'], writes=['B2'])
                        S, skey = B2, 'B2'
                    c.op('dve', lambda v: v.tensor_tensor(S[:, 32:4448], S[:, 32:4448], invc[:, 32:4448], ALU.mult), reads=[skey, 'invc'], writes=[skey])
                    c.op('dve', lambda v: v.tensor_tensor(Y[:, 32:4448], S[:, 32:4448], U[:, 32:4448], ALU.subtract), reads=[skey, 'U'], writes=['Y'])
                    for (t0, n) in groups:
                        pc0 = t0 + 32 if t0 < 256 else t0 + 96
                        pi = nxt('pP', 2)
                        c.op('pe', lambda t: t.matmul(pP[pi][:, :n], wp[:, g, :], Y[:, pc0:pc0 + n], start=True, stop=True), reads=['wp', 'Y'], writes=['pP%d' % pi])
                        si = nxt('stgp', 2)
                        c.op('act', lambda a: a.activation(stgp[si][:, :n], pP[pi][:, :n], AF.Identity, scale=pscol[:, g:g + 1]), reads=['pP%d' % pi, 'pscol'], writes=['stgp%d' % si])
                        c.dma('sp', yaT[g * 128:(g + 1) * 128, t0:t0 + n], stgp[si][:, :n], reads=['stgp%d' % si])
            if stop == 'p4':
                break

            c.barrier()
            with ExitStack() as st:
                wa = sb(st, "wa", [128, 4, 1024], BF16)
                wbb = sb(st, "wbb", [128, 4, 1024], BF16)
                wc = sb(st, "wc", [64, 8, 1024], BF16)
                wo = sb(st, "wo", [128, 8, 1024], BF16)
                wr = sb(st, "wr", [128, 8, 36], BF16)
                brb = sb(st, "brb", [128, 36], F32)
                ya = [sb(st, "ya%d" % i, [128, 4, 512], BF16) for i in range(2)]
                yb = [sb(st, "yb%d" % i, [128, 4, 512], BF16) for i in range(2)]
                yc = [sb(st, "yc%d" % i, [64, 8, 512], BF16) for i in range(2)]
                gt = [sb(st, "gt%d" % i, [128, 3, 512], BF16) for i in range(2)]
                m1 = sb(st, "m1", [128, 512], F32)
                m2 = sb(st, "m2", [128, 512], F32)
                m3 = sb(st, "m3", [128, 512], F32)
                mT = sb(st, "mT", [128, 8, 512], BF16)
                xt5 = [sb(st, "xt5_%d" % i, [128, 1024], F32) for i in range(2)]
                tt = [sb(st, "tt%d" % i, [128, 1024], F32) for i in range(2)]
                xr = [sb(st, "xr%d" % i, [128, 1024], F32) for i in range(2)]
                x1 = [sb(st, "x1_%d" % i, [128, 1024], F32) for i in range(2)]
                h2t = [sb(st, "h2t%d" % i, [128, 8, 128], BF16) for i in range(2)]
                lb_ = ln_bufs(st)
                rt_ = [{}, {}]
                for nm, shp in (("lg", [128, 36]), ("mg", [128, 1]), ("nmg", [128, 1]), ("eg", [128, 4]), ("sg", [128, 1]), ("pgv", [128, 1]), ("oh", [128, 4]),
                                ("sel", [128, 8]), ("top8", [128, 8]), ("dlt", [128, 1]), ("e2", [128, 1]), ("den", [128, 1]), ("w1", [128, 1]), ("w2", [128, 1]),
                                ("mk1", [128, 8]), ("mk2", [128, 8]), ("c8", [128, 8])):
                    for i_ in range(2):
                        rt_[i_][nm] = sb(st, "r%d_" % i_ + nm, shp, F32)
                cbt = [sb(st, "cbt%d" % i, [128, 32], F32) for i in range(2)]
                pa = ps(st, "pa", [128, 512])
                pb = ps(st, "pb", [128, 512])
                pcc = ps(st, "pcc", [128, 512])
                pmx = [ps(st, "pmx%d" % i, [128, 512]) for i in range(2)]
                pr = ps(st, "pr", [128, 512])
                c.dma('pool', wa[:], w_br_a[l].rearrange("(k p) n -> p k n", p=128), writes=['wa'])
                c.dma('pool', wbb[:], w_br_b[l].rearrange("(k p) n -> p k n", p=128), writes=['wbb'])
                c.dma('pool', wc[:], w_br_c[l].rearrange("(k p) n -> p k n", p=64), writes=['wc'])
                c.dma('pool', wo[:], w_out[l].rearrange("(k p) n -> p k n", p=128), writes=['wo'])
                c.dma('pool', wr[:], w_r[l].rearrange("(k p) n -> p k n", p=128), writes=['wr'])
                c.dma('sp', brb[:], b_r[l].partition_broadcast(128), writes=['brb'])
                gT3 = gT.rearrange("(g q) n -> q g n", g=3)
                grp5 = groups[1:] if last else groups
                for (t0, n) in grp5:
                    w = 1 if t0 < 256 else 0
                    i2 = nxt('g5', 2)
                    c.dma('sp', ya[i2][:, :, :n], yaT[:, t0:t0 + n].rearrange("(k p) n -> p k n", p=128), writes=['ya%d' % i2])
                    c.dma('sp', yb[i2][:, :, :n], ybT[:, t0:t0 + n].rearrange("(k p) n -> p k n", p=128), writes=['yb%d' % i2])
                    c.dma('sp', yc[i2][:, :, :n], ycT[:, t0:t0 + n].rearrange("(k p) n -> p k n", p=64), writes=['yc%d' % i2])
                    for oc in range(8):
                        gi = nxt('gt', 2)
                        c.dma('sp', gt[gi][:, :, :n], gT3[oc * 128:(oc + 1) * 128, :, t0:t0 + n], writes=['gt%d' % gi])
                        osl = slice(oc * 128, (oc + 1) * 128)
                        for k in range(4):
                            c.op('pe', lambda t: t.matmul(pa[:, :n], wa[:, k, osl], ya[i2][:, k, :n], start=(k == 0), stop=(k == 3)), reads=['wa', 'ya%d' % i2], writes=['pa'], sig=(k == 3))
                        for k in range(4):
                            c.op('pe', lambda t: t.matmul(pb[:, :n], wbb[:, k, osl], yb[i2][:, k, :n], start=(k == 0), stop=(k == 3)), reads=['wbb', 'yb%d' % i2], writes=['pb'], sig=(k == 3))
                        for k in range(8):
                            c.op('pe', lambda t: t.matmul(pcc[:, :n], wc[:, k, osl], yc[i2][:, k, :n], start=(k == 0), stop=(k == 7)), reads=['wc', 'yc%d' % i2], writes=['pcc'], sig=(k == 7))
                        c.op('dve', lambda v: v.tensor_tensor(m1[:, :n], pa[:, :n], gt[gi][:, 0, :n], ALU.mult), reads=['pa', 'gt%d' % gi], writes=['m1'])
                        c.op('dve', lambda v: v.tensor_tensor(m2[:, :n], pb[:, :n], gt[gi][:, 1, :n], ALU.mult), reads=['pb', 'gt%d' % gi], writes=['m2'])
                        c.op('pool', lambda g_: g_.tensor_tensor(m1[:, :n], m1[:, :n], m2[:, :n], ALU.add), reads=['m1', 'm2'], writes=['m1'])
                        c.op('dve', lambda v: v.tensor_tensor(m3[:, :n], pcc[:, :n], gt[gi][:, 2, :n], ALU.mult), reads=['pcc', 'gt%d' % gi], writes=['m3'])
                        c.op('pool', lambda g_: g_.tensor_tensor(mT[:, oc, :n], m1[:, :n], m3[:, :n], ALU.add), reads=['m1', 'm3'], writes=['mT'])
                    def tile_ops(tl, sl):
                        tcol = tl * 128 - t0
                        pmxs = pmx if sl == 0 else [pa, pb]
                        pmk = ['pmx0', 'pmx1'] if sl == 0 else ['pa', 'pb']
                        prs, prk = (pr, 'pr') if sl == 0 else (pcc, 'pcc')
                        tts, xrs = tt[sl], xr[sl]
                        tk, xk = 'tt%d' % sl, 'xr%d' % sl
                        K5 = lambda nm: nm + '_%d' % sl
                        c.dma('sp', xt5[sl][:], xs[tl * 128:(tl + 1) * 128, :], writes=['xt5_%d' % sl])
                        yield
                        for half in range(2):
                            for k in range(8):
                                c.op('pe', lambda t: t.matmul(pmxs[half][:], mT[:, k, tcol:tcol + 128], wo[:, k, half * 512:(half + 1) * 512], start=(k == 0), stop=(k == 7)),
                                     reads=['mT', 'wo'], writes=[pmk[half]], sig=(k == 7))
                            yield
                        for half in range(2):
                            hs = slice(half * 512, (half + 1) * 512)
                            c.op('dve', lambda v: v.tensor_tensor(tts[:, hs], pmxs[half][:], G[:, 0, w, hs], ALU.mult), reads=[pmk[half], 'G'], writes=[tk])
                            yield
                        c.op('dve', lambda v: v.scalar_tensor_tensor(xrs[:], xt5[sl][:], ALPHA, tts[:], ALU.mult, ALU.add), reads=['xt5_%d' % sl, tk], writes=[xk])
                        yield
                        mv, rs = ln_stats(lb_, xrs[:], xk, sl)
                        yield
                        c.op('dve', lambda v: v.tensor_scalar(tts[:], xrs[:], mv[:, 0:1], rs[:], ALU.subtract, ALU.mult), reads=[xk, 'mv%d' % sl, 'rs%d' % sl], writes=[tk])
                        yield
                        c.op('pool', lambda g_: g_.tensor_tensor(tts[:], tts[:], lnp[:, 0, :], ALU.mult), reads=[tk, 'lnp'], writes=[tk])
                        yield
                        c.op('pool', lambda g_: g_.tensor_tensor(x1[sl][:], tts[:], lnp[:, 1, :], ALU.add), reads=[tk, 'lnp'], writes=['x1_%d' % sl])
                        yield
                        c.dma('sp', xs[tl * 128:(tl + 1) * 128, :], x1[sl][:], reads=['x1_%d' % sl])
                        yield
                        ln_to_hT(lb_, x1[sl][:], 'x1_%d' % sl, lambda k: h2t[sl][:, k, :], 'h2t%d' % sl, 32, 24, w, slot=sl)
                        yield
                        c.dma('sp', h2T[:, tl * 128:(tl + 1) * 128].rearrange("(k p) n -> p k n", p=128), h2t[sl][:], reads=['h2t%d' % sl])
                        for k in range(8):
                            c.op('pe', lambda t: t.matmul(prs[:, 0:36], h2t[sl][:, k, :], wr[:, k, :], start=(k == 0), stop=(k == 7)), reads=['h2t%d' % sl, 'wr'], writes=[prk], sig=(k == 7))
                        yield
                        R_ = rt_[sl]

                        def V(fn, rd, wr_):
                            c.op('dve', fn, reads=[x_ if x_ in (prk, 'brb') else K5(x_) for x_ in rd], writes=[K5(x_) for x_ in wr_])
                        V(lambda v: v.tensor_tensor(R_['lg'][:], prs[:, 0:36], brb[:], ALU.add), [prk, 'brb'], ['lg'])
                        yield
                        V(lambda v: v.reduce_max(R_['mg'][:], R_['lg'][:, 0:4], AX.X), ['lg'], ['mg'])
                        yield
                        V(lambda v: v.tensor_scalar(R_['nmg'][:], R_['mg'][:], -1.0, None, ALU.mult), ['mg'], ['nmg'])
                        yield
                        c.op('act', lambda a: a.activation(R_['eg'][:], R_['lg'][:, 0:4], AF.Exp, bias=R_['nmg'][:], scale=1.0, accum_out=R_['sg'][:]), reads=[K5('lg'), K5('nmg')], writes=[K5('eg'), K5('sg')])
                        yield
                        V(lambda v: v.reciprocal(R_['pgv'][:], R_['sg'][:]), ['sg'], ['pgv'])
                        yield
                        V(lambda v: v.tensor_scalar(R_['oh'][:], R_['lg'][:, 0:4], R_['mg'][:], None, ALU.is_equal), ['lg', 'mg'], ['oh'])
                        yield
                        le = R_['lg'][:, 4:36].rearrange("p (g e) -> p g e", e=8)
                        V(lambda v: v.tensor_scalar(R_['sel'][:], le[:, 0, :], R_['oh'][:, 0:1], None, ALU.mult), ['lg', 'oh'], ['sel'])
                        yield
                        for g4 in range(1, 4):
                            V(lambda v: v.scalar_tensor_tensor(R_['sel'][:], le[:, g4, :], R_['oh'][:, g4:g4 + 1], R_['sel'][:], ALU.mult, ALU.add), ['lg', 'oh', 'sel'], ['sel'])
                            yield
                        V(lambda v: v.max(R_['top8'][:], R_['sel'][:]), ['sel'], ['top8'])
                        yield
                        V(lambda v: v.tensor_tensor(R_['dlt'][:], R_['top8'][:, 1:2], R_['top8'][:, 0:1], ALU.subtract), ['top8'], ['dlt'])
                        yield
                        c.op('act', lambda a: a.activation(R_['e2'][:], R_['dlt'][:], AF.Exp), reads=[K5('dlt')], writes=[K5('e2')])
                        yield
                        V(lambda v: v.tensor_scalar(R_['den'][:], R_['e2'][:], 1.0, None, ALU.add), ['e2'], ['den'])
                        yield
                        V(lambda v: v.reciprocal(R_['den'][:], R_['den'][:]), ['den'], ['den'])
                        yield
                        V(lambda v: v.tensor_tensor(R_['w1'][:], R_['pgv'][:], R_['den'][:], ALU.mult), ['pgv', 'den'], ['w1'])
                        yield
                        V(lambda v: v.tensor_tensor(R_['w2'][:], R_['w1'][:], R_['e2'][:], ALU.mult), ['w1', 'e2'], ['w2'])
                        yield
                        V(lambda v: v.tensor_scalar(R_['mk1'][:], R_['sel'][:], R_['top8'][:, 0:1], R_['w1'][:], ALU.is_equal, ALU.mult), ['sel', 'top8', 'w1'], ['mk1'])
                        yield
                        V(lambda v: v.tensor_scalar(R_['mk2'][:], R_['sel'][:], R_['top8'][:, 1:2], R_['w2'][:], ALU.is_equal, ALU.mult), ['sel', 'top8', 'w2'], ['mk2'])
                        yield
                        V(lambda v: v.tensor_tensor(R_['c8'][:], R_['mk1'][:], R_['mk2'][:], ALU.add), ['mk1', 'mk2'], ['c8'])
                        yield
                        for g4 in range(4):
                            V(lambda v: v.tensor_scalar(cbt[sl][:, g4 * 8:(g4 + 1) * 8], R_['c8'][:], R_['oh'][:, g4:g4 + 1], None, ALU.mult), ['c8', 'oh'], ['cbt'])
                            yield
                        c.dma('sp', comb[tl * 128:(tl + 1) * 128, :], cbt[sl][:], reads=[K5('cbt')])
                        yield

                    tls = list(range(t0 // 128, (t0 + n) // 128))
                    for i0 in range(0, len(tls), 2):
                        gens = [tile_ops(tl_, j_) for j_, tl_ in enumerate(tls[i0:i0 + 2])]
                        while gens:
                            for g_ in list(gens):
                                try:
                                    next(g_)
                                except StopIteration:
                                    gens.remove(g_)
            if stop == 'p5':
                break

            blocks6 = [(2, 18), (18, 34)] if last else [(0, 17), (17, 34)]
            for (ta, tb_) in blocks6:
                ntile = tb_ - ta
                ntok = ntile * 128
                c0 = ta * 128
                c.barrier()
                with ExitStack() as st:
                    acc = sb(st, "acc", [128, 17, 1024], F32)
                    with ExitStack() as st2:
                        h2 = sb(st2, "h2", [128, 8, 2176], BF16)
                        cbm = sb(st2, "cbm", [128, 17, 32], F32)
                        wg = [sb(st2, "wg%d" % i, [128, 8, 512], BF16) for i in range(2)]
                        wu = [sb(st2, "wu%d" % i, [128, 8, 512], BF16) for i in range(2)]
                        wd = [sb(st2, "wd%d" % i, [128, 4, 1024], BF16) for i in range(2)]
                        sgl = [sb(st2, "sgl%d" % i, [128, 512], F32) for i in range(2)]
                        actT = [sb(st2, "actT%d" % i, [128, 4, 512], BF16) for i in range(2)]
                        pG = [ps(st2, "pG%d" % i, [128, 512]) for i in range(2)]
                        pU = [ps(st2, "pU%d" % i, [128, 512]) for i in range(2)]
                        pO6 = [ps(st2, "pO6_%d" % i, [128, 512]) for i in range(4)]
                        c.dma('sp', h2[:, :, :ntok], h2T[:, c0:c0 + ntok].rearrange("(k p) n -> p k n", p=128), writes=['h2'])
                        c.dma('sp', cbm[:, :ntile, :], comb[c0:c0 + ntok, :].rearrange("(t p) e -> p t e", p=128), writes=['cbm'])
                        def moe_down(e, s, sb0, n, ai):
                            for ti in range(n // 128):
                                tl = sb0 // 128 + ti
                                for half in range(2):
                                    oi = nxt('pO6', 4)
                                    hs = slice(half * 512, (half + 1) * 512)
                                    for dc in range(4):
                                        c.op('pe', lambda t: t.matmul(pO6[oi][:], actT[ai][:, dc, ti * 128:(ti + 1) * 128], wd[s][:, dc, hs], start=(dc == 0), stop=(dc == 3)),
                                             reads=['actT%d' % ai, 'wd%d' % s], writes=['pO6_%d' % oi], sig=(dc == 3))
                                    akey = 'acc%d_%d' % (tl, half)
                                    if e == 0:
                                        c.op('dve', lambda v: v.tensor_scalar(acc[:, tl, hs], pO6[oi][:], cbm[:, tl, 0:1], None, ALU.mult), reads=['pO6_%d' % oi, 'cbm'], writes=[akey])
                                    else:
                                        c.op('dve', lambda v: v.scalar_tensor_tensor(acc[:, tl, hs], pO6[oi][:], cbm[:, tl, e:e + 1], acc[:, tl, hs], ALU.mult, ALU.add),
                                             reads=['pO6_%d' % oi, 'cbm', akey], writes=[akey])

                        prev = None
                        for e in range(32):
                            s = e % 2
                            c.dma('pool', wg[s][:], w_gate[l, e].rearrange("(k p) n -> p k n", p=128), writes=['wg%d' % s])
                            c.dma('pool', wu[s][:], w_up[l, e].rearrange("(k p) n -> p k n", p=128), writes=['wu%d' % s])
                            for sb0 in range(0, ntok, 512):
                                n = min(512, ntok - sb0)
                                ai = nxt('actT', 2)
                                for dc in range(4):
                                    gi = nxt('pG', 2)
                                    for k in range(8):
                                        c.op('pe', lambda t: t.matmul(pG[gi][:, :n], wg[s][:, k, dc * 128:(dc + 1) * 128], h2[:, k, sb0:sb0 + n], start=(k == 0), stop=(k == 7)),
                                             reads=['wg%d' % s, 'h2'], writes=['pG%d' % gi], sig=(k == 7))
                                    for k in range(8):
                                        c.op('pe', lambda t: t.matmul(pU[gi][:, :n], wu[s][:, k, dc * 128:(dc + 1) * 128], h2[:, k, sb0:sb0 + n], start=(k == 0), stop=(k == 7)),
                                             reads=['wu%d' % s, 'h2'], writes=['pU%d' % gi], sig=(k == 7))
                                    c.op('act', lambda a: a.activation(sgl[gi][:, :n], pG[gi][:, :n], AF.Silu), reads=['pG%d' % gi], writes=['sgl%d' % gi])
                                    c.op('dve', lambda v: v.tensor_tensor(actT[ai][:, dc, :n], sgl[gi][:, :n], pU[gi][:, :n], ALU.mult), reads=['sgl%d' % gi, 'pU%d' % gi], writes=['actT%d' % ai])
                                if prev is not None:
                                    moe_down(*prev)
                                if sb0 == 0:
                                    c.dma('pool', wd[s][:], w_down[l, e].rearrange("(k p) n -> p k n", p=128), writes=['wd%d' % s])
                                prev = (e, s, sb0, n, ai)
                        moe_down(*prev)
                    c.barrier()
                    with ExitStack() as st2:
                        lb6 = ln_bufs(st2, full=False)
                        xt6 = [sb(st2, "xt6_%d" % i, [128, 1024], F32) for i in range(2)]
                        t6 = [sb(st2, "t6_%d" % i, [128, 1024], F32) for i in range(2)]
                        for tl in range(ntile):
                            gt_ = ta + tl
                            w = 1 if gt_ < 2 else 0
                            xi = nxt('xt6', 2)
                            c.dma('sp', xt6[xi][:], xs[gt_ * 128:(gt_ + 1) * 128, :], writes=['xt6_%d' % xi])
                            c.op('pool', lambda g_: g_.tensor_tensor(acc[:, tl, :], acc[:, tl, :], G[:, 1, w, :], ALU.mult), reads=['G'], writes=['acct%d' % tl])
                            c.op('dve', lambda v: v.scalar_tensor_tensor(t6[xi][:], xt6[xi][:], ALPHA, acc[:, tl, :], ALU.mult, ALU.add), reads=['xt6_%d' % xi, 'acct%d' % tl], writes=['t6_%d' % xi])
                            slot = nxt('lnslot', 2)
                            mv, rs = ln_stats(lb6, t6[xi][:], 't6_%d' % xi, slot)
                            c.op('dve', lambda v: v.tensor_scalar(t6[xi][:], t6[xi][:], mv[:, 0:1], rs[:], ALU.subtract, ALU.mult), reads=['t6_%d' % xi, 'mv%d' % slot, 'rs%d' % slot], writes=['t6_%d' % xi])
                            c.op('pool', lambda g_: g_.tensor_tensor(t6[xi][:], t6[xi][:], lnp[:, 2, :], ALU.mult), reads=['t6_%d' % xi, 'lnp'], writes=['t6_%d' % xi])
                            c.op('pool', lambda g_: g_.tensor_tensor(xt6[xi][:], t6[xi][:], lnp[:, 3, :], ALU.add), reads=['t6_%d' % xi, 'lnp'], writes=['xt6_%d' % xi])
                            if last:
                                c.dma('sp', out[(gt_ - 2) * 128:(gt_ - 1) * 128, :], xt6[xi][:], reads=['xt6_%d' % xi])
                            else:
                                c.dma('sp', xs[gt_ * 128:(gt_ + 1) * 128, :], xt6[xi][:], reads=['xt6_%d' % xi])
            if stop == 'p6':
                break
        c.barrier()
    return nc


def _host_consts():
    ident = np.eye(128, dtype=np.float32)
    s = np.arange(128)[:, None]
    t = np.arange(128)[None, :]
    same = (s // 32) == (t // 32)
    masks = np.stack([(same & (s <= t)), (same & (s >= t))]).astype(np.int32)
    invc = np.zeros((4, PADL), np.float32)
    for g, win in enumerate((2, 4, 8, 16)):
        for (n, off) in ((TC, 32), (T, 352)):
            pos = np.arange(n)
            lo = np.clip(pos - win // 2, 0, n)
            hi = np.clip(pos - win // 2 + win, 0, n)
            invc[g, off:off + n] = 1.0 / (hi - lo).astype(np.float32)
    cm = (np.arange(128)[:, None] // 32 == np.arange(4)[None, :]).astype(np.float32)
    return ident, masks, invc, cm


def _bias_table(rpb):
    p = np.arange(128)
    krow_l = p // 64
    kc = p % 64
    qc = np.arange(64)
    c0 = np.clip(qc - 8, 0, 48)
    valid = (kc[:, None] >= c0[None, :]) & (kc[:, None] < c0[None, :] + 16)
    dc = np.clip(kc[:, None] - qc[None, :] + 15, 0, 30)
    tb = np.full((2, 8, 128, 8, 4, 64), NEG, np.float32)
    for delta in range(8):
        for j in range(4):
            dr = (2 * j + krow_l) - delta + 7
            val = rpb[:, :, dr[:, None], dc]
            tb[:, :, :, delta, j, :] = np.where(valid[None, None], val, NEG)
    return np.ascontiguousarray(tb.reshape(2, 8, 128, 8 * 4 * 64))


def prep_inputs(inp):
    f = lambda a: np.ascontiguousarray(np.asarray(a, dtype=np.float32))
    ident, masks, invc, cm = _host_consts()
    shared = {
        "w_ada": f(inp["w_ada"]), "b_ada": f(inp["b_ada"]),
        "b_ada_col": f(np.asarray(inp["b_ada"]).reshape(2, 48, 128).transpose(0, 2, 1)),
        "w_in": f(inp["w_in"]), "w_pool": f(inp["w_pool"]),
        "pscale_col": f(np.asarray(inp["pool_scale"]).reshape(2, 4, 128).transpose(0, 2, 1)),
        "lbl": f(np.stack([np.asarray(inp["lb_logits_fwd"]).reshape(2, 4, 128), np.asarray(inp["lb_logits_bwd"]).reshape(2, 4, 128)]).transpose(3, 0, 1, 2)),
        "gain_col": f(np.asarray(inp["hg_gain"]).transpose(0, 2, 1)),
        "tb": _bias_table(np.asarray(inp["rpb"], dtype=np.float32)),
        "w_br_a": f(inp["w_br_a"]), "w_br_b": f(inp["w_br_b"]), "w_br_c": f(inp["w_br_c"]), "w_out": f(inp["w_out"]),
        "lnp": f(np.stack([inp["ln1_g"], inp["ln1_b"], inp["ln2_g"], inp["ln2_b"]], axis=1)),
        "w_r": f(np.concatenate([inp["w_rg"], inp["w_re"]], axis=2)),
        "b_r": f(np.concatenate([inp["b_rg"], inp["b_re"]], axis=1)),
        "w_gate": f(inp["w_gate"]), "w_up": f(inp["w_up"]), "w_down": f(inp["w_down"]),
        "ident": ident, "masks": masks, "invc": invc, "cm": cm,
    }
    x = np.asarray(inp["x"], dtype=np.float32)
    ctx = np.asarray(inp["ctx"], dtype=np.float32)
    cc = np.asarray(inp["c"], dtype=np.float32)
    c_ctx = np.asarray(inp["c_ctx"], dtype=np.float32)
    maps = []
    for b in range(8):
        d = dict(shared)
        d["x"] = np.ascontiguousarray(x[b])
        d["ctx"] = np.ascontiguousarray(ctx[b])
        d["ccol"] = np.ascontiguousarray(np.stack([cc[b].reshape(8, 128).T, c_ctx.reshape(8, 128).T], axis=2))
        maps.append(d)
    return maps


def kernel(**inputs):
    nc = build()
    maps = prep_inputs(inputs)
    res = run_bass_kernel_spmd(nc, maps, core_ids=list(range(8)))
    return np.stack([np.asarray(r["out"], dtype=np.float32) for r in res.results], axis=0)
```

```python
import numpy as np
from contextlib import ExitStack
import concourse.bass as bass
import concourse.mybir as mybir
from concourse.bass_utils import run_bass_kernel_spmd

F32 = mybir.dt.float32
BF16 = mybir.dt.bfloat16
AF = mybir.ActivationFunctionType
ALU = mybir.AluOpType
AX = mybir.AxisListType

D = 1024
T = 4096
TC = 256
TT = T + TC
NT = TT // 128
DIN = 7680
ALPHA = (2.0 * 2) ** 0.25
NEG = -30000.0
PADL = 4480


class Ctx:
    def __init__(self, nc, es):
        self.nc = nc
        self.eng = {'pe': nc.tensor, 'act': nc.scalar, 'dve': nc.vector, 'pool': nc.gpsimd, 'sp': nc.sync}
        self.sem = {}
        self.cnt = {}
        for n in ['pe', 'act', 'dve', 'pool']:
            self.sem[n] = es.enter_context(nc.semaphore('s_' + n))
            self.cnt[n] = 0
        self.ring = {}
        self.ringpos = {}
        for q in ['sp', 'act', 'pool']:
            self.ring[q] = []
            for i in range(16):
                nm = 'd_%s%d' % (q, i)
                self.sem[nm] = es.enter_context(nc.semaphore(nm))
                self.cnt[nm] = 0
                self.ring[q].append(nm)
            self.ringpos[q] = 0
        self.seen = {e: {} for e in self.eng}
        self.lastw = {}
        self.readers = {}
        self.pending = {e: False for e in self.eng}
        self.nwaits = 0
        self.nins = 0

    def _wait(self, e, s, v):
        if self.seen[e].get(s, 0) >= v:
            return
        self.eng[e].wait_ge(self.sem[s], v)
        self.seen[e][s] = v
        self.nwaits += 1

    def _deps(self, e, reads, writes, is_dma):
        for r in reads:
            lw = self.lastw.get(r)
            if lw is not None:
                if lw[2] == 'pe' and e == 'pe' and not is_dma:
                    continue
                self._wait(e, lw[0], lw[1])
        for w in writes:
            lw = self.lastw.get(w)
            if lw is not None and (is_dma or lw[2] != e or lw[3]):
                self._wait(e, lw[0], lw[1])
            for (s, v, re, rdma) in self.readers.get(w, {}).values():
                if is_dma or rdma or re != e:
                    self._wait(e, s, v)

    def _commit(self, e, reads, writes, s, v, is_dma):
        for r in reads:
            self.readers.setdefault(r, {})[s] = (s, v, e, is_dma)
        for w in writes:
            self.lastw[w] = (s, v, e, is_dma)
            self.readers[w] = {}

    def op(self, e, fn, reads=(), writes=(), sig=True):
        self._deps(e, reads, writes, False)
        ins = fn(self.eng[e])
        if sig:
            self.cnt[e] += 1
            ins.then_inc(self.sem[e], 1)
            v = self.cnt[e]
            self.pending[e] = False
        else:
            v = self.cnt[e] + 1
            self.pending[e] = True
        self._commit(e, reads, writes, e, v, False)
        self.nins += 1
        return ins

    def dma(self, q, out, in_, reads=(), writes=(), **kw):
        self._deps(q, reads, writes, True)
        s = self.ring[q][self.ringpos[q] % len(self.ring[q])]
        self.ringpos[q] += 1
        if self.cnt[s] > 0:
            self._wait(q, s, self.cnt[s])
        ins = self.eng[q].dma_start(out=out, in_=in_, **kw)
        self.cnt[s] += 16
        ins.then_inc(self.sem[s], 16)
        self._commit(q, reads, writes, s, self.cnt[s], True)
        self.nins += 1
        return ins

    def barrier(self):
        for e in self.eng:
            assert not self.pending[e]
        for e in self.eng:
            for s in self.sem:
                if self.cnt[s] > 0:
                    self._wait(e, s, self.cnt[s])
        self.lastw = {}
        self.readers = {}


def build(dbg=False, nlayers=2, stop=None, lite=False, skip01=False):
    nc = bass.Bass("TRN2", target_bir_lowering=False)

    def din(name, shape, dt=F32):
        return nc.dram_tensor(name, list(shape), dt, kind="ExternalInput").ap()

    def scr(name, shape, dt):
        return nc.dram_tensor(name, list(shape), dt, kind=("ExternalOutput" if dbg else "Internal")).ap()

    x_in = din("x", [T, D])
    ctx_in = din("ctx", [TC, D])
    ccol_in = din("ccol", [128, 8, 2])
    w_ada = din("w_ada", [2, D, 6 * D])
    b_ada = din("b_ada", [2, 6 * D])
    b_ada_col = din("b_ada_col", [2, 128, 48])
    w_in = din("w_in", [2, D, DIN])
    w_pool = din("w_pool", [2, 4, 128, 128])
    pscale_col = din("pscale_col", [2, 128, 4])
    lbl_in = din("lbl", [128, 2, 2, 4])
    gain_col = din("gain_col", [2, 128, 4])
    tb_in = din("tb", [2, 8, 128, 8 * 4 * 64])
    w_br_a = din("w_br_a", [2, 512, D])
    w_br_b = din("w_br_b", [2, 512, D])
    w_br_c = din("w_br_c", [2, 512, D])
    w_out = din("w_out", [2, D, D])
    lnp_in = din("lnp", [2, 4, D])
    w_r = din("w_r", [2, D, 36])
    b_r = din("b_r", [2, 36])
    w_gate = din("w_gate", [2, 32, D, 512] if not lite else [2, 1, 1, 1])
    w_up = din("w_up", [2, 32, D, 512] if not lite else [2, 1, 1, 1])
    w_down = din("w_down", [2, 32, 512, D] if not lite else [2, 1, 1, 1])
    ident_in = din("ident", [128, 128])
    masks_in = din("masks", [2, 128, 128], mybir.dt.int32)
    invc_in = din("invc", [4, PADL])
    cm_in = din("cm", [128, 4])
    out = nc.dram_tensor("out", [T, D], F32, kind="ExternalOutput").ap()

    xs = scr("xs", [TT, D], F32)
    zaT = scr("zaT", [512, TT], F32)
    qsT = scr("qsT", [512, TT], F32)
    zfT = scr("zfT", [1024, TT], F32)
    sgT = scr("sgT", [512, TT], BF16)
    qT = scr("qT", [512, TT], BF16)
    kT = scr("kT", [512, TT], BF16)
    gT = scr("gT", [3072, TT], BF16)
    vi = scr("vi", [TT, 512], BF16)
    vv = scr("vv", [TT, 512], BF16)
    yaT = scr("yaT", [512, TT], BF16)
    ybT = scr("ybT", [512, TT], BF16)
    ycT = scr("ycT", [512, TT], BF16)
    h2T = scr("h2T", [D, TT], BF16)
    comb = scr("comb", [TT, 32], F32)

    with ExitStack() as es:
        c = Ctx(nc, es)

        uid = [0]

        def sb(st, name, shape, dt):
            uid[0] += 1
            return st.enter_context(nc.sbuf_tensor("sb%d_%s" % (uid[0], name), list(shape), dt))

        def ps(st, name, shape, dt=F32):
            uid[0] += 1
            return st.enter_context(nc.psum_tensor("ps%d_%s" % (uid[0], name), list(shape), dt))

        ident = sb(es, "ident", [128, 128], BF16)
        ones32 = sb(es, "ones32", [128, 128], F32)
        ones16 = sb(es, "ones16", [128, 128], BF16)
        masks = sb(es, "masks", [128, 2, 128], mybir.dt.int32)
        cm = sb(es, "cm", [128, 4], F32)
        eps_ln = sb(es, "eps_ln", [128, 1], F32)
        eps_rms = sb(es, "eps_rms", [128, 1], F32)
        lbc = sb(es, "lbc", [128, 2, 2, 4], F32)
        omlc = sb(es, "omlc", [128, 2, 2, 4], F32)
        lbl = sb(es, "lbl", [128, 2, 2, 4], F32)
        modc = sb(es, "modc", [128, 48, 2], F32)
        G = sb(es, "G", [128, 2, 2, 1024], F32)
        lnp = sb(es, "lnp", [128, 4, 1024], F32)
        gcol = sb(es, "gcol", [128, 4], F32)
        pscol = sb(es, "pscol", [128, 4], F32)

        c.dma('pool', ident[:], ident_in[:, :], writes=['ident'])
        c.dma('sp', masks[:], masks_in.rearrange("m p q -> p m q"), writes=['masks'])
        c.dma('sp', cm[:], cm_in[:, :], writes=['cm'])
        c.dma('sp', lbl[:], lbl_in[:, :, :, :], writes=['lbl'])
        c.op('dve', lambda v: v.memset(ones32[:], 1.0), writes=['ones32'])
        c.op('dve', lambda v: v.memset(ones16[:], 1.0), writes=['ones16'])
        c.op('dve', lambda v: v.memset(eps_ln[:], 1e-5), writes=['eps_ln'])
        c.op('dve', lambda v: v.memset(eps_rms[:], 1e-6), writes=['eps_rms'])
        c.op('dve', lambda v: v.memset(lbc[:], 0.0), writes=['lbc'])
        c.op('dve', lambda v: v.tensor_tensor(lbl[:, :, 1, :], lbl[:, :, 1, :], lbl[:, :, 0, :], ALU.subtract), reads=['lbl'], writes=['lbl'])
        c.op('act', lambda a: a.activation(lbc[:, :, 1, :], lbl[:, :, 1, :], AF.Sigmoid), reads=['lbl', 'lbc'], writes=['lbc'])
        c.op('dve', lambda v: v.tensor_scalar(omlc[:], lbc[:], -1.0, 1.0, ALU.mult, ALU.add), reads=['lbc'], writes=['omlc'])
        c.dma('sp', xs[0:TC, :], ctx_in[:, :])
        c.dma('sp', xs[TC:TT, :], x_in[:, :])

        rot = {}

        def nxt(name, n):
            i = rot.get(name, 0)
            rot[name] = i + 1
            return i % n

        def ln_stats(st, xap, xkey, slot):
            stt, mv, rs = st['st'][slot], st['mv'][slot], st['rs'][slot]
            for i in range(2):
                c.op('dve', lambda v: v.bn_stats(stt[:, i, :], xap[:, i * 512:(i + 1) * 512]), reads=[xkey], writes=['st%d_%d' % (slot, i)])
            c.op('dve', lambda v: v.bn_aggr(mv[:], stt[:].rearrange("p a b -> p (a b)")), reads=['st%d_0' % slot, 'st%d_1' % slot], writes=['mv%d' % slot])
            c.op('act', lambda a: a.activation(rs[:], mv[:, 1:2], AF.Sqrt, bias=eps_ln[:], scale=1.0), reads=['mv%d' % slot, 'eps_ln'], writes=['rs%d' % slot])
            c.op('dve', lambda v: v.reciprocal(rs[:], rs[:]), reads=['rs%d' % slot], writes=['rs%d' % slot])
            return mv, rs

        def ln_to_hT(st, xap, xkey, dst, dkey, sc_chunk0, sh_chunk0, w, slot=None):
            if slot is None:
                slot = nxt('lnslot', 2)
            mv, rs = ln_stats(st, xap, xkey, slot)
            hn, pT = st['hn'][slot], st['pT'][slot]
            c.op('dve', lambda v: v.tensor_scalar(hn[:], xap, mv[:, 0:1], rs[:], ALU.subtract, ALU.mult), reads=[xkey, 'mv%d' % slot, 'rs%d' % slot], writes=['hn%d' % slot])
            for k in range(8):
                c.op('pe', lambda t: t.transpose(pT[:, k, :], hn[:, k * 128:(k + 1) * 128], ident[:]), reads=['hn%d' % slot, 'ident'], writes=['pT%d' % slot])
            for k in range(8):
                c.op('act', lambda a: a.activation(dst(k), pT[:, k, :], AF.Identity, bias=modc[:, sh_chunk0 + k, w:w + 1], scale=modc[:, sc_chunk0 + k, w:w + 1]),
                     reads=['pT%d' % slot, 'modc'], writes=[dkey])

        def ln_bufs(st, full=True):
            d = {'st': [], 'mv': [], 'rs': [], 'hn': [], 'pT': []}
            for i in range(2):
                d['st'].append(sb(st, "lnst%d" % i, [128, 2, 6], F32))
                d['mv'].append(sb(st, "lnmv%d" % i, [128, 2], F32))
                d['rs'].append(sb(st, "lnrs%d" % i, [128, 1], F32))
                if full:
                    d['hn'].append(sb(st, "lnhn%d" % i, [128, 1024], BF16))
                    d['pT'].append(ps(st, "lnpT%d" % i, [128, 8, 128], BF16))
            return d

        for l in range(nlayers):
            last = (l == 1)
            c.barrier()
            if not skip01:
                with ExitStack() as st:
                    wada = sb(st, "wada", [128, 8, 6144], BF16)
                    ccol = sb(st, "ccol", [128, 8, 2], F32)
                    sc = sb(st, "sc", [128, 8, 2], BF16)
                    scb = sb(st, "scb", [128, 2, 8, 128], BF16)
                    bcol = sb(st, "bcol", [128, 48], F32)
                    bbc = sb(st, "bbc", [128, 2, 1024], F32)
                    pc = ps(st, "pc", [128, 48, 2])
                    pg = [ps(st, "pg%d" % i, [128, 512]) for i in range(2)]
                    for k in range(8):
                        c.dma('pool', wada[:, k, :], w_ada[l, k * 128:(k + 1) * 128, :], writes=['wada%d' % k])
                    c.dma('sp', ccol[:], ccol_in[:, :, :], writes=['ccol'])
                    c.dma('sp', gcol[:], gain_col[l], writes=['gcol'])
                    c.dma('sp', pscol[:], pscale_col[l], writes=['pscol'])
                    c.dma('sp', bcol[:], b_ada_col[l], writes=['bcol'])
                    c.dma('sp', bbc[:, 0, :], b_ada[l, 2048:3072].partition_broadcast(128), writes=['bbc0'])
                    c.dma('sp', bbc[:, 1, :], b_ada[l, 5120:6144].partition_broadcast(128), writes=['bbc1'])
                    for i in range(4):
                        c.dma('sp', lnp[:, i, :], lnp_in[l, i, :].partition_broadcast(128), writes=['lnp'])
                    c.op('act', lambda a: a.activation(sc[:], ccol[:], AF.Silu), reads=['ccol'], writes=['sc'])
                    for w in range(2):
                        for k in range(8):
                            c.op('dve', lambda v: v.tensor_copy(scb[:, w, k, :], sc[:, k, w:w + 1].to_broadcast([128, 128])), reads=['sc'], writes=['scb'])
                    for j in range(48):
                        for k in range(8):
                            c.op('pe', lambda t: t.matmul(pc[:, j, :], wada[:, k, j * 128:(j + 1) * 128], sc[:, k, :], start=(k == 0), stop=(k == 7)),
                                 reads=['wada%d' % k, 'sc'], writes=['pc'], sig=(k == 7))
                    for w in range(2):
                        c.op('dve', lambda v: v.tensor_tensor(modc[:, :, w], pc[:, :, w], bcol[:], ALU.add), reads=['pc', 'bcol'], writes=['modc'])
                    for ch0 in (8, 32):
                        c.op('dve', lambda v: v.tensor_scalar(modc[:, ch0:ch0 + 8, :], modc[:, ch0:ch0 + 8, :], 1.0, None, ALU.add), reads=['modc'], writes=['modc'])
                    for w in range(2):
                        for gi, c0 in enumerate((2048, 5120)):
                            for half in range(2):
                                pi = nxt('pg', 2)
                                for k in range(8):
                                    c.op('pe', lambda t: t.matmul(pg[pi][:], scb[:, w, k, :], wada[:, k, c0 + half * 512:c0 + (half + 1) * 512], start=(k == 0), stop=(k == 7)),
                                         reads=['scb', 'wada%d' % k], writes=['pg%d' % pi], sig=(k == 7))
                                c.op('dve', lambda v: v.tensor_tensor(G[:, gi, w, half * 512:(half + 1) * 512], pg[pi][:], bbc[:, gi, half * 512:(half + 1) * 512], ALU.add),
                                     reads=['pg%d' % pi, 'bbc%d' % gi], writes=['G'])
            if stop == 'p0':
                break

            c.barrier()
            if not skip01:
                with ExitStack() as st:
                    hT = sb(st, "hT", [128, 8, TT], BF16)
                    lb_ = ln_bufs(st)
                    xt = [sb(st, "xt%d" % i, [128, 1024], F32) for i in range(3)]
                    win = [sb(st, "win%d" % i, [128, 8, 512], BF16) for i in range(2)]
                    stg32 = [sb(st, "stg32_%d" % i, [128, 512], F32) for i in range(3)]
                    stg16 = [sb(st, "stg16_%d" % i, [128, 512], BF16) for i in range(3)]
                    pm = [ps(st, "pm%d" % i, [128, 512]) for i in range(4)]
                    def emit_ln(tl):
                        xi = nxt('xt', 3)
                        w = 1 if tl < 2 else 0
                        c.dma('sp', xt[xi][:], xs[tl * 128:(tl + 1) * 128, :], writes=['xt%d' % xi])
                        ln_to_hT(lb_, xt[xi][:], 'xt%d' % xi, lambda k: hT[:, k, tl * 128:(tl + 1) * 128], 'hT%d' % tl, 8, 0, w)
                    groups = [(0, 256)] + [(256 + i * 512, 512) for i in range(8)]
                    gtiles = [list(range(t0_ // 128, (t0_ + n_) // 128)) for (t0_, n_) in groups]
                    for gi_ in range(len(groups)):
                        for tl_ in gtiles[gi_]:
                            emit_ln(tl_)
                    fdst = {0: (zaT, 0, F32, None), 1: (qsT, 0, F32, AF.Silu), 2: (zfT, 0, F32, None), 3: (zfT, 512, F32, None),
                            5: (sgT, 0, BF16, AF.Silu), 6: (qT, 0, BF16, 'q'), 7: (kT, 0, BF16, None)}
                    for i in range(6):
                        fdst[9 + i] = (gT, i * 512, BF16, AF.Sigmoid)
                    for cc in range(15):
                        ws = nxt('win', 2)
                        c.dma('pool', win[ws][:], w_in[l, :, cc * 512:(cc + 1) * 512].rearrange("(k p) n -> p k n", p=128), writes=['win%d' % ws])
                        for gi_, (t0, n) in enumerate(groups):
                            tiles = list(range(t0 // 128, (t0 + n) // 128))
                            hkeys = ['hT%d' % t for t in tiles]
                            if cc in (4, 8):
                                dstd = vi if cc == 4 else vv
                                for tl in tiles:
                                    pi = nxt('pm', 4)
                                    for k in range(8):
                                        c.op('pe', lambda t: t.matmul(pm[pi][:], hT[:, k, tl * 128:(tl + 1) * 128], win[ws][:, k, :], start=(k == 0), stop=(k == 7)),
                                             reads=['hT%d' % tl, 'win%d' % ws], writes=['pm%d' % pi], sig=(k == 7))
                                    si = nxt('stg16', 3)
                                    c.op('dve', lambda v: v.tensor_copy(stg16[si][:], pm[pi][:]), reads=['pm%d' % pi], writes=['stg16_%d' % si])
                                    c.dma('sp', dstd[tl * 128:(tl + 1) * 128, :], stg16[si][:], reads=['stg16_%d' % si])
                            else:
                                dd, r0, dt, fn = fdst[cc]
                                for sub in range(4):
                                    pi = nxt('pm', 4)
                                    for k in range(8):
                                        c.op('pe', lambda t: t.matmul(pm[pi][:, :n], win[ws][:, k, sub * 128:(sub + 1) * 128], hT[:, k, t0:t0 + n], start=(k == 0), stop=(k == 7)),
                                             reads=hkeys + ['win%d' % ws], writes=['pm%d' % pi], sig=(k == 7))
                                    if dt == F32:
                                        si = nxt('stg32', 3)
                                        stg, skey = stg32[si], 'stg32_%d' % si
                                    else:
                                        si = nxt('stg16', 3)
                                        stg, skey = stg16[si], 'stg16_%d' % si
                                    if fn is None:
                                        c.op('dve', lambda v: v.tensor_copy(stg[:, :n], pm[pi][:, :n]), reads=['pm%d' % pi], writes=[skey])
                                    elif fn == 'q':
                                        c.op('act', lambda a: a.activation(stg[:, :n], pm[pi][:, :n], AF.Identity, scale=0.125), reads=['pm%d' % pi], writes=[skey])
                                    else:
                                        c.op('act', lambda a: a.activation(stg[:, :n], pm[pi][:, :n], fn), reads=['pm%d' % pi], writes=[skey])
                                    rr = r0 + sub * 128
                                    c.dma('sp', dd[rr:rr + 128, t0:t0 + n], stg[:, :n], reads=[skey])
            if stop == 'p1':
                break

            slabs = [(0, 256)] + [(256 + i * 512, 512) for i in range(8)]
            SM = 512
            order = [list(range(NT)), [1, 0] + list(range(NT - 1, 1, -1))]
            P2 = {'p2a0': 0, 'p2a1': 1, 'p2a2': 2, 'p2a': 3, 'p2b': 4}.get(stop, 5)
            for h in range(4):
                c.barrier()
                with ExitStack() as st:
                    seg = sb(st, "seg", [128, SM], F32)
                    tmp = [[sb(st, "tmp%d_%d" % (d_, i), [128, SM], F32) for i in range(6)] for d_ in range(2)]
                    tq = [sb(st, "tq%d" % d_, [128, SM], F32) for d_ in range(2)]
                    kdS = [sb(st, "kdS%d" % d_, [128, SM], BF16) for d_ in range(2)]
                    ksS = [sb(st, "ksS%d" % d_, [128, SM], BF16) for d_ in range(2)]
                    qd = [sb(st, "qd%d" % d, [128, TT], BF16) for d in range(2)]
                    AT = [sb(st, "AT%d" % d, [128, NT, 128], BF16) for d in range(2)]
                    ksTm = [sb(st, "ksT%d" % d, [128, NT, 128], BF16) for d in range(2)]
                    vihm = sb(st, "vihm", [128, NT, 4, 128], BF16)
                    gdec = [sb(st, "gdec%d" % d, [128, NT * 4], F32) for d in range(2)]
                    emdec = [sb(st, "emdec%d" % d, [128, NT * 4 + 1], F32) for d in range(2)]
                    vih = sb(st, "vih", [128, NT, 128], BF16)
                    od = [sb(st, "od%d" % d, [128, TT], F32) for d in range(2)]
                    S32 = [[sb(st, "S32_%d_%d" % (d, i), [128, 128], F32) for i in range(2)] for d in range(2)]
                    S16 = [[sb(st, "S16_%d_%d" % (d, i), [128, 128], BF16) for i in range(8)] for d in range(2)]
                    pK = ps(st, "pK", [128, 8, 128], BF16)
                    pO = [ps(st, "pO%d" % d, [128, 512]) for d in range(2)]
                    pKV = [ps(st, "pKV%d" % i, [128, 512]) for i in range(4)]
                    pA = ps(st, "pA", [128, 512])

                    c.op('dve', lambda v: v.memset(seg[:], 1.0), writes=['seg'])
                    for d in range(2):
                        c.op('pool', lambda g_: g_.memset(AT[d][:], 0.0), writes=['AT%d' % d])
                        c.op('dve', lambda v: v.memset(emdec[d][:], 1.0), writes=['emdec%d' % d])
                    c.op('dve', lambda v: v.memset(seg[:].rearrange("p (c k) -> p c k", k=32)[:, :, 0:1], 0.0), writes=['seg'])
                    c.dma('sp', vih[:], vi[:, h * 128:(h + 1) * 128].rearrange("(t p) d -> p t d", p=128), writes=['vih'])
                    for tl in range(NT):
                        for c4 in range(4):
                            c.op('pool', lambda g_: g_.tensor_scalar(vihm[:, tl, c4, :], vih[:, tl, :], cm[:, c4:c4 + 1], 1.0, ALU.mult, ALU.mult), reads=['vih', 'cm'], writes=['vihm'])
                    def slab_ops(d, s0, n):
                        lbcol = lbc[:, d, l, h:h + 1]
                        omcol = omlc[:, d, l, h:h + 1]
                        K_ = ['t%d_%d' % (d, i) for i in range(6)]
                        A_, B_, C_, D_, E_, F_ = [t_[:, :n] for t_ in tmp[d]]
                        nch = n // 32
                        c.dma('sp', A_, zfT[d * 512 + h * 128:d * 512 + (h + 1) * 128, s0:s0 + n], writes=[K_[0]])
                        yield
                        c.dma('sp', tq[d][:, :n], qsT[h * 128:(h + 1) * 128, s0:s0 + n], writes=['tq%d' % d])
                        yield
                        c.op('act', lambda a: a.activation(B_, A_, AF.Sigmoid), reads=[K_[0]], writes=[K_[1]])
                        yield
                        c.op('act', lambda a: a.activation(C_, A_, AF.Sigmoid, scale=-1.0), reads=[K_[0]], writes=[K_[2]])
                        yield
                        c.op('act', lambda a: a.activation(B_, B_, AF.Ln, bias=lbcol, scale=omcol), reads=[K_[1], 'lbc', 'omlc'], writes=[K_[1]])
                        yield
                        c.op('dve', lambda v: v.tensor_tensor_scan(D_, seg[:, :n], B_, 0.0, ALU.mult, ALU.add), reads=['seg', K_[1]], writes=[K_[3]])
                        yield
                        D3 = D_.rearrange("p (c k) -> p c k", k=32)
                        if d == 0:
                            bb, bkey = D_, K_[3]
                            b3 = D3
                            blast = D3[:, :, 31:32]
                        else:
                            E3 = E_.rearrange("p (c k) -> p c k", k=32)
                            c.op('dve', lambda v: v.tensor_tensor(E_, B_, D_, ALU.subtract), reads=[K_[1], K_[3]], writes=[K_[4]])
                            yield
                            c.op('dve', lambda v: v.tensor_tensor(E3, E3, D3[:, :, 31:32].to_broadcast([128, nch, 32]), ALU.add), reads=[K_[4], K_[3]], writes=[K_[4]])
                            yield
                            bb, bkey = E_, K_[4]
                            b3 = E3
                            blast = E3[:, :, 0:1]
                        c.op('act', lambda a: a.activation(gdec[d][:, s0 // 32:s0 // 32 + nch], blast.rearrange("p c k -> p (c k)"), AF.Exp), reads=[bkey], writes=['gdec%d' % d])
                        yield
                        A3 = A_.rearrange("p (c k) -> p c k", k=32)
                        c.op('dve', lambda v: v.tensor_tensor(A3, blast.to_broadcast([128, nch, 32]), b3, ALU.subtract), reads=[bkey, K_[0]], writes=[K_[0]])
                        yield
                        c.op('act', lambda a: a.activation(A_, A_, AF.Exp), reads=[K_[0]], writes=[K_[0]])
                        yield
                        B3 = B_.rearrange("p (c k) -> p c k", k=32)
                        c.op('act', lambda a: a.activation(emdec[d][:, s0 // 32:s0 // 32 + nch], b3[:, :, 16:17].rearrange("p c k -> p (c k)"), AF.Exp), reads=[bkey], writes=['emdec%d' % d])
                        yield
                        c.op('dve', lambda v: v.tensor_tensor(B3, b3, b3[:, :, 16:17].to_broadcast([128, nch, 32]), ALU.subtract), reads=[bkey, K_[1]], writes=[K_[1]])
                        yield
                        c.op('act', lambda a: a.activation(F_, B_, AF.Exp, scale=-1.0), reads=[K_[1]], writes=[K_[5]])
                        yield
                        c.op('act', lambda a: a.activation(B_, B_, AF.Exp), reads=[K_[1]], writes=[K_[1]])
                        yield
                        c.op('dve', lambda v: v.tensor_tensor(qd[d][:, s0:s0 + n], tq[d][:, :n], B_, ALU.mult), reads=['tq%d' % d, K_[1]], writes=['qd%d' % d])
                        yield
                        c.op('dve', lambda v: v.scalar_tensor_tensor(kdS[d][:, :n], C_, omcol, F_, ALU.mult, ALU.mult), reads=[K_[2], K_[5], 'omlc'], writes=['kdS%d' % d])
                        yield
                        c.op('dve', lambda v: v.scalar_tensor_tensor(ksS[d][:, :n], C_, omcol, A_, ALU.mult, ALU.mult), reads=[K_[2], K_[0], 'omlc'], writes=['ksS%d' % d])
                        yield
                        for ti in range(n // 128 if P2 != 1 else 0):
                            tl = s0 // 128 + ti
                            c.op('pe', lambda t: t.matmul(pA[:, 0:128], kdS[d][:, ti * 128:(ti + 1) * 128], qd[d][:, tl * 128:(tl + 1) * 128], start=True, stop=True),
                                 reads=['kdS%d' % d, 'qd%d' % d], writes=['pA'])
                            c.op('dve', lambda v: v.copy_predicated(AT[d][:, tl, :], masks[:, d, :], pA[:, 0:128]), reads=['pA', 'masks'], writes=['AT%d' % d])
                            yield
                            c.op('pe', lambda t: t.transpose(pK[:, 0, :], ksS[d][:, ti * 128:(ti + 1) * 128], ident[:]), reads=['ksS%d' % d, 'ident'], writes=['pK'])
                            c.op('act', lambda a: a.copy(ksTm[d][:, tl, :], pK[:, 0, :]), reads=['pK'], writes=['ksTm%d' % d])
                            yield
                    for (s0, n) in slabs:
                        gens = [slab_ops(d_, s0, n) for d_ in range([0, 1, 1, 2, 2, 2][P2])]
                        while gens:
                            for g_ in list(gens):
                                try:
                                    next(g_)
                                except StopIteration:
                                    gens.remove(g_)
                    RING = 8
                    if P2 >= 4:
                        for d in range(2):
                            c.op('dve', lambda v: v.memset(S32[d][0][:], 0.0), writes=['S32_%d_0' % d])
                            c.op('dve', lambda v: v.memset(S16[d][0][:], 0.0), writes=['S16_%d_0' % d])
                        cseq = [[tl_ * 4 + ci_ for tl_ in order[d_] for ci_ in ([0, 1, 2, 3] if d_ == 0 else [3, 2, 1, 0])] + [NT * 4] for d_ in range(2)]

                        def opart(d, s_):
                            tl = order[d][s_]
                            po = pO[d]
                            pok = 'pO%d' % d
                            c.op('pe', lambda t: t.matmul(po[:, 0:128], vih[:, tl, :], AT[d][:, tl, :], start=True, stop=False), reads=['vih', 'AT%d' % d], writes=[pok], sig=False)
                            for n_i in range(4):
                                k = s_ * 4 + n_i
                                cpos = cseq[d][k]
                                ci = cpos % 4
                                c.op('pe', lambda t: t.matmul(po[:, ci * 32:(ci + 1) * 32], S16[d][k % RING][:], qd[d][:, cpos * 32:(cpos + 1) * 32], start=False, stop=(n_i == 3)),
                                     reads=['S16_%d_%d' % (d, k % RING), 'qd%d' % d], writes=[pok], sig=(n_i == 3))
                            c.op('act', lambda a: a.copy(od[d][:, tl * 128:(tl + 1) * 128], po[:, 0:128]), reads=[pok], writes=['od%d' % d])

                        for s_ in range(NT):
                            for d in range(2):
                                tl = order[d][s_]
                                bank = pKV[d * 2 + s_ % 2]
                                bkey = 'pKV%d' % (d * 2 + s_ % 2)
                                for n_i in range(4):
                                    ci = cseq[d][s_ * 4 + n_i] % 4
                                    c.op('pe', lambda t: t.matmul(bank[:, n_i * 128:(n_i + 1) * 128], ksTm[d][:, tl, :], vihm[:, tl, ci, :], start=True, stop=True),
                                         reads=['ksTm%d' % d, 'vihm'], writes=[bkey], sig=(n_i == 3))
                            if s_ >= 1:
                                for d in range(2):
                                    opart(d, s_ - 1)
                            for n_i in range(4):
                                for d in range(2):
                                    bank = pKV[d * 2 + s_ % 2]
                                    bkey = 'pKV%d' % (d * 2 + s_ % 2)
                                    k = s_ * 4 + n_i
                                    cur = k % 2
                                    nx = 1 - cur
                                    cpos = cseq[d][k]
                                    cnext = cseq[d][k + 1]
                                    c.op('dve', lambda v: v.scalar_tensor_tensor(S32[d][nx][:], S32[d][cur][:], gdec[d][:, cpos:cpos + 1], bank[:, n_i * 128:(n_i + 1) * 128], ALU.mult, ALU.add),
                                         reads=['S32_%d_%d' % (d, cur), 'gdec%d' % d, bkey], writes=['S32_%d_%d' % (d, nx)])
                                    c.op('pool', lambda g: g.tensor_scalar(S16[d][(k + 1) % RING][:], S32[d][nx][:], emdec[d][:, cnext:cnext + 1], 1.0, ALU.mult, ALU.mult),
                                         reads=['S32_%d_%d' % (d, nx), 'emdec%d' % d], writes=['S16_%d_%d' % (d, (k + 1) % RING)])
                        for d in range(2):
                            opart(d, NT - 1)
                    blocks = [(i * 512, min(512, TT - i * 512)) for i in range(9)] if P2 >= 5 else []
                    c.barrier()
                    sq = tmp[0][0:2]
                    rt = tmp[0][2:4]
                    sgt = [kdS[0], ksS[0]]
                    ybt = [sb(st, "ybt%d" % i, [128, 512], BF16) for i in range(2)]
                    for (b0, n) in blocks:
                        i2 = nxt('ro', 2)
                        osl = od[0][:, b0:b0 + n]
                        c.dma('sp', sgt[i2][:, :n], sgT[h * 128:(h + 1) * 128, b0:b0 + n], writes=['sgt%d' % i2])
                        c.op('dve', lambda v: v.tensor_tensor(osl, osl, od[1][:, b0:b0 + n], ALU.add), reads=['od0', 'od1'], writes=['od0'])
                        c.op('act', lambda a: a.activation(sq[i2][:, :n], osl, AF.Square), reads=['od0'], writes=['sq%d' % i2])
                        c.op('pe', lambda t: t.matmul(pA[:, :n], ones32[:], sq[i2][:, :n], start=True, stop=True), reads=['ones32', 'sq%d' % i2], writes=['pA'])
                        c.op('act', lambda a: a.activation(rt[i2][:, :n], pA[:, :n], AF.Sqrt, bias=eps_rms[:], scale=1.0 / 128), reads=['pA', 'eps_rms'], writes=['rt%d' % i2])
                        c.op('dve', lambda v: v.reciprocal(rt[i2][:, :n], rt[i2][:, :n]), reads=['rt%d' % i2], writes=['rt%d' % i2])
                        c.op('dve', lambda v: v.tensor_tensor(rt[i2][:, :n], rt[i2][:, :n], osl, ALU.mult), reads=['rt%d' % i2, 'od0'], writes=['rt%d' % i2])
                        c.op('dve', lambda v: v.scalar_tensor_tensor(ybt[i2][:, :n], rt[i2][:, :n], gcol[:, h:h + 1], sgt[i2][:, :n], ALU.mult, ALU.mult),
                             reads=['rt%d' % i2, 'gcol', 'sgt%d' % i2], writes=['ybt%d' % i2])
                        c.dma('sp', ybT[h * 128:(h + 1) * 128, b0:b0 + n], ybt[i2][:, :n], reads=['ybt%d' % i2])
                if P2 < 5:
                    break
            if stop == 'p2' or P2 < 5:
                break
            for h in range(8):
                c.barrier()
                with ExitStack() as st:
                    qh = sb(st, "qh", [64, TT], BF16)
                    kh = sb(st, "kh", [64, TT], BF16)
                    vA = sb(st, "vA", [128, 32, 64], BF16)
                    vB = sb(st, "vB", [128, 31, 64], BF16)
                    vC = sb(st, "vC", [128, 2, 64], BF16)
                    tbh = sb(st, "tbh", [128, 8, 4, 64], BF16)
                    ych = sb(st, "ych", [64, TT], BF16)
                    E = [sb(st, "E%d" % i, [128, 6, 64], BF16) for i in range(2)]
                    rec = [sb(st, "rec%d" % i, [64, 64], F32) for i in range(2)]
                    Esum = [sb(st, "Esum%d" % i, [128, 64], F32) for i in range(2)]
                    E2 = sb(st, "E2", [128, 2, 256], BF16)
                    rec2 = sb(st, "rec2", [64, 256], F32)
                    pS = [ps(st, "pS%d" % i, [128, 512]) for i in range(2)]
                    pN = [ps(st, "pN%d" % i, [128, 512]) for i in range(2)]
                    pS2 = ps(st, "pS2", [128, 512])
                    pN2 = ps(st, "pN2", [128, 512])
                    c.dma('sp', qh[:], qT[h * 64:(h + 1) * 64, :], writes=['qh'])
                    c.dma('sp', kh[:], kT[h * 64:(h + 1) * 64, :], writes=['kh'])
                    c.dma('sp', vA[:], vv[256:4352, h * 64:(h + 1) * 64].rearrange("(t p) d -> p t d", p=128), writes=['vA'])
                    c.dma('sp', vB[:], vv[320:4288, h * 64:(h + 1) * 64].rearrange("(t p) d -> p t d", p=128), writes=['vB'])
                    c.dma('sp', vC[:], vv[0:256, h * 64:(h + 1) * 64].rearrange("(t p) d -> p t d", p=128), writes=['vC'])
                    c.dma('pool', tbh[:], tb_in[l, h].rearrange("p (a b q) -> p a b q", a=8, b=4), writes=['tbh'])
                    c.op('act', lambda a: a.activation(tbh[:], tbh[:], AF.Exp), reads=['tbh'], writes=['tbh'])

                    def att_geom(r):
                        r0 = min(max(r - 4, 0), 56)
                        return r0, r - r0, 256 + r * 64, 256 + r0 * 64

                    def att_qk(r):
                        i2 = r % 2
                        r0, delta, qc0, kb = att_geom(r)
                        pSv = pS[i2][:, 0:384].rearrange("p (j q) -> p j q", q=64)
                        for j in range(4):
                            c.op('pe', lambda t: t.matmul(pSv[:, j, :], kh[:, kb + j * 128:kb + (j + 1) * 128], qh[:, qc0:qc0 + 64], start=True, stop=True),
                                 reads=['kh', 'qh'], writes=['pS%d' % i2], sig=False)
                        for j in range(2):
                            c.op('pe', lambda t: t.matmul(pSv[:, 4 + j, :], kh[:, j * 128:(j + 1) * 128], qh[:, qc0:qc0 + 64], start=True, stop=True),
                                 reads=['kh', 'qh'], writes=['pS%d' % i2], sig=(j == 1))
                        c.op('act', lambda a: a.activation(E[i2][:], pSv, AF.Exp), reads=['pS%d' % i2], writes=['E%d' % i2])
                        c.op('pool', lambda g_: g_.tensor_tensor(E[i2][:, 0:4, :], E[i2][:, 0:4, :], tbh[:, delta, :, :], ALU.mult), reads=['E%d' % i2, 'tbh'], writes=['E%d' % i2])
                        c.op('dve', lambda v: v.tensor_reduce(Esum[i2][:], E[i2][:].rearrange("p j q -> p q j"), AX.X, ALU.add), reads=['E%d' % i2], writes=['Esum%d' % i2])

                    def att_pv(r):
                        i2 = r % 2
                        r0, delta, qc0, kb = att_geom(r)

                        def Vt(j):
                            if j >= 4:
                                return vC[:, j - 4, :]
                            if r0 % 2 == 0:
                                return vA[:, r0 // 2 + j, :]
                            return vB[:, (r0 - 1) // 2 + j, :]
                        pNv = pN[i2][0:64, 0:128].rearrange("p (a q) -> p a q", q=64)
                        for j in range(6):
                            c.op('pe', lambda t: t.matmul(pNv[:, 0, :], Vt(j), E[i2][:, j, :], start=(j == 0), stop=(j == 5)),
                                 reads=['vA', 'vB', 'vC', 'E%d' % i2], writes=['pN%d' % i2], sig=False)
                        c.op('pe', lambda t: t.matmul(pNv[:, 1, :], ones32[:, 0:64], Esum[i2][:], start=True, stop=True), reads=['ones32', 'Esum%d' % i2], writes=['pN%d' % i2])
                        c.op('dve', lambda v: v.reciprocal(rec[i2][:], pNv[:, 1, :]), reads=['pN%d' % i2], writes=['rec%d' % i2])
                        c.op('dve', lambda v: v.tensor_tensor(ych[:, qc0:qc0 + 64], pNv[:, 0, :], rec[i2][:], ALU.mult), reads=['pN%d' % i2, 'rec%d' % i2], writes=['ych'])

                    for r in range(64):
                        att_qk(r)
                        if r >= 1:
                            att_pv(r - 1)
                    att_pv(63)
                    if not last:
                        pS2v = pS2[:, 0:512].rearrange("p (j q) -> p j q", q=256)
                        for j in range(2):
                            c.op('pe', lambda t: t.matmul(pS2v[:, j, :], kh[:, j * 128:(j + 1) * 128], qh[:, 0:256], start=True, stop=True),
                                 reads=['kh', 'qh'], writes=['pS2'], sig=(j == 1))
                        c.op('act', lambda a: a.activation(E2[:], pS2v, AF.Exp), reads=['pS2'], writes=['E2'])
                        pN2v = pN2[0:64, 0:512].rearrange("p (a q) -> p a q", q=256)
                        for j in range(2):
                            c.op('pe', lambda t: t.matmul(pN2v[:, 0, :], vC[:, j, :], E2[:, j, :], start=(j == 0), stop=(j == 1)), reads=['vC', 'E2'], writes=['pN2'], sig=False)
                        for j in range(2):
                            c.op('pe', lambda t: t.matmul(pN2v[:, 1, :], ones16[:, 0:64], E2[:, j, :], start=(j == 0), stop=(j == 1)), reads=['ones16', 'E2'], writes=['pN2'], sig=(j == 1))
                        c.op('dve', lambda v: v.reciprocal(rec2[:], pN2v[:, 1, :]), reads=['pN2'], writes=['rec2'])
                        c.op('dve', lambda v: v.tensor_tensor(ych[:, 0:256], pN2v[:, 0, :], rec2[:], ALU.mult), reads=['pN2', 'rec2'], writes=['ych'])
                    cc0 = 256 if last else 0
                    c.dma('sp', ycT[h * 64:(h + 1) * 64, cc0:TT], ych[:, cc0:TT], reads=['ych'])
            if stop == 'p3':
                break

            groups = [(0, 256)] + [(256 + i * 512, 512) for i in range(8)]
            c.barrier()
            with ExitStack() as st:
                U = sb(st, "U", [128, PADL], F32)
                B1 = sb(st, "B1", [128, PADL], F32)
                B2 = sb(st, "B2", [128, PADL], F32)
                invc = sb(st, "invc", [128, PADL], F32)
                Y = sb(st, "Y", [128, PADL], BF16)
                wp = sb(st, "wp", [128, 4, 128], BF16)
                stgp = [sb(st, "stgp%d" % i, [128, 512], BF16) for i in range(2)]
                pP = [ps(st, "pP%d" % i, [128, 512]) for i in range(2)]
                L = PADL
                c.op('dve', lambda v: v.memset(U[:], 0.0), writes=['U'])
                c.dma('pool', wp[:], w_pool[l].rearrange("g c d -> c g d"), writes=['wp'])
                for g in range(4):
                    c.dma('sp', U[:, 32:288], zaT[g * 128:(g + 1) * 128, 0:256], writes=['U'])
                    c.dma('sp', U[:, 352:4448], zaT[g * 128:(g + 1) * 128, 256:TT], writes=['U'])
                    c.dma('sp', invc[:], invc_in[g].partition_broadcast(128), writes=['invc'])
                    c.op('dve', lambda v: v.tensor_tensor(B1[:, 1:L], U[:, 0:L - 1], U[:, 1:L], ALU.add), reads=['U'], writes=['B1'])
                    S, skey = B1, 'B1'
                    if g >= 1:
                        c.op('dve', lambda v: v.tensor_tensor(B2[:, 2:L - 1], B1[:, 1:L - 2], B1[:, 3:L], ALU.add), reads=['B1'], writes=['B2'])
                        S, skey = B2, 'B2'
                    if g >= 2:
                        c.op('dve', lambda v: v.tensor_tensor(B1[:, 4:L - 3], B2[:, 2:L - 5], B2[:, 6:L - 1], ALU.add), reads=['B2'], writes=['B1'])
                        S, skey = B1, 'B1'
                    if g >= 3:
                        c.op('dve', lambda v: v.tensor_tensor(B2[:, 8:L - 7], B1[:, 4:L - 11], B1[:, 12:L - 3], ALU.add), reads=['B1'], writes=['B2'])
                        S, skey = B2, 'B2'
                    c.op('dve', lambda v: v.tensor_tensor(S[:, 32:4448], S[:, 32:4448], invc[:, 32:4448], ALU.mult), reads=[skey, 'invc'], writes=[skey])
                    c.op('dve', lambda v: v.tensor_tensor(Y[:, 32:4448], S[:, 32:4448], U[:, 32:4448], ALU.subtract), reads=[skey, 'U'], writes=['Y'])
                    for (t0, n) in groups:
                        pc0 = t0 + 32 if t0 < 256 else t0 + 96
                        pi = nxt('pP', 2)
                        c.op('pe', lambda t: t.matmul(pP[pi][:, :n], wp[:, g, :], Y[:, pc0:pc0 + n], start=True, stop=True), reads=['wp', 'Y'], writes=['pP%d' % pi])
                        si = nxt('stgp', 2)
                        c.op('act', lambda a: a.activation(stgp[si][:, :n], pP[pi][:, :n], AF.Identity, scale=pscol[:, g:g + 1]), reads=['pP%d' % pi, 'pscol'], writes=['stgp%d' % si])
                        c.dma('sp', yaT[g * 128:(g + 1) * 128, t0:t0 + n], stgp[si][:, :n], reads=['stgp%d' % si])
            if stop == 'p4':
                break

            c.barrier()
            with ExitStack() as st:
                wa = sb(st, "wa", [128, 4, 1024], BF16)
                wbb = sb(st, "wbb", [128, 4, 1024], BF16)
                wc = sb(st, "wc", [64, 8, 1024], BF16)
                wo = sb(st, "wo", [128, 8, 1024], BF16)
                wr = sb(st, "wr", [128, 8, 36], BF16)
                brb = sb(st, "brb", [128, 36], F32)
                ya = [sb(st, "ya%d" % i, [128, 4, 512], BF16) for i in range(2)]
                yb = [sb(st, "yb%d" % i, [128, 4, 512], BF16) for i in range(2)]
                yc = [sb(st, "yc%d" % i, [64, 8, 512], BF16) for i in range(2)]
                gt = [sb(st, "gt%d" % i, [128, 3, 512], BF16) for i in range(2)]
                m1 = sb(st, "m1", [128, 512], F32)
                m2 = sb(st, "m2", [128, 512], F32)
                m3 = sb(st, "m3", [128, 512], F32)
                mT = sb(st, "mT", [128, 8, 512], BF16)
                xt5 = [sb(st, "xt5_%d" % i, [128, 1024], F32) for i in range(2)]
                tt = [sb(st, "tt%d" % i, [128, 1024], F32) for i in range(2)]
                xr = [sb(st, "xr%d" % i, [128, 1024], F32) for i in range(2)]
                x1 = [sb(st, "x1_%d" % i, [128, 1024], F32) for i in range(2)]
                h2t = [sb(st, "h2t%d" % i, [128, 8, 128], BF16) for i in range(2)]
                lb_ = ln_bufs(st)
                rt_ = [{}, {}]
                for nm, shp in (("lg", [128, 36]), ("mg", [128, 1]), ("nmg", [128, 1]), ("eg", [128, 4]), ("sg", [128, 1]), ("pgv", [128, 1]), ("oh", [128, 4]),
                                ("sel", [128, 8]), ("top8", [128, 8]), ("dlt", [128, 1]), ("e2", [128, 1]), ("den", [128, 1]), ("w1", [128, 1]), ("w2", [128, 1]),
                                ("mk1", [128, 8]), ("mk2", [128, 8]), ("c8", [128, 8])):
                    for i_ in range(2):
                        rt_[i_][nm] = sb(st, "r%d_" % i_ + nm, shp, F32)
                cbt = [sb(st, "cbt%d" % i, [128, 32], F32) for i in range(2)]
                pa = ps(st, "pa", [128, 512])
                pb = ps(st, "pb", [128, 512])
                pcc = ps(st, "pcc", [128, 512])
                pmx = [ps(st, "pmx%d" % i, [128, 512]) for i in range(2)]
                pr = ps(st, "pr", [128, 512])
                c.dma('pool', wa[:], w_br_a[l].rearrange("(k p) n -> p k n", p=128), writes=['wa'])
                c.dma('pool', wbb[:], w_br_b[l].rearrange("(k p) n -> p k n", p=128), writes=['wbb'])
                c.dma('pool', wc[:], w_br_c[l].rearrange("(k p) n -> p k n", p=64), writes=['wc'])
                c.dma('pool', wo[:], w_out[l].rearrange("(k p) n -> p k n", p=128), writes=['wo'])
                c.dma('pool', wr[:], w_r[l].rearrange("(k p) n -> p k n", p=128), writes=['wr'])
                c.dma('sp', brb[:], b_r[l].partition_broadcast(128), writes=['brb'])
                gT3 = gT.rearrange("(g q) n -> q g n", g=3)
                grp5 = groups[1:] if last else groups
                for (t0, n) in grp5:
                    w = 1 if t0 < 256 else 0
                    i2 = nxt('g5', 2)
                    c.dma('sp', ya[i2][:, :, :n], yaT[:, t0:t0 + n].rearrange("(k p) n -> p k n", p=128), writes=['ya%d' % i2])
                    c.dma('sp', yb[i2][:, :, :n], ybT[:, t0:t0 + n].rearrange("(k p) n -> p k n", p=128), writes=['yb%d' % i2])
                    c.dma('sp', yc[i2][:, :, :n], ycT[:, t0:t0 + n].rearrange("(k p) n -> p k n", p=64), writes=['yc%d' % i2])
                    for oc in range(8):
                        gi = nxt('gt', 2)
                        c.dma('sp', gt[gi][:, :, :n], gT3[oc * 128:(oc + 1) * 128, :, t0:t0 + n], writes=['gt%d' % gi])
                        osl = slice(oc * 128, (oc + 1) * 128)
                        for k in range(4):
                            c.op('pe', lambda t: t.matmul(pa[:, :n], wa[:, k, osl], ya[i2][:, k, :n], start=(k == 0), stop=(k == 3)), reads=['wa', 'ya%d' % i2], writes=['pa'], sig=(k == 3))
                        for k in range(4):
                            c.op('pe', lambda t: t.matmul(pb[:, :n], wbb[:, k, osl], yb[i2][:, k, :n], start=(k == 0), stop=(k == 3)), reads=['wbb', 'yb%d' % i2], writes=['pb'], sig=(k == 3))
                        for k in range(8):
                            c.op('pe', lambda t: t.matmul(pcc[:, :n], wc[:, k, osl], yc[i2][:, k, :n], start=(k == 0), stop=(k == 7)), reads=['wc', 'yc%d' % i2], writes=['pcc'], sig=(k == 7))
                        c.op('dve', lambda v: v.tensor_tensor(m1[:, :n], pa[:, :n], gt[gi][:, 0, :n], ALU.mult), reads=['pa', 'gt%d' % gi], writes=['m1'])
                        c.op('dve', lambda v: v.tensor_tensor(m2[:, :n], pb[:, :n], gt[gi][:, 1, :n], ALU.mult), reads=['pb', 'gt%d' % gi], writes=['m2'])
                        c.op('pool', lambda g_: g_.tensor_tensor(m1[:, :n], m1[:, :n], m2[:, :n], ALU.add), reads=['m1', 'm2'], writes=['m1'])
                        c.op('dve', lambda v: v.tensor_tensor(m3[:, :n], pcc[:, :n], gt[gi][:, 2, :n], ALU.mult), reads=['pcc', 'gt%d' % gi], writes=['m3'])
                        c.op('pool', lambda g_: g_.tensor_tensor(mT[:, oc, :n], m1[:, :n], m3[:, :n], ALU.add), reads=['m1', 'm3'], writes=['mT'])
                    def tile_ops(tl, sl):
                        tcol = tl * 128 - t0
                        pmxs = pmx if sl == 0 else [pa, pb]
                        pmk = ['pmx0', 'pmx1'] if sl == 0 else ['pa', 'pb']
                        prs, prk = (pr, 'pr') if sl == 0 else (pcc, 'pcc')
                        tts, xrs = tt[sl], xr[sl]
                        tk, xk = 'tt%d' % sl, 'xr%d' % sl
                        K5 = lambda nm: nm + '_%d' % sl
                        c.dma('sp', xt5[sl][:], xs[tl * 128:(tl + 1) * 128, :], writes=['xt5_%d' % sl])
                        yield
                        for half in range(2):
                            for k in range(8):
                                c.op('pe', lambda t: t.matmul(pmxs[half][:], mT[:, k, tcol:tcol + 128], wo[:, k, half * 512:(half + 1) * 512], start=(k == 0), stop=(k == 7)),
                                     reads=['mT', 'wo'], writes=[pmk[half]], sig=(k == 7))
                            yield
                        for half in range(2):
                            hs = slice(half * 512, (half + 1) * 512)
                            c.op('dve', lambda v: v.tensor_tensor(tts[:, hs], pmxs[half][:], G[:, 0, w, hs], ALU.mult), reads=[pmk[half], 'G'], writes=[tk])
                            yield
                        c.op('dve', lambda v: v.scalar_tensor_tensor(xrs[:], xt5[sl][:], ALPHA, tts[:], ALU.mult, ALU.add), reads=['xt5_%d' % sl, tk], writes=[xk])
                        yield
                        mv, rs = ln_stats(lb_, xrs[:], xk, sl)
                        yield
                        c.op('dve', lambda v: v.tensor_scalar(tts[:], xrs[:], mv[:, 0:1], rs[:], ALU.subtract, ALU.mult), reads=[xk, 'mv%d' % sl, 'rs%d' % sl], writes=[tk])
                        yield
                        c.op('pool', lambda g_: g_.tensor_tensor(tts[:], tts[:], lnp[:, 0, :], ALU.mult), reads=[tk, 'lnp'], writes=[tk])
                        yield
                        c.op('pool', lambda g_: g_.tensor_tensor(x1[sl][:], tts[:], lnp[:, 1, :], ALU.add), reads=[tk, 'lnp'], writes=['x1_%d' % sl])
                        yield
                        c.dma('sp', xs[tl * 128:(tl + 1) * 128, :], x1[sl][:], reads=['x1_%d' % sl])
                        yield
                        ln_to_hT(lb_, x1[sl][:], 'x1_%d' % sl, lambda k: h2t[sl][:, k, :], 'h2t%d' % sl, 32, 24, w, slot=sl)
                        yield
                        c.dma('sp', h2T[:, tl * 128:(tl + 1) * 128].rearrange("(k p) n -> p k n", p=128), h2t[sl][:], reads=['h2t%d' % sl])
                        for k in range(8):
                            c.op('pe', lambda t: t.matmul(prs[:, 0:36], h2t[sl][:, k, :], wr[:, k, :], start=(k == 0), stop=(k == 7)), reads=['h2t%d' % sl, 'wr'], writes=[prk], sig=(k == 7))
                        yield
                        R_ = rt_[sl]

                        def V(fn, rd, wr_):
                            c.op('dve', fn, reads=[x_ if x_ in (prk, 'brb') else K5(x_) for x_ in rd], writes=[K5(x_) for x_ in wr_])
                        V(lambda v: v.tensor_tensor(R_['lg'][:], prs[:, 0:36], brb[:], ALU.add), [prk, 'brb'], ['lg'])
                        yield
                        V(lambda v: v.reduce_max(R_['mg'][:], R_['lg'][:, 0:4], AX.X), ['lg'], ['mg'])
                        yield
                        V(lambda v: v.tensor_scalar(R_['nmg'][:], R_['mg'][:], -1.0, None, ALU.mult), ['mg'], ['nmg'])
                        yield
                        c.op('act', lambda a: a.activation(R_['eg'][:], R_['lg'][:, 0:4], AF.Exp, bias=R_['nmg'][:], scale=1.0, accum_out=R_['sg'][:]), reads=[K5('lg'), K5('nmg')], writes=[K5('eg'), K5('sg')])
                        yield
                        V(lambda v: v.reciprocal(R_['pgv'][:], R_['sg'][:]), ['sg'], ['pgv'])
                        yield
                        V(lambda v: v.tensor_scalar(R_['oh'][:], R_['lg'][:, 0:4], R_['mg'][:], None, ALU.is_equal), ['lg', 'mg'], ['oh'])
                        yield
                        le = R_['lg'][:, 4:36].rearrange("p (g e) -> p g e", e=8)
                        V(lambda v: v.tensor_scalar(R_['sel'][:], le[:, 0, :], R_['oh'][:, 0:1], None, ALU.mult), ['lg', 'oh'], ['sel'])
                        yield
                        for g4 in range(1, 4):
                            V(lambda v: v.scalar_tensor_tensor(R_['sel'][:], le[:, g4, :], R_['oh'][:, g4:g4 + 1], R_['sel'][:], ALU.mult, ALU.add), ['lg', 'oh', 'sel'], ['sel'])
                            yield
                        V(lambda v: v.max(R_['top8'][:], R_['sel'][:]), ['sel'], ['top8'])
                        yield
                        V(lambda v: v.tensor_tensor(R_['dlt'][:], R_['top8'][:, 1:2], R_['top8'][:, 0:1], ALU.subtract), ['top8'], ['dlt'])
                        yield
                        c.op('act', lambda a: a.activation(R_['e2'][:], R_['dlt'][:], AF.Exp), reads=[K5('dlt')], writes=[K5('e2')])
                        yield
                        V(lambda v: v.tensor_scalar(R_['den'][:], R_['e2'][:], 1.0, None, ALU.add), ['e2'], ['den'])
                        yield
                        V(lambda v: v.reciprocal(R_['den'][:], R_['den'][:]), ['den'], ['den'])
                        yield
                        V(lambda v: v.tensor_tensor(R_['w1'][:], R_['pgv'][:], R_['den'][:], ALU.mult), ['pgv', 'den'], ['w1'])
                        yield
                        V(lambda v: v.tensor_tensor(R_['w2'][:], R_['w1'][:], R_['e2'][:], ALU.mult), ['w1', 'e2'], ['w2'])
                        yield
                        V(lambda v: v.tensor_scalar(R_['mk1'][:], R_['sel'][:], R_['top8'][:, 0:1], R_['w1'][:], ALU.is_equal, ALU.mult), ['sel', 'top8', 'w1'], ['mk1'])
                        yield
                        V(lambda v: v.tensor_scalar(R_['mk2'][:], R_['sel'][:], R_['top8'][:, 1:2], R_['w2'][:], ALU.is_equal, ALU.mult), ['sel', 'top8', 'w2'], ['mk2'])
                        yield
                        V(lambda v: v.tensor_tensor(R_['c8'][:], R_['mk1'][:], R_['mk2'][:], ALU.add), ['mk1', 'mk2'], ['c8'])
                        yield
                        for g4 in range(4):
                            V(lambda v: v.tensor_scalar(cbt[sl][:, g4 * 8:(g4 + 1) * 8], R_['c8'][:], R_['oh'][:, g4:g4 + 1], None, ALU.mult), ['c8', 'oh'], ['cbt'])
                            yield
                        c.dma('sp', comb[tl * 128:(tl + 1) * 128, :], cbt[sl][:], reads=[K5('cbt')])
                        yield

                    tls = list(range(t0 // 128, (t0 + n) // 128))
                    for i0 in range(0, len(tls), 2):
                        gens = [tile_ops(tl_, j_) for j_, tl_ in enumerate(tls[i0:i0 + 2])]
                        while gens:
                            for g_ in list(gens):
                                try:
                                    next(g_)
                                except StopIteration:
                                    gens.remove(g_)
            if stop == 'p5':
                break

            blocks6 = [(2, 13), (13, 24), (24, 34)] if last else [(0, 12), (12, 23), (23, 34)]
            for (ta, tb_) in blocks6:
                ntile = tb_ - ta
                ntok = ntile * 128
                c0 = ta * 128
                c.barrier()
                with ExitStack() as st:
                    acc = sb(st, "acc", [128, 12, 1024], F32)
                    with ExitStack() as st2:
                        h2 = sb(st2, "h2", [128, 8, 1536], BF16)
                        cbm = sb(st2, "cbm", [128, 12, 32], F32)
                        wg = [sb(st2, "wg%d" % i, [128, 8, 512], BF16) for i in range(2)]
                        wu = [sb(st2, "wu%d" % i, [128, 8, 512], BF16) for i in range(2)]
                        wd = [sb(st2, "wd%d" % i, [128, 4, 1024], BF16) for i in range(2)]
                        sgl = [sb(st2, "sgl%d" % i, [128, 512], F32) for i in range(2)]
                        actT = [sb(st2, "actT%d" % i, [128, 4, 512], BF16) for i in range(2)]
                        pG = [ps(st2, "pG%d" % i, [128, 512]) for i in range(2)]
                        pU = [ps(st2, "pU%d" % i, [128, 512]) for i in range(2)]
                        pO6 = [ps(st2, "pO6_%d" % i, [128, 512]) for i in range(4)]
                        c.dma('sp', h2[:, :, :ntok], h2T[:, c0:c0 + ntok].rearrange("(k p) n -> p k n", p=128), writes=['h2'])
                        c.dma('sp', cbm[:, :ntile, :], comb[c0:c0 + ntok, :].rearrange("(t p) e -> p t e", p=128), writes=['cbm'])
                        def moe_down(e, s, sb0, n, ai):
                            for ti in range(n // 128):
                                tl = sb0 // 128 + ti
                                for half in range(2):
                                    oi = nxt('pO6', 4)
                                    hs = slice(half * 512, (half + 1) * 512)
                                    for dc in range(4):
                                        c.op('pe', lambda t: t.matmul(pO6[oi][:], actT[ai][:, dc, ti * 128:(ti + 1) * 128], wd[s][:, dc, hs], start=(dc == 0), stop=(dc == 3)),
                                             reads=['actT%d' % ai, 'wd%d' % s], writes=['pO6_%d' % oi], sig=(dc == 3))
                                    akey = 'acc%d_%d' % (tl, half)
                                    if e == 0:
                                        c.op('dve', lambda v: v.tensor_scalar(acc[:, tl, hs], pO6[oi][:], cbm[:, tl, 0:1], None, ALU.mult), reads=['pO6_%d' % oi, 'cbm'], writes=[akey])
                                    else:
                                        c.op('dve', lambda v: v.scalar_tensor_tensor(acc[:, tl, hs], pO6[oi][:], cbm[:, tl, e:e + 1], acc[:, tl, hs], ALU.mult, ALU.add),
                                             reads=['pO6_%d' % oi, 'cbm', akey], writes=[akey])

                        prev = None
                        for e in range(32):
                            s = e % 2
                            c.dma('pool', wg[s][:], w_gate[l, e].rearrange("(k p) n -> p k n", p=128), writes=['wg%d' % s])
                            c.dma('pool', wu[s][:], w_up[l, e].rearrange("(k p) n -> p k n", p=128), writes=['wu%d' % s])
                            for sb0 in range(0, ntok, 512):
                                n = min(512, ntok - sb0)
                                ai = nxt('actT', 2)
                                for dc in range(4):
                                    gi = nxt('pG', 2)
                                    for k in range(8):
                                        c.op('pe', lambda t: t.matmul(pG[gi][:, :n], wg[s][:, k, dc * 128:(dc + 1) * 128], h2[:, k, sb0:sb0 + n], start=(k == 0), stop=(k == 7)),
                                             reads=['wg%d' % s, 'h2'], writes=['pG%d' % gi], sig=(k == 7))
                                    for k in range(8):
                                        c.op('pe', lambda t: t.matmul(pU[gi][:, :n], wu[s][:, k, dc * 128:(dc + 1) * 128], h2[:, k, sb0:sb0 + n], start=(k == 0), stop=(k == 7)),
                                             reads=['wu%d' % s, 'h2'], writes=['pU%d' % gi], sig=(k == 7))
                                    c.op('act', lambda a: a.activation(sgl[gi][:, :n], pG[gi][:, :n], AF.Silu), reads=['pG%d' % gi], writes=['sgl%d' % gi])
                                    c.op('dve', lambda v: v.tensor_tensor(actT[ai][:, dc, :n], sgl[gi][:, :n], pU[gi][:, :n], ALU.mult), reads=['sgl%d' % gi, 'pU%d' % gi], writes=['actT%d' % ai])
                                if prev is not None:
                                    moe_down(*prev)
                                if sb0 == 0:
                                    c.dma('pool', wd[s][:], w_down[l, e].rearrange("(k p) n -> p k n", p=128), writes=['wd%d' % s])
                                prev = (e, s, sb0, n, ai)
                        moe_down(*prev)
                    c.barrier()
                    with ExitStack() as st2:
                        lb6 = ln_bufs(st2, full=False)
                        xt6 = [sb(st2, "xt6_%d" % i, [128, 1024], F32) for i in range(2)]
                        t6 = [sb(st2, "t6_%d" % i, [128, 1024], F32) for i in range(2)]
                        for tl in range(ntile):
                            gt_ = ta + tl
                            w = 1 if gt_ < 2 else 0
                            xi = nxt('xt6', 2)
                            c.dma('sp', xt6[xi][:], xs[gt_ * 128:(gt_ + 1) * 128, :], writes=['xt6_%d' % xi])
                            c.op('pool', lambda g_: g_.tensor_tensor(acc[:, tl, :], acc[:, tl, :], G[:, 1, w, :], ALU.mult), reads=['G'], writes=['acct%d' % tl])
                            c.op('dve', lambda v: v.scalar_tensor_tensor(t6[xi][:], xt6[xi][:], ALPHA, acc[:, tl, :], ALU.mult, ALU.add), reads=['xt6_%d' % xi, 'acct%d' % tl], writes=['t6_%d' % xi])
                            slot = nxt('lnslot', 2)
                            mv, rs = ln_stats(lb6, t6[xi][:], 't6_%d' % xi, slot)
                            c.op('dve', lambda v: v.tensor_scalar(t6[xi][:], t6[xi][:], mv[:, 0:1], rs[:], ALU.subtract, ALU.mult), reads=['t6_%d' % xi, 'mv%d' % slot, 'rs%d' % slot], writes=['t6_%d' % xi])
                            c.op('pool', lambda g_: g_.tensor_tensor(t6[xi][:], t6[xi][:], lnp[:, 2, :], ALU.mult), reads=['t6_%d' % xi, 'lnp'], writes=['t6_%d' % xi])
                            c.op('pool', lambda g_: g_.tensor_tensor(xt6[xi][:], t6[xi][:], lnp[:, 3, :], ALU.add), reads=['t6_%d' % xi, 'lnp'], writes=['xt6_%d' % xi])
                            if last:
                                c.dma('sp', out[(gt_ - 2) * 128:(gt_ - 1) * 128, :], xt6[xi][:], reads=['xt6_%d' % xi])
                            else:
                                c.dma('sp', xs[gt_ * 128:(gt_ + 1) * 128, :], xt6[xi][:], reads=['xt6_%d' % xi])
            if stop == 'p6':
                break
        c.barrier()
    return nc


def _host_consts():
    ident = np.eye(128, dtype=np.float32)
    s = np.arange(128)[:, None]
    t = np.arange(128)[None, :]
    same = (s // 32) == (t // 32)
    masks = np.stack([(same & (s <= t)), (same & (s >= t))]).astype(np.int32)
    invc = np.zeros((4, PADL), np.float32)
    for g, win in enumerate((2, 4, 8, 16)):
        for (n, off) in ((TC, 32), (T, 352)):
            pos = np.arange(n)
            lo = np.clip(pos - win // 2, 0, n)
            hi = np.clip(pos - win // 2 + win, 0, n)
            invc[g, off:off + n] = 1.0 / (hi - lo).astype(np.float32)
    cm = (np.arange(128)[:, None] // 32 == np.arange(4)[None, :]).astype(np.float32)
    return ident, masks, invc, cm


def _bias_table(rpb):
    p = np.arange(128)
    krow_l = p // 64
    kc = p % 64
    qc = np.arange(64)
    c0 = np.clip(qc - 8, 0, 48)
    valid = (kc[:, None] >= c0[None, :]) & (kc[:, None] < c0[None, :] + 16)
    dc = np.clip(kc[:, None] - qc[None, :] + 15, 0, 30)
    tb = np.full((2, 8, 128, 8, 4, 64), NEG, np.float32)
    for delta in range(8):
        for j in range(4):
            dr = (2 * j + krow_l) - delta + 7
            val = rpb[:, :, dr[:, None], dc]
            tb[:, :, :, delta, j, :] = np.where(valid[None, None], val, NEG)
    return np.ascontiguousarray(tb.reshape(2, 8, 128, 8 * 4 * 64))


def prep_inputs(inp):
    f = lambda a: np.ascontiguousarray(np.asarray(a, dtype=np.float32))
    ident, masks, invc, cm = _host_consts()
    shared = {
        "w_ada": f(inp["w_ada"]), "b_ada": f(inp["b_ada"]),
        "b_ada_col": f(np.asarray(inp["b_ada"]).reshape(2, 48, 128).transpose(0, 2, 1)),
        "w_in": f(inp["w_in"]), "w_pool": f(inp["w_pool"]),
        "pscale_col": f(np.asarray(inp["pool_scale"]).reshape(2, 4, 128).transpose(0, 2, 1)),
        "lbl": f(np.stack([np.asarray(inp["lb_logits_fwd"]).reshape(2, 4, 128), np.asarray(inp["lb_logits_bwd"]).reshape(2, 4, 128)]).transpose(3, 0, 1, 2)),
        "gain_col": f(np.asarray(inp["hg_gain"]).transpose(0, 2, 1)),
        "tb": _bias_table(np.asarray(inp["rpb"], dtype=np.float32)),
        "w_br_a": f(inp["w_br_a"]), "w_br_b": f(inp["w_br_b"]), "w_br_c": f(inp["w_br_c"]), "w_out": f(inp["w_out"]),
        "lnp": f(np.stack([inp["ln1_g"], inp["ln1_b"], inp["ln2_g"], inp["ln2_b"]], axis=1)),
        "w_r": f(np.concatenate([inp["w_rg"], inp["w_re"]], axis=2)),
        "b_r": f(np.concatenate([inp["b_rg"], inp["b_re"]], axis=1)),
        "w_gate": f(inp["w_gate"]), "w_up": f(inp["w_up"]), "w_down": f(inp["w_down"]),
        "ident": ident, "masks": masks, "invc": invc, "cm": cm,
    }
    x = np.asarray(inp["x"], dtype=np.float32)
    ctx = np.asarray(inp["ctx"], dtype=np.float32)
    cc = np.asarray(inp["c"], dtype=np.float32)
    c_ctx = np.asarray(inp["c_ctx"], dtype=np.float32)
    maps = []
    for b in range(8):
        d = dict(shared)
        d["x"] = np.ascontiguousarray(x[b])
        d["ctx"] = np.ascontiguousarray(ctx[b])
        d["ccol"] = np.ascontiguousarray(np.stack([cc[b].reshape(8, 128).T, c_ctx.reshape(8, 128).T], axis=2))
        maps.append(d)
    return maps


def kernel(**inputs):
    nc = build()
    maps = prep_inputs(inputs)
    res = run_bass_kernel_spmd(nc, maps, core_ids=list(range(8)))
    return np.stack([np.asarray(r["out"], dtype=np.float32) for r in res.results], axis=0)
```
